# Optimizing a Trainium2 kernel written in Bass

```python
import math
import jax, jax.numpy as jnp
from jax import lax
import numpy as np

D_MODEL = 2048
BATCH = 4
SEQ = 2048
DEPTH = 2

GRID_W = 64
HEAD_DIM = 128
N_Q_HEADS = 12
N_KV_HEADS = 4
GQA_GROUP = N_Q_HEADS // N_KV_HEADS
ATTN_WIDTH = N_Q_HEADS * HEAD_DIM
KV_WIDTH = N_KV_HEADS * HEAD_DIM
Q_BLOCK = 128
ROPE_THETA = 10000.0
ROPE_AXIS_DIM = HEAD_DIM // 2
N_FOURIER_GROUPS = 4
FOURIER_GROUP = 128
FOURIER_WIDTH = N_FOURIER_GROUPS * FOURIER_GROUP
MIX_WIDTH = ATTN_WIDTH + FOURIER_WIDTH
IN_WIDTH = ATTN_WIDTH + 2 * KV_WIDTH + FOURIER_WIDTH
POOL_WINDOWS = (2, 4, 8, 16)
N_POOL_GROUPS = len(POOL_WINDOWS)
POOL_GROUP = D_MODEL // N_POOL_GROUPS
MEM_LEN = 256
N_MEM_HEADS = 4
MEM_HEAD_DIM = D_MODEL // N_MEM_HEADS
N_EXPERTS = 16
EXPERT_FF = D_MODEL // 2
EC_CAPACITY_FACTOR = 2
NORM_EPS = 1e-6
N_EVEN = (DEPTH + 1) // 2
N_ODD = DEPTH // 2

kernel_name = "hybrid_attn_fourier_pool_ec_moe_encoder"


def rms_norm(x, gain):
    xf = x.astype(jnp.float32)
    y = xf * lax.rsqrt(jnp.mean(xf * xf, axis=-1, keepdims=True) + NORM_EPS)
    return (y * gain.astype(jnp.float32)).astype(x.dtype)


def axial_rope_tables(seq_len):
    rows = seq_len // GRID_W
    row_idx = jnp.repeat(jnp.arange(rows), GRID_W).astype(jnp.float32)
    col_idx = jnp.tile(jnp.arange(GRID_W), rows).astype(jnp.float32)
    inv_freq = 1.0 / (ROPE_THETA ** (jnp.arange(0, ROPE_AXIS_DIM, 2, dtype=jnp.float32) / ROPE_AXIS_DIM))
    ang = jnp.concatenate([row_idx[:, None] * inv_freq[None, :],
                           col_idx[:, None] * inv_freq[None, :]], axis=-1)
    return jnp.cos(ang), jnp.sin(ang)


def apply_axial_rope(x, cos, sin):
    xf = x.astype(jnp.float32)
    x1 = xf[..., 0::2]
    x2 = xf[..., 1::2]
    c = cos[None, :, None, :]
    s = sin[None, :, None, :]
    out = jnp.stack([x1 * c - x2 * s, x1 * s + x2 * c], axis=-1).reshape(x.shape)
    return out.astype(x.dtype)


def blocked_bidirectional_gqa(q, k, v):
    b, s = q.shape[0], q.shape[1]
    n_blocks = s // Q_BLOCK
    scale = HEAD_DIM ** -0.5
    qb = q.reshape(b, n_blocks, Q_BLOCK, N_KV_HEADS, GQA_GROUP, HEAD_DIM).transpose(1, 0, 2, 3, 4, 5)

    def one_block(q_blk):
        scores = jnp.einsum('bqkgd,bskd->bkgqs', q_blk, k,
                            preferred_element_type=jnp.float32) * scale
        probs = jax.nn.softmax(scores, axis=-1).astype(v.dtype)
        return jnp.einsum('bkgqs,bskd->bqkgd', probs, v)

    o = lax.map(one_block, qb)
    return o.transpose(1, 0, 2, 3, 4, 5).reshape(b, s, ATTN_WIDTH)


def fourier_groups(u, w_fourier):
    z = jnp.fft.fft2(u.astype(jnp.float32), axes=(1, 3), norm='ortho').real
    return jnp.einsum('bsgc,gcd->bsgd', z.astype(u.dtype), w_fourier)


def attn_fourier_mixer(h, cos, sin, w_in, q_gain, k_gain, w_fourier, w_out):
    b, s, _ = h.shape
    proj = h @ w_in
    q, k, v, f = jnp.split(proj, [ATTN_WIDTH, ATTN_WIDTH + KV_WIDTH,
                                  ATTN_WIDTH + 2 * KV_WIDTH], axis=-1)
    q = apply_axial_rope(rms_norm(q.reshape(b, s, N_Q_HEADS, HEAD_DIM), q_gain), cos, sin)
    k = apply_axial_rope(rms_norm(k.reshape(b, s, N_KV_HEADS, HEAD_DIM), k_gain), cos, sin)
    q = q.reshape(b, s, N_KV_HEADS, GQA_GROUP, HEAD_DIM)
    v = v.reshape(b, s, N_KV_HEADS, HEAD_DIM)
    o_attn = blocked_bidirectional_gqa(q, k, v)
    o_four = fourier_groups(f.reshape(b, s, N_FOURIER_GROUPS, FOURIER_GROUP),
                            w_fourier).reshape(b, s, FOURIER_WIDTH)
    return jnp.concatenate([o_attn, o_four], axis=-1) @ w_out


def centred_mean_minus_self(u, window):
    b, s, c = u.shape
    t = jnp.arange(s)
    lo = jnp.clip(t - window // 2, 0, s)
    hi = jnp.clip(t + window - window // 2, 0, s)
    uf = u.astype(jnp.float32)
    cs = jnp.concatenate([jnp.zeros((b, 1, c), jnp.float32), jnp.cumsum(uf, axis=1)], axis=1)
    total = cs[:, hi] - cs[:, lo]
    count = (hi - lo).astype(jnp.float32)[None, :, None]
    return (total / count - uf).astype(u.dtype)


def pool_mixer(h, w_pool, pool_scale):
    b, s, _ = h.shape
    groups = h.reshape(b, s, N_POOL_GROUPS, POOL_GROUP)
    pooled = jnp.stack([centred_mean_minus_self(groups[:, :, g], w)
                        for g, w in enumerate(POOL_WINDOWS)], axis=2)
    mixed = jnp.einsum('bsgc,gcd->bsgd', pooled, w_pool).reshape(b, s, D_MODEL)
    return mixed * pool_scale


def memory_cross_attention(h, mem_n, w_q, w_k, w_v, w_o):
    b, s, _ = h.shape
    m = mem_n.shape[1]
    q = (h @ w_q).reshape(b, s, N_MEM_HEADS, MEM_HEAD_DIM)
    k = (mem_n @ w_k).reshape(b, m, N_MEM_HEADS, MEM_HEAD_DIM)
    v = (mem_n @ w_v).reshape(b, m, N_MEM_HEADS, MEM_HEAD_DIM)
    scores = jnp.einsum('bqhd,bmhd->bhqm', q, k,
                        preferred_element_type=jnp.float32) * (MEM_HEAD_DIM ** -0.5)
    probs = jax.nn.softmax(scores, axis=-1).astype(v.dtype)
    o = jnp.einsum('bhqm,bmhd->bqhd', probs, v).reshape(b, s, D_MODEL)
    return o @ w_o


def expert_choice_moe(h, w_router, w_gate, w_up, w_down):
    b, s, d = h.shape
    capacity = EC_CAPACITY_FACTOR * s // N_EXPERTS
    logits = jnp.einsum('bsd,de->bse', h, w_router, preferred_element_type=jnp.float32)
    affinity = jax.nn.softmax(logits, axis=-1)
    gate, idx = lax.top_k(affinity.transpose(0, 2, 1), capacity)
    xs = jax.vmap(lambda hb, ib: hb[ib])(h, idx)
    a = jnp.einsum('becd,edf->becf', xs, w_gate)
    u = jnp.einsum('becd,edf->becf', xs, w_up)
    y = jnp.einsum('becf,efd->becd', jax.nn.silu(a) * u, w_down)
    y = y * gate[..., None].astype(y.dtype)
    return jax.vmap(lambda yb, ib: jnp.zeros((s, d), yb.dtype)
                    .at[ib.reshape(-1)].add(yb.reshape(-1, d)))(y, idx)


def setup_inputs(seed: int = 0) -> dict:
    key = jax.random.key(seed)
    ks = jax.random.split(key, 24)
    f32 = jnp.float32

    def w(k, shape, fan_in):
        return jax.random.normal(k, shape, f32) * (fan_in ** -0.5)

    def gain(k, shape):
        return 1.0 + 0.02 * jax.random.normal(k, shape, f32)

    return {
        "x": jax.random.normal(ks[0], (BATCH, SEQ, D_MODEL), f32),
        "mem": jax.random.normal(ks[1], (BATCH, MEM_LEN, D_MODEL), f32),
        "mix_norm": gain(ks[2], (DEPTH, D_MODEL)),
        "attn_w_in": w(ks[3], (N_EVEN, D_MODEL, IN_WIDTH), D_MODEL),
        "q_gain": gain(ks[4], (N_EVEN, HEAD_DIM)),
        "k_gain": gain(ks[5], (N_EVEN, HEAD_DIM)),
        "fourier_w": w(ks[6], (N_EVEN, N_FOURIER_GROUPS, FOURIER_GROUP, FOURIER_GROUP), FOURIER_GROUP),
        "attn_w_out": w(ks[7], (N_EVEN, MIX_WIDTH, D_MODEL), MIX_WIDTH),
        "pool_w": w(ks[8], (N_ODD, N_POOL_GROUPS, POOL_GROUP, POOL_GROUP), POOL_GROUP),
        "pool_scale": gain(ks[9], (N_ODD, D_MODEL)),
        "cross_norm": gain(ks[10], (DEPTH, D_MODEL)),
        "mem_norm": gain(ks[11], (DEPTH, D_MODEL)),
        "cross_w_q": w(ks[12], (DEPTH, D_MODEL, D_MODEL), D_MODEL),
        "cross_w_k": w(ks[13], (DEPTH, D_MODEL, D_MODEL), D_MODEL),
        "cross_w_v": w(ks[14], (DEPTH, D_MODEL, D_MODEL), D_MODEL),
        "cross_w_o": w(ks[15], (DEPTH, D_MODEL, D_MODEL), D_MODEL),
        "ffn_norm": gain(ks[16], (DEPTH, D_MODEL)),
        "router_w": w(ks[17], (DEPTH, D_MODEL, N_EXPERTS), D_MODEL),
        "expert_w_gate": w(ks[18], (DEPTH, N_EXPERTS, D_MODEL, EXPERT_FF), D_MODEL),
        "expert_w_up": w(ks[19], (DEPTH, N_EXPERTS, D_MODEL, EXPERT_FF), D_MODEL),
        "expert_w_down": w(ks[20], (DEPTH, N_EXPERTS, EXPERT_FF, D_MODEL), EXPERT_FF),
        "final_norm": gain(ks[21], (D_MODEL,)),
    }


def reference(x, mem, mix_norm, attn_w_in, q_gain, k_gain, fourier_w, attn_w_out,
              pool_w, pool_scale, cross_norm, mem_norm, cross_w_q, cross_w_k,
              cross_w_v, cross_w_o, ffn_norm, router_w, expert_w_gate, expert_w_up,
              expert_w_down, final_norm):
    seq_len = x.shape[1]
    cos, sin = axial_rope_tables(seq_len)
    for layer in range(DEPTH):
        h = rms_norm(x, mix_norm[layer])
        i = layer // 2
        if layer % 2 == 0:
            x = x + attn_fourier_mixer(h, cos, sin, attn_w_in[i], q_gain[i], k_gain[i],
                                       fourier_w[i], attn_w_out[i])
        else:
            x = x + pool_mixer(h, pool_w[i], pool_scale[i])
        x = x + memory_cross_attention(rms_norm(x, cross_norm[layer]),
                                       rms_norm(mem, mem_norm[layer]),
                                       cross_w_q[layer], cross_w_k[layer],
                                       cross_w_v[layer], cross_w_o[layer])
        x = x + expert_choice_moe(rms_norm(x, ffn_norm[layer]), router_w[layer],
                                  expert_w_gate[layer], expert_w_up[layer],
                                  expert_w_down[layer])
    return rms_norm(x, final_norm)
```

```python
from contextlib import ExitStack
import numpy as np
import concourse.bass as bass
import concourse.mybir as mybir
from concourse.bass_utils import run_bass_kernel_spmd

F32 = mybir.dt.float32
BF16 = mybir.dt.bfloat16
I32 = mybir.dt.int32
U32 = mybir.dt.uint32
U8 = mybir.dt.uint8
ALU = mybir.AluOpType
AF = mybir.ActivationFunctionType
AX = mybir.AxisListType

S_ = 2048
D_ = 2048
NE = 16
CAP = 256
ENGS = ["sync", "scalar", "gpsimd", "vector", "tensor"]
DMA_K = 8


class Op:
    __slots__ = ("eng", "fn", "deps", "dma", "idx", "needed", "sig", "waits", "dma_j")

    def __init__(self, eng, fn, deps, dma):
        self.eng = eng
        self.fn = fn
        self.deps = deps
        self.dma = dma
        self.needed = False
        self.sig = None
        self.waits = []
        self.dma_j = None


class Sched:
    def __init__(self, nc):
        self.nc = nc
        self.ops = []
        self.last_w = {}
        self.readers = {}
        self.dma_count = {e: 0 for e in ENGS}
        self.dma_ops = {e: [] for e in ENGS}
        self.last_on = {e: None for e in ENGS}
        self.last_comp = {e: None for e in ENGS}
        self.fence = set()
        self.fenced = {e: True for e in ENGS}

    def barrier(self):
        f = set()
        for e in ENGS:
            if self.last_on[e] is not None:
                f.add(self.last_on[e])
            if self.last_comp[e] is not None:
                f.add(self.last_comp[e])
            n = self.dma_count[e]
            for j in range(max(0, n - DMA_K), n):
                f.add(self.dma_ops[e][j].idx)
        self.fence = f
        self.fenced = {e: False for e in ENGS}
        self.last_w = {}
        self.readers = {}

    def op(self, eng, fn, reads=(), writes=(), dma=False):
        pr = [r for r in reads if r.startswith("ps")]
        if pr:
            reads = [r for r in reads if not r.startswith("ps")]
            writes = list(writes) + pr
        deps = set()
        if not self.fenced[eng]:
            deps |= self.fence
            self.fenced[eng] = True
        for r in reads:
            w = self.last_w.get(r)
            if w is not None:
                deps.add(w)
        for w_ in writes:
            w = self.last_w.get(w_)
            if w is not None:
                deps.add(w)
            for r in self.readers.get(w_, ()):
                deps.add(r)
        o = Op(eng, fn, deps, dma)
        o.idx = len(self.ops)
        if dma:
            j = self.dma_count[eng]
            o.dma_j = j
            self.dma_count[eng] += 1
            if j >= DMA_K:
                deps.add(self.dma_ops[eng][j - DMA_K].idx)
            self.dma_ops[eng].append(o)
        deps.discard(o.idx)
        self.ops.append(o)
        self.last_on[eng] = o.idx
        if not dma:
            self.last_comp[eng] = o.idx
        for r in reads:
            self.readers.setdefault(r, []).append(o.idx)
        for w_ in writes:
            self.last_w[w_] = o.idx
            self.readers[w_] = []
        return o

    def dma(self, eng, out, in_, reads=(), writes=(), **kw):
        return self.op(eng, lambda e: e.dma_start(out=out, in_=in_, **kw), reads, writes, dma=True)

    def finalize(self):
        nc = self.nc
        ops = self.ops
        tail = []
        for e in ENGS:
            n = self.dma_count[e]
            for j in range(max(0, n - DMA_K), n):
                tail.append(self.dma_ops[e][j].idx)
        fin = Op("sync", None, set(tail), False)
        fin.idx = len(ops)
        ops.append(fin)
        for o in ops:
            for d in o.deps:
                p = ops[d]
                if p.eng == "tensor" and o.eng == "tensor" and not p.dma and not o.dma:
                    continue
                p.needed = True
        with ExitStack() as st:
            csem = {e: st.enter_context(nc.semaphore("c_" + e)) for e in ENGS}
            dsem = {e: [st.enter_context(nc.semaphore("d_%s%d" % (e, k))) for k in range(DMA_K)]
                    for e in ENGS if self.dma_count[e] > 0}
            cnt = {e: 0 for e in ENGS}
            for o in ops:
                if o.dma:
                    k = o.dma_j % DMA_K
                    o.sig = (dsem[o.eng][k], 16, 16 * (o.dma_j // DMA_K + 1))
                elif o.needed:
                    cnt[o.eng] += 1
                    o.sig = (csem[o.eng], 1, cnt[o.eng])
            seen = {e: {} for e in ENGS}
            for o in ops:
                need = {}
                for d in o.deps:
                    p = ops[d]
                    if p.sig is None:
                        continue
                    if p.eng == "tensor" and o.eng == "tensor" and not p.dma and not o.dma:
                        continue
                    sem, _, val = p.sig
                    key = id(sem)
                    if key not in need or need[key][1] < val:
                        need[key] = (sem, val)
                sn = seen[o.eng]
                for key, (sem, val) in need.items():
                    if sn.get(key, 0) >= val:
                        continue
                    sn[key] = val
                    o.waits.append((sem, val))
            by_eng = {e: [o for o in ops if o.eng == e] for e in ENGS}

            def emit(name, e):
                for o in by_eng[name]:
                    for sem, val in o.waits:
                        e.wait_ge(sem, val)
                    if o.fn is None:
                        continue
                    ins = o.fn(e)
                    if o.sig is not None:
                        ins.then_inc(o.sig[0], o.sig[1])

            with nc.Block() as block:
                @block.sync
                def _(e):
                    emit("sync", e)

                @block.scalar
                def _(e):
                    emit("scalar", e)

                @block.gpsimd
                def _(e):
                    emit("gpsimd", e)

                @block.vector
                def _(e):
                    emit("vector", e)

                @block.tensor
                def _(e):
                    emit("tensor", e)
        return {e: len(by_eng[e]) for e in ENGS}


ARENA = 207 * 1024


class B:
    def __init__(self, nc, st):
        self.nc = nc
        self.S = Sched(nc)
        self.arena = st.enter_context(nc.sbuf_tensor("arena", [128, ARENA], U8))
        self.off = 0
        self.mark = 0
        self.psf = [st.enter_context(nc.psum_tensor("ps%d" % i, [128, 512], F32)) for i in range(8)]
        self.wslot = 0
        self.uid = 0

    def alloc(self, name, free_shape, dt):
        n = 1
        for s in free_shape:
            n *= s
        nb = n * (2 if dt == BF16 else 4)
        nb = (nb + 63) // 64 * 64
        assert self.off + nb <= ARENA, (name, self.off, nb)
        v = self.arena[:, self.off:self.off + nb].bitcast(dt)
        self.off += nb
        nn = 1
        for s in free_shape:
            nn *= s
        v = v[:, 0:nn]
        if len(free_shape) == 2:
            v = v.rearrange("p (a b) -> p a b", a=free_shape[0])
        elif len(free_shape) == 3:
            v = v.rearrange("p (a b c) -> p a b c", a=free_shape[0], b=free_shape[1])
        return v

    def phase(self):
        self.S.barrier()
        self.off = self.mark

    def ps(self, i):
        return self.psf[i][:]

    def psb(self, i):
        return self.psf[i][:].bitcast(BF16).rearrange("p (a b) -> p a b", a=8)


def build_program(stage=99):
    nc = bass.Bass("TRN2", target_bir_lowering=False)

    def din(name, shape, dt=F32):
        return nc.dram_tensor(name, list(shape), dt, kind="ExternalInput").ap()

    def dscr(name, shape, dt):
        return nc.dram_tensor(name, list(shape), dt, kind="Internal").ap()

    x_in = din("x", [S_, D_])
    mem_in = din("mem", [256, D_])
    mix_norm = din("mix_norm", [2, D_])
    w_in = din("attn_w_in", [D_, 3072])
    q_gain = din("q_gain", [128])
    k_gain = din("k_gain", [128])
    fourier_w = din("fourier_w", [4, 128, 128])
    w_out = din("attn_w_out", [D_, D_])
    pool_w = din("pool_w", [4, 512, 512])
    pool_scale = din("pool_scale", [D_])
    cross_norm = din("cross_norm", [2, D_])
    mem_norm = din("mem_norm", [2, D_])
    cw_q = din("cross_w_q", [2, D_, D_])
    cw_k = din("cross_w_k", [2, D_, D_])
    cw_v = din("cross_w_v", [2, D_, D_])
    cw_o = din("cross_w_o", [2, D_, D_])
    ffn_norm = din("ffn_norm", [2, D_])
    router_w = din("router_w", [2, D_, NE])
    ew_gate = din("expert_w_gate", [2, NE, D_, 1024])
    ew_up = din("expert_w_up", [2, NE, D_, 1024])
    ew_down = din("expert_w_down", [2, NE, 1024, D_])
    final_norm = din("final_norm", [D_])
    c_rope = din("c_rope", [2, S_, 256])
    c_dftS = din("c_dftS", [2, S_, S_])
    c_dftC = din("c_dftC", [2, 128, 128])
    c_band = din("c_band", [4, 6, 128, 512])
    c_bedge = din("c_bedge", [4, 2, 128, 128])
    y_out = nc.dram_tensor("y", [S_, D_], F32, kind="ExternalOutput").ap()

    XA = dscr("XA", [S_, D_], F32)
    XB = dscr("XB", [S_, D_], F32)
    qT_d = dscr("qT_d", [12, 128, S_], BF16)
    kT_d = dscr("kT_d", [4, 128, S_], BF16)
    v_d = dscr("v_d", [S_, 512], BF16)
    fT_d = dscr("fT_d", [4, 128, S_], BF16)
    mixT_d = dscr("mixT_d", [16, 128, S_], BF16)
    hmoe_d = dscr("hmoe_d", [S_, D_], BF16)

    with ExitStack() as st:
        b = B(nc, st)
        S = b.S
        identf = b.alloc("identf", [128], F32)
        identb = b.alloc("identb", [128], BF16)
        onesb = b.alloc("onesb", [128], BF16)
        onesf = b.alloc("onesf", [128], F32)
        eps = b.alloc("eps", [1], F32)
        gb = b.alloc("gb", [D_], F32)
        S.op("gpsimd", lambda e: e.memset(identf, 0.0), writes=["identf"])
        S.op("gpsimd", lambda e: e.affine_select(out=identf, in_=identf, pattern=[[-1, 128]],
                                                 compare_op=ALU.not_equal, fill=1.0, base=0,
                                                 channel_multiplier=1), reads=["identf"], writes=["identf"])
        S.op("vector", lambda e: e.tensor_copy(out=identb, in_=identf), reads=["identf"], writes=["identb"])
        S.op("vector", lambda e: e.memset(onesb, 1.0), writes=["onesb"])
        S.op("vector", lambda e: e.memset(onesf, 1.0), writes=["onesf"])
        S.op("vector", lambda e: e.memset(eps, 1e-6), writes=["eps"])
        b.mark = b.off
        CONST = ["identf", "identb", "onesb", "onesf", "eps"]

        def keep_consts():
            pass

        def load_gain(gain_ap):
            S.dma("sync", gb, gain_ap.partition_broadcast(128), writes=["gb"])

        def norm_tile(src_rows, ti, xts, xn, ssq, rstd, out_fp32=None):
            xt = xts[ti % 2]
            xk = "xt%d" % (ti % 2)
            S.dma("sync", xt, src_rows, writes=[xk])
            S.op("scalar", lambda e: e.activation(out=xn, in_=xt, func=AF.Square, accum_out=ssq),
                 reads=[xk], writes=["xn", "ssq"])
            S.op("scalar", lambda e: e.activation(out=rstd, in_=ssq, func=AF.Sqrt, scale=1.0 / D_, bias=eps),
                 reads=["ssq"], writes=["rstd"])
            S.op("vector", lambda e: e.reciprocal(out=rstd, in_=rstd), reads=["rstd"], writes=["rstd"])
            if out_fp32 is not None:
                S.op("vector", lambda e: e.scalar_tensor_tensor(out=out_fp32, in0=xt, scalar=rstd, in1=gb,
                                                                op0=ALU.mult, op1=ALU.mult),
                     reads=[xk, "rstd", "gb"], writes=["xnf"])
                S.op("gpsimd", lambda e: e.tensor_copy(out=xn, in_=out_fp32), reads=["xnf"], writes=["xn"])
            else:
                S.op("vector", lambda e: e.scalar_tensor_tensor(out=xn, in0=xt, scalar=rstd, in1=gb,
                                                                op0=ALU.mult, op1=ALU.mult),
                     reads=[xk, "rstd", "gb"], writes=["xn"])

        def transpose_to(hT, col0, xn, kc=16):
            for half in range(kc // 8):
                pb = b.psb(half)
                pk = "ps%d" % half
                for k in range(8):
                    kk = half * 8 + k
                    S.op("tensor", lambda e, k=k, kk=kk, pb=pb: e.transpose(out=pb[:, k, :], in_=xn[:, kk * 128:(kk + 1) * 128],
                                                                           identity=identb),
                         reads=["xn"], writes=[pk])
                eng = "vector" if half == 0 else "scalar"
                dst = hT[:, half * 8:(half + 1) * 8, col0:col0 + 128]
                if eng == "vector":
                    S.op("vector", lambda e, dst=dst, pb=pb: e.tensor_copy(out=dst, in_=pb), reads=[pk], writes=["hT"])
                else:
                    S.op("scalar", lambda e, dst=dst, pb=pb: e.copy(out=dst, in_=pb), reads=[pk], writes=["hT"])

        def norm_loop(rows_of, n_tiles, hT, hkey="hT", junk=None):
            xts = [b.alloc("xt0", [D_], F32), b.alloc("xt1", [D_], F32)]
            xn2 = [b.alloc("xn0", [D_], BF16), b.alloc("xn1", [D_], BF16)]
            if junk is None:
                junk = b.alloc("njunk", [D_], BF16)
            ssq2 = [b.alloc("ssq0", [1], F32), b.alloc("ssq1", [1], F32)]
            rstd2 = [b.alloc("rstd0", [1], F32), b.alloc("rstd1", [1], F32)]

            def load(ti):
                S.dma("sync", xts[ti % 2], rows_of(ti), writes=["xt%d" % (ti % 2)])

            def early(ti):
                i = ti % 2
                xt, xn, ssq, rstd = xts[i], xn2[i], ssq2[i], rstd2[i]
                S.op("scalar", lambda e: e.activation(out=junk, in_=xt, func=AF.Square, accum_out=ssq),
                     reads=["xt%d" % i], writes=["njunk", "ssq%d" % i])
                S.op("scalar", lambda e: e.activation(out=rstd, in_=ssq, func=AF.Sqrt, scale=1.0 / D_, bias=eps),
                     reads=["ssq%d" % i], writes=["rstd%d" % i])
                S.op("vector", lambda e: e.reciprocal(out=rstd, in_=rstd), reads=["rstd%d" % i], writes=["rstd%d" % i])
                S.op("vector", lambda e: e.scalar_tensor_tensor(out=xn, in0=xt, scalar=rstd, in1=gb, op0=ALU.mult, op1=ALU.mult),
                     reads=["xt%d" % i, "rstd%d" % i, "gb"], writes=["xn%d" % i])

            def late(ti):
                i = ti % 2
                xn = xn2[i]
                for half in range(2):
                    pb = b.psb(half)
                    pk = "ps%d" % half
                    for k in range(8):
                        kk = half * 8 + k
                        S.op("tensor", lambda e, k=k, kk=kk, pb=pb: e.transpose(out=pb[:, k, :], in_=xn[:, kk * 128:(kk + 1) * 128],
                                                                               identity=identb),
                             reads=["xn%d" % i], writes=[pk])
                    dst = hT[:, half * 8:(half + 1) * 8, ti * 128:(ti + 1) * 128]
                    if half == 0:
                        S.op("vector", lambda e, dst=dst, pb=pb: e.tensor_copy(out=dst, in_=pb), reads=[pk], writes=[hkey])
                    else:
                        S.op("scalar", lambda e, dst=dst, pb=pb: e.copy(out=dst, in_=pb), reads=[pk], writes=[hkey])

            load(0)
            for ti in range(n_tiles + 1):
                if ti + 1 < n_tiles:
                    load(ti + 1)
                if ti < n_tiles:
                    early(ti)
                if ti >= 1:
                    late(ti - 1)

        def norm_phase_alloc():
            xts = [b.alloc("xt0", [D_], F32), b.alloc("xt1", [D_], F32)]
            xn = b.alloc("xn", [D_], BF16)
            ssq = b.alloc("ssq", [1], F32)
            rstd = b.alloc("rstd", [1], F32)
            return xts, xn, ssq, rstd

        def wpiece(src_ap, kdim=16):
            slot = b.wslot % len(wring)
            b.wslot += 1
            wt = wring[slot].rearrange("p (k n) -> p k n", k=kdim)
            S.dma("gpsimd", wt, src_ap, writes=["w%d" % slot])
            return wt, "w%d" % slot

        def linear_tok(hT, T, w_src, n_cols, epilogue, kc=16, banks=(2, 3, 4, 5, 6, 7), hkey="hT"):
            cnt = 0
            nblk = n_cols // 512
            nxt = wpiece(w_src(0).rearrange("(k p) n -> p k n", p=128))
            for nb in range(nblk):
                wt, wk = nxt
                if nb + 1 < nblk:
                    nxt = wpiece(w_src(nb + 1).rearrange("(k p) n -> p k n", p=128))
                for ti in range(T // 128):
                    bank = banks[cnt % len(banks)]
                    cnt += 1
                    pk = "ps%d" % bank
                    for k in range(kc):
                        S.op("tensor", lambda e, k=k, ti=ti, bank=bank, wt=wt: e.matmul(
                            b.ps(bank), lhsT=hT[:, k, ti * 128:(ti + 1) * 128], rhs=wt[:, k, :],
                            start=(k == 0), stop=(k == kc - 1)), reads=[hkey, wk], writes=[pk])
                    epilogue(nb, ti, b.ps(bank), pk)

        def linear_feat(hT, T, w_src, n_cols, epilogue, kc=16, banks=(2, 3, 4, 5, 6, 7), hkey="hT"):
            cnt = 0
            nblk = n_cols // 512
            nxt = wpiece(w_src(0).rearrange("(k p) n -> p k n", p=128))
            for nb in range(nblk):
                wt, wk = nxt
                if nb + 1 < nblk:
                    nxt = wpiece(w_src(nb + 1).rearrange("(k p) n -> p k n", p=128))
                for nt in range(4):
                    for tb in range(T // 512):
                        bank = banks[cnt % len(banks)]
                        cnt += 1
                        pk = "ps%d" % bank
                        for k in range(kc):
                            S.op("tensor", lambda e, k=k, nt=nt, tb=tb, bank=bank, wt=wt: e.matmul(
                                b.ps(bank), lhsT=wt[:, k, nt * 128:(nt + 1) * 128],
                                rhs=hT[:, k, tb * 512:(tb + 1) * 512],
                                start=(k == 0), stop=(k == kc - 1)), reads=[hkey, wk], writes=[pk])
                        epilogue(nb * 4 + nt, tb, b.ps(bank), pk)

        TG = 1024
        NTG = S_ // TG

        def make_resid_epilogue(X_src, X_dst, t_base, order, scale_b=None):
            NRX = 3
            rx = [b.alloc("rx%d" % i, [512], F32) for i in range(NRX)]
            ctr = [0]
            issued = [0]
            sc_tmp = b.alloc("sc_tmp", [512], F32) if scale_b is not None else None

            def issue_loads(upto):
                while issued[0] < min(upto, len(order)):
                    k = issued[0]
                    nb_, ti_ = order[k]
                    rows = slice(t_base + ti_ * 128, t_base + (ti_ + 1) * 128)
                    cols = slice(nb_ * 512, (nb_ + 1) * 512)
                    S.dma("sync", rx[k % NRX], X_src[rows, cols], writes=["rx%d" % (k % NRX)])
                    issued[0] += 1

            def ep(nb, ti, ps, pk):
                k = ctr[0]
                ctr[0] += 1
                assert order[k] == (nb, ti), (order[k], nb, ti)
                issue_loads(k + 2)
                i = k % NRX
                r = rx[i]
                rk = "rx%d" % i
                rows = slice(t_base + ti * 128, t_base + (ti + 1) * 128)
                cols = slice(nb * 512, (nb + 1) * 512)
                if scale_b is not None:
                    S.op("vector", lambda e: e.tensor_tensor(out=sc_tmp, in0=ps, in1=scale_b[:, cols], op=ALU.mult),
                         reads=[pk, "scale_b"], writes=["sc_tmp"])
                    S.op("gpsimd", lambda e: e.tensor_tensor(out=r, in0=sc_tmp, in1=r, op=ALU.add),
                         reads=["sc_tmp", rk], writes=[rk])
                else:
                    S.op("vector", lambda e: e.tensor_tensor(out=r, in0=ps, in1=r, op=ALU.add),
                         reads=[pk, rk], writes=[rk])
                S.dma("sync", X_dst[rows, cols], r, reads=[rk], writes=["Xdst"])
            return ep

        def phase_attn_proj():
            for tg in range(NTG):
                b.phase()
                global wring
                wring = [b.alloc("w%d" % i, [8192], BF16) for i in range(3)]
                hT = b.alloc("hT", [16, TG], BF16)
                load_gain(mix_norm[0])
                norm_loop(lambda ti: x_in[tg * TG + ti * 128: tg * TG + (ti + 1) * 128, :], TG // 128, hT)
                NB3 = 3
                stage = [b.alloc("stg0", [4, TG], BF16), b.alloc("stg1", [4, TG], BF16)]
                junk = [b.alloc("junk%d" % i, [512], BF16) for i in range(NB3)]
                xg = [b.alloc("xg%d" % i, [512], F32) for i in range(NB3)]
                ro = [b.alloc("ro%d" % i, [512], F32) for i in range(NB3)]
                tt = [[b.alloc("tt%d_%d" % (i, j), [256], F32) for j in range(4)] for i in range(NB3)]
                qr = [b.alloc("qr%d" % i, [512], BF16) for i in range(NB3)]
                ssq4 = [b.alloc("ssq4_%d" % i, [4], F32) for i in range(NB3)]
                rs4 = [b.alloc("rs4_%d" % i, [4], F32) for i in range(NB3)]
                gq4 = b.alloc("gq4", [512], F32)
                gk4 = b.alloc("gk4", [512], F32)
                cs = [b.alloc("cs0", [2, 256], F32), b.alloc("cs1", [2, 256], F32)]
                vst = [b.alloc("vst0", [512], BF16), b.alloc("vst1", [512], BF16)]
                for h in range(4):
                    S.dma("sync", gq4[:, h * 128:(h + 1) * 128], q_gain.partition_broadcast(128), writes=["gq4"])
                    S.dma("sync", gk4[:, h * 128:(h + 1) * 128], k_gain.partition_broadcast(128), writes=["gk4"])
                cctr = [0]
                stB = {}
                stC = {}

                def load_cs(n):
                    ti_ = n % (TG // 128)
                    rows = slice(tg * TG + ti_ * 128, tg * TG + (ti_ + 1) * 128)
                    S.dma("sync", cs[n % 2][:, 0, :], c_rope[0, rows, :], writes=["cs%d" % (n % 2)])
                    S.dma("sync", cs[n % 2][:, 1, :], c_rope[1, rows, :], writes=["cs%d" % (n % 2)])

                def run_stage(d, n):
                    f = d.pop(n, None)
                    if f is not None:
                        f()

                def flush():
                    n = cctr[0]
                    run_stage(stB, n - 1)
                    run_stage(stC, n - 2)
                    run_stage(stC, n - 1)

                def qk_epilogue(nb, ti, ps, pk):
                    gg, ggk = (gq4, "gq4") if nb < 3 else (gk4, "gk4")
                    n = cctr[0]
                    cctr[0] += 1
                    eb = n % NB3
                    c2 = cs[n % 2]
                    ck = "cs%d" % (n % 2)
                    if n == 0:
                        load_cs(0)
                    if n + 1 < 4 * (TG // 128):
                        load_cs(n + 1)
                    jk, sq, rs = junk[eb], ssq4[eb], rs4[eb]
                    x_, r_, q_ = xg[eb], ro[eb], qr[eb]
                    t1, t2, t3, t4 = tt[eb]
                    K = lambda nm: "%s%d" % (nm, eb)
                    for h in range(4):
                        S.op("scalar", lambda e, h=h: e.activation(out=jk[:, h * 128:(h + 1) * 128],
                                                                   in_=ps[:, h * 128:(h + 1) * 128], func=AF.Square,
                                                                   accum_out=sq[:, h:h + 1]),
                             reads=[pk], writes=[K("junk"), K("ssq4")])
                    S.op("scalar", lambda e: e.activation(out=rs, in_=sq, func=AF.Sqrt, scale=1.0 / 128, bias=eps),
                         reads=[K("ssq4")], writes=[K("rs4")])
                    S.op("vector", lambda e: e.tensor_tensor(out=x_, in0=ps, in1=gg, op=ALU.mult), reads=[pk, ggk], writes=[K("xg")])
                    xv = x_.rearrange("p (x two) -> p x two", two=2)
                    rv = r_.rearrange("p (x two) -> p x two", two=2)
                    x1 = xv[:, :, 0]
                    x2 = xv[:, :, 1]
                    cc = c2[:, 0, :]
                    ss = c2[:, 1, :]
                    S.op("gpsimd", lambda e: e.tensor_tensor(out=t2, in0=x2, in1=ss, op=ALU.mult), reads=[K("xg"), ck], writes=[K("t2")])
                    S.op("gpsimd", lambda e: e.tensor_tensor(out=t3, in0=x1, in1=ss, op=ALU.mult), reads=[K("xg"), ck], writes=[K("t3")])
                    S.op("gpsimd", lambda e: e.tensor_tensor(out=t4, in0=x2, in1=cc, op=ALU.mult), reads=[K("xg"), ck], writes=[K("t4")])
                    S.op("vector", lambda e: e.tensor_tensor(out=t1, in0=x1, in1=cc, op=ALU.mult), reads=[K("xg"), ck], writes=[K("t1")])

                    def stage_b():
                        S.op("vector", lambda e: e.reciprocal(out=rs, in_=rs), reads=[K("rs4")], writes=[K("rs4")])
                        S.op("vector", lambda e: e.tensor_tensor(out=rv[:, :, 0], in0=t1, in1=t2, op=ALU.subtract),
                             reads=[K("t1"), K("t2")], writes=[K("ro")])
                        S.op("vector", lambda e: e.tensor_tensor(out=rv[:, :, 1], in0=t3, in1=t4, op=ALU.add),
                             reads=[K("t3"), K("t4")], writes=[K("ro")])
                        for h in range(4):
                            S.op("vector", lambda e, h=h: e.tensor_scalar(out=q_[:, h * 128:(h + 1) * 128], in0=r_[:, h * 128:(h + 1) * 128],
                                                                         scalar1=rs[:, h:h + 1], scalar2=None, op0=ALU.mult),
                                 reads=[K("ro"), K("rs4")], writes=[K("qr%d" % h)])

                    pb = b.psb(n % 2)
                    pbk = "ps%d" % (n % 2)
                    sg = stage[nb % 2]

                    def stage_c():
                        for h in range(4):
                            S.op("tensor", lambda e, h=h: e.transpose(out=pb[:, h, :], in_=q_[:, h * 128:(h + 1) * 128], identity=identb),
                                 reads=[K("qr%d" % h)], writes=[pbk])
                        S.op("scalar", lambda e: e.copy(out=sg[:, :, ti * 128:(ti + 1) * 128], in_=pb[:, 0:4, :]),
                             reads=[pbk], writes=["stg%d" % (nb % 2)])
                        if ti == TG // 128 - 1:
                            cols = slice(tg * TG, (tg + 1) * TG)
                            if nb < 3:
                                dst = qT_d[nb * 4:(nb + 1) * 4, :, cols]
                            else:
                                dst = kT_d[:, :, cols]
                            S.dma("sync", dst.rearrange("h p t -> p h t"), sg, reads=["stg%d" % (nb % 2)], writes=["qkT_d"])

                    stB[n] = stage_b
                    stC[n] = stage_c
                    run_stage(stB, n - 1)
                    run_stage(stC, n - 2)

                def w_src(nb):
                    return w_in[:, nb * 512:(nb + 1) * 512]

                linear_tok(hT, TG, w_src, 4 * 512, qk_epilogue)
                flush()

                def v_epilogue(nb, ti, ps, pk):
                    vb = vst[ti % 2]
                    S.op("scalar", lambda e: e.copy(out=vb, in_=ps), reads=[pk], writes=["vst%d" % (ti % 2)])
                    S.dma("sync", v_d[tg * TG + ti * 128: tg * TG + (ti + 1) * 128, :], vb, reads=["vst%d" % (ti % 2)], writes=["v_d"])

                linear_tok(hT, TG, lambda nb: w_in[:, 2048:2560], 512, v_epilogue)

                fst = [b.alloc("fst0", [512], BF16), b.alloc("fst1", [512], BF16)]
                fctr = [0]

                def f_epilogue(nt, tb, ps, pk):
                    i = fctr[0] % 2
                    fctr[0] += 1
                    S.op("vector", lambda e: e.tensor_copy(out=fst[i], in_=ps), reads=[pk], writes=["fst%d" % i])
                    S.dma("sync", fT_d[nt, :, tg * TG + tb * 512: tg * TG + (tb + 1) * 512], fst[i],
                          reads=["fst%d" % i], writes=["fT_d"])

                linear_feat(hT, TG, lambda nb: w_in[:, 2560:3072], 512, f_epilogue)

        def phase_attention():
            b.phase()
            kT = b.alloc("kT", [4, S_], BF16)
            V = b.alloc("V", [16, 512], BF16)
            qT = [b.alloc("qT0", [S_], BF16), b.alloc("qT1", [S_], BF16)]
            eT = [b.alloc("eT%d" % i, [512], BF16) for i in range(4)]
            rsum = [b.alloc("rsum0", [512], F32), b.alloc("rsum1", [512], F32)]
            ost = [b.alloc("ost0", [512], BF16), b.alloc("ost1", [512], BF16)]
            S.dma("sync", kT, kT_d.rearrange("h p t -> p h t"), reads=["qkT_d"], writes=["kT"])
            S.dma("sync", V, v_d.rearrange("(j p) d -> p j d", p=128), reads=["v_d"], writes=["V"])
            scale = 128 ** -0.5
            items = [(h, qb, st_) for h in range(12) for qb in range(4) for st_ in range(16)]

            def load_q(h):
                S.dma("sync", qT[h % 2], qT_d[h], reads=["qkT_d"], writes=["qT%d" % (h % 2)])

            def emit_sc(i):
                h, qb, st_ = items[i]
                g = h // 3
                q = qT[h % 2]
                scb = 2 + (i % 2)
                S.op("tensor", lambda e: e.matmul(
                    b.ps(scb), lhsT=kT[:, g, st_ * 128:(st_ + 1) * 128], rhs=q[:, qb * 512:(qb + 1) * 512],
                    start=True, stop=True), reads=["kT", "qT%d" % (h % 2)], writes=["ps%d" % scb])

            load_q(0)
            emit_sc(0)
            for i, (h, qb, st_) in enumerate(items):
                g = h // 3
                blk = i // 16
                ob = 4 + (blk % 2)
                sb_ = 6 + (blk % 2)
                scb = 2 + (i % 2)
                ei = i % 4
                if qb == 0 and st_ == 0 and h + 1 < 12:
                    load_q(h + 1)
                if i + 1 < len(items):
                    emit_sc(i + 1)
                S.op("scalar", lambda e, scb=scb, ei=ei: e.activation(out=eT[ei], in_=b.ps(scb), func=AF.Exp, scale=scale),
                     reads=["ps%d" % scb], writes=["eT%d" % ei])
                S.op("tensor", lambda e, st_=st_, ob=ob, ei=ei, g=g: e.matmul(
                    b.ps(ob), lhsT=V[:, st_, g * 128:(g + 1) * 128], rhs=eT[ei], start=(st_ == 0), stop=(st_ == 15)),
                     reads=["V", "eT%d" % ei], writes=["ps%d" % ob])
                S.op("tensor", lambda e, st_=st_, sb_=sb_, ei=ei: e.matmul(
                    b.ps(sb_), lhsT=onesb, rhs=eT[ei], start=(st_ == 0), stop=(st_ == 15)),
                     reads=["eT%d" % ei], writes=["ps%d" % sb_])
                if st_ == 15:
                    rs = rsum[blk % 2]
                    rk = "rsum%d" % (blk % 2)
                    o = ost[blk % 2]
                    okk = "ost%d" % (blk % 2)
                    S.op("vector", lambda e, sb_=sb_, rs=rs: e.reciprocal(out=rs, in_=b.ps(sb_)), reads=["ps%d" % sb_], writes=[rk])
                    S.op("vector", lambda e, ob=ob, o=o, rs=rs: e.tensor_tensor(out=o, in0=b.ps(ob), in1=rs, op=ALU.mult),
                         reads=["ps%d" % ob, rk], writes=[okk])
                    S.dma("sync", mixT_d[h, :, qb * 512:(qb + 1) * 512], o, reads=[okk], writes=["mixT_d"])

        def phase_fourier():
            b.phase()
            global wring
            wring = [b.alloc("w%d" % i, [8192], BF16) for i in range(4)]
            dC = b.alloc("dC", [2, 128], F32)
            fw = b.alloc("fw", [4, 128], F32)
            AB = b.alloc("AB", [4, 256], BF16)
            fT = b.alloc("fT", [4, S_], BF16)
            P12 = b.alloc("P12", [16, 4 * 256], BF16)
            ost = [b.alloc("ost0", [512], BF16), b.alloc("ost1", [512], BF16)]
            S.dma("sync", dC, c_dftC.rearrange("a p c -> p a c"), writes=["dC"])
            S.dma("sync", fw, fourier_w.rearrange("g p c -> p g c"), writes=["fw"])
            S.dma("sync", fT, fT_d.rearrange("g p t -> p g t"), reads=["fT_d"], writes=["fT"])
            for g in range(4):
                for a in range(2):
                    S.op("tensor", lambda e, g=g, a=a: e.matmul(b.ps(2)[:, a * 128:(a + 1) * 128], lhsT=dC[:, a, :], rhs=fw[:, g, :],
                                                                start=True, stop=True), reads=["dC", "fw"], writes=["ps2"])
                S.op("vector", lambda e, g=g: e.tensor_copy(out=AB[:, g, :], in_=b.ps(2)[:, 0:256]), reads=["ps2"], writes=["AB"])
            for j in range(16):
                for half in range(2):
                    bank = 3 + half
                    for gg in range(2):
                        g = half * 2 + gg
                        S.op("tensor", lambda e, g=g, gg=gg, j=j, bank=bank: e.matmul(
                            b.ps(bank)[:, gg * 256:(gg + 1) * 256], lhsT=fT[:, g, j * 128:(j + 1) * 128], rhs=AB[:, g, :],
                            start=True, stop=True), reads=["fT", "AB"], writes=["ps%d" % bank])
                    eng = "vector" if half == 0 else "scalar"
                    dst = P12[:, j, half * 512:(half + 1) * 512]
                    if eng == "vector":
                        S.op("vector", lambda e, dst=dst, bank=bank: e.tensor_copy(out=dst, in_=b.ps(bank)), reads=["ps%d" % bank], writes=["P12"])
                    else:
                        S.op("scalar", lambda e, dst=dst, bank=bank: e.copy(out=dst, in_=b.ps(bank)), reads=["ps%d" % bank], writes=["P12"])
            oc = 0
            for tb in range(4):
                cw, ck = wpiece(c_dftS[0][:, tb * 512:(tb + 1) * 512].rearrange("(k p) n -> p k n", p=128))
                sw, sk = wpiece(c_dftS[1][:, tb * 512:(tb + 1) * 512].rearrange("(k p) n -> p k n", p=128))
                for g in range(4):
                    bank = 5 + (oc % 2)
                    for j in range(16):
                        S.op("tensor", lambda e, g=g, j=j, bank=bank, cw=cw: e.matmul(
                            b.ps(bank), lhsT=P12[:, j, g * 256:g * 256 + 128], rhs=cw[:, j, :], start=(j == 0), stop=False),
                             reads=["P12", ck], writes=["ps%d" % bank])
                    for j in range(16):
                        S.op("tensor", lambda e, g=g, j=j, bank=bank, sw=sw: e.matmul(
                            b.ps(bank), lhsT=P12[:, j, g * 256 + 128:g * 256 + 256], rhs=sw[:, j, :], start=False, stop=(j == 15)),
                             reads=["P12", sk], writes=["ps%d" % bank])
                    o = ost[oc % 2]
                    okk = "ost%d" % (oc % 2)
                    oc += 1
                    S.op("vector", lambda e, o=o, bank=bank: e.tensor_copy(out=o, in_=b.ps(bank)), reads=["ps%d" % bank], writes=[okk])
                    S.dma("sync", mixT_d[12 + g, :, tb * 512:(tb + 1) * 512], o, reads=[okk], writes=["mixT_d"])

        def phase_outproj(W, X_src, X_dst):
            b.phase()
            wres = [b.alloc("wres%d" % i, [16, 512], BF16) for i in range(4)]
            for nb in range(4):
                S.dma("gpsimd", wres[nb], W[:, nb * 512:(nb + 1) * 512].rearrange("(k p) n -> p k n", p=128), writes=["wres%d" % nb])
            hTs = [b.alloc("hTa", [16, 512], BF16), b.alloc("hTb", [16, 512], BF16)]
            rx = [b.alloc("rxa", [D_], F32), b.alloc("rxb", [D_], F32), b.alloc("rxc", [D_], F32)]
            NT = S_ // 128
            cnt = 0

            def load_r(ti):
                S.dma("sync", rx[ti % 3], X_src[ti * 128:(ti + 1) * 128, :], writes=["rx%d" % (ti % 3)])

            def load_h(tb):
                S.dma("sync", hTs[tb % 2], mixT_d[:, :, tb * 512:(tb + 1) * 512].rearrange("k p t -> p k t"), reads=["mixT_d"],
                      writes=["hTs%d" % (tb % 2)])

            load_h(0)
            load_r(0)
            load_r(1)
            for ti in range(NT):
                tb = ti // 4
                if ti % 4 == 0 and tb + 1 < NT // 4:
                    load_h(tb + 1)
                if ti + 2 < NT:
                    load_r(ti + 2)
                hT = hTs[tb % 2]
                hk = "hTs%d" % (tb % 2)
                r = rx[ti % 3]
                rk = "rx%d" % (ti % 3)
                tc = (ti % 4) * 128
                for nb in range(4):
                    bank = cnt % 8
                    cnt += 1
                    for k in range(16):
                        S.op("tensor", lambda e, k=k, nb=nb, bank=bank, hT=hT, tc=tc: e.matmul(
                            b.ps(bank), lhsT=hT[:, k, tc:tc + 128], rhs=wres[nb][:, k, :], start=(k == 0), stop=(k == 15)),
                             reads=[hk, "wres%d" % nb], writes=["ps%d" % bank])
                    S.op("vector", lambda e, nb=nb, bank=bank, r=r: e.tensor_tensor(out=r[:, nb * 512:(nb + 1) * 512], in0=b.ps(bank),
                                                                                 in1=r[:, nb * 512:(nb + 1) * 512], op=ALU.add),
                         reads=["ps%d" % bank, rk], writes=[rk])
                S.dma("sync", X_dst[ti * 128:(ti + 1) * 128, :], r, reads=[rk], writes=["Xdst"])

        def phase_cross(layer, X_src, X_dst):
            b.phase()
            global wring
            wring = [b.alloc("w%d" % i, [8192], BF16) for i in range(3)]
            kTm = b.alloc("kTm", [16, 256], BF16)
            Vm = b.alloc("Vm", [2, D_], BF16)
            memT = b.alloc("hT", [16, 256], BF16)
            mark_inner = b.off
            load_gain(mem_norm[layer])
            norm_loop(lambda ti: mem_in[ti * 128:(ti + 1) * 128, :], 2, memT)

            def k_ep(ntile, tb, ps, pk):
                S.op("vector", lambda e: e.tensor_copy(out=kTm[:, ntile, :], in_=ps[:, 0:256]), reads=[pk], writes=["kTm"])

            cnt = 0
            for nb in range(4):
                wt, wk = wpiece(cw_k[layer][:, nb * 512:(nb + 1) * 512].rearrange("(k p) n -> p k n", p=128))
                for nt in range(4):
                    bank = 2 + (cnt % 4)
                    cnt += 1
                    for k in range(16):
                        S.op("tensor", lambda e, k=k, nt=nt, bank=bank, wt=wt: e.matmul(
                            b.ps(bank)[:, 0:256], lhsT=wt[:, k, nt * 128:(nt + 1) * 128], rhs=memT[:, k, :],
                            start=(k == 0), stop=(k == 15)), reads=["hT", wk], writes=["ps%d" % bank])
                    k_ep(nb * 4 + nt, 0, b.ps(bank), "ps%d" % bank)

            def v_ep(nb, ti, ps, pk):
                S.op("scalar", lambda e: e.copy(out=Vm[:, ti, nb * 512:(nb + 1) * 512], in_=ps), reads=[pk], writes=["Vm"])

            linear_tok(memT, 256, lambda nb: cw_v[layer][:, nb * 512:(nb + 1) * 512], D_, v_ep)
            scale = 512 ** -0.5
            for tg in range(NTG):
                S.barrier()
                b.off = mark_inner
                hT = b.alloc("hTq", [16, TG], BF16)
                ocT = b.alloc("ocT", [16, TG], BF16)
                qTh = [b.alloc("qTh0", [4, TG], BF16), b.alloc("qTh1", [4, TG], BF16)]
                ejunk = b.alloc("ejunk", [D_], BF16)
                eT = [ejunk[:, i * 512:(i + 1) * 512] for i in range(4)]
                rsum = b.alloc("rsum", [512], F32)
                load_gain(cross_norm[layer])
                norm_loop(lambda ti: X_src[tg * TG + ti * 128: tg * TG + (ti + 1) * 128, :], TG // 128, hT, junk=ejunk)
                rsum2 = [rsum, b.alloc("rsum_b", [512], F32)]
                qw = {}

                def q_load(h):
                    qw[h] = wpiece(cw_q[layer][:, h * 512:(h + 1) * 512].rearrange("(k p) n -> p k n", p=128))

                qcnt = [0]

                def q_proj(h, part):
                    wt, wk = qw[h]
                    qh = qTh[h % 2]
                    qk = "qTh%d" % (h % 2)
                    for nt in (2 * part, 2 * part + 1):
                        for tb in range(TG // 512):
                            bank = qcnt[0] % 2
                            qcnt[0] += 1
                            for k in range(16):
                                S.op("tensor", lambda e, k=k, nt=nt, tb=tb, bank=bank, wt=wt: e.matmul(
                                    b.ps(bank), lhsT=wt[:, k, nt * 128:(nt + 1) * 128], rhs=hT[:, k, tb * 512:(tb + 1) * 512],
                                    start=(k == 0), stop=(k == 15)), reads=["hT", wk], writes=["ps%d" % bank])
                            dst = qh[:, nt, tb * 512:(tb + 1) * 512]
                            if bank == 0:
                                S.op("vector", lambda e, dst=dst, bank=bank: e.tensor_copy(out=dst, in_=b.ps(bank)), reads=["ps%d" % bank], writes=[qk])
                            else:
                                S.op("scalar", lambda e, dst=dst, bank=bank: e.copy(out=dst, in_=b.ps(bank)), reads=["ps%d" % bank], writes=[qk])

                ectr = [0]
                pvc = [0]

                def scores(h, tb):
                    qh = qTh[h % 2]
                    qk = "qTh%d" % (h % 2)
                    es = []
                    for mt in range(2):
                        scb = 2 + mt
                        ei = ectr[0] % 4
                        ectr[0] += 1
                        es.append(ei)
                        for dk in range(4):
                            S.op("tensor", lambda e, dk=dk, mt=mt, scb=scb: e.matmul(
                                b.ps(scb), lhsT=kTm[:, h * 4 + dk, mt * 128:(mt + 1) * 128],
                                rhs=qh[:, dk, tb * 512:(tb + 1) * 512], start=(dk == 0), stop=(dk == 3)),
                                 reads=["kTm", qk], writes=["ps%d" % scb])
                        S.op("scalar", lambda e, scb=scb, ei=ei: e.activation(out=eT[ei], in_=b.ps(scb), func=AF.Exp, scale=scale),
                             reads=["ps%d" % scb], writes=["eT%d" % ei])
                    return es

                def pv(h, tb, es):
                    rs = rsum2[(h * 2 + tb) % 2]
                    rk = "rsum%d" % ((h * 2 + tb) % 2)
                    for mt in range(2):
                        S.op("tensor", lambda e, mt=mt, ei=es[mt]: e.matmul(b.ps(4), lhsT=onesb, rhs=eT[ei], start=(mt == 0), stop=(mt == 1)),
                             reads=["eT%d" % es[mt]], writes=["ps4"])
                    S.op("vector", lambda e: e.reciprocal(out=rs, in_=b.ps(4)), reads=["ps4"], writes=[rk])
                    for dv in range(4):
                        pbk = 6 + (pvc[0] % 2)
                        pvc[0] += 1
                        for mt in range(2):
                            S.op("tensor", lambda e, mt=mt, dv=dv, pbk=pbk, ei=es[mt]: e.matmul(
                                b.ps(pbk), lhsT=Vm[:, mt, (h * 4 + dv) * 128:(h * 4 + dv + 1) * 128], rhs=eT[ei],
                                start=(mt == 0), stop=(mt == 1)), reads=["Vm", "eT%d" % es[mt]], writes=["ps%d" % pbk])
                        S.op("vector", lambda e, dv=dv, pbk=pbk: e.tensor_tensor(
                            out=ocT[:, h * 4 + dv, tb * 512:(tb + 1) * 512], in0=b.ps(pbk), in1=rs, op=ALU.mult),
                             reads=["ps%d" % pbk, rk], writes=["ocT"])

                q_load(0)
                q_load(1)
                q_proj(0, 0)
                q_proj(0, 1)
                for h in range(4):
                    if h + 2 < 4:
                        q_load(h + 2)
                    for tb in range(TG // 512):
                        es = scores(h, tb)
                        if h + 1 < 4:
                            q_proj(h + 1, tb)
                        pv(h, tb, es)
                ep = make_resid_epilogue(X_src, X_dst, tg * TG, [(nb, ti) for nb in range(4) for ti in range(TG // 128)])
                linear_tok(ocT, TG, lambda nb: cw_o[layer][:, nb * 512:(nb + 1) * 512], D_, ep, hkey="ocT")

        def phase_moe(layer, X):
            b.phase()
            global wring
            wring = [b.alloc("w%d" % i, [8192], BF16) for i in range(6)]
            for i_ in range(6):
                src_, kd_ = ((ew_gate if i_ in (0, 2) else ew_up)[layer, 0][:, (i_ // 2) * 512:(i_ // 2 + 1) * 512].rearrange("(k p) n -> p k n", p=128), 16) \
                    if i_ < 4 else (ew_down[layer, 0][:, (i_ - 4) * 1024:(i_ - 3) * 1024].rearrange("(k p) n -> p k n", p=128), 8)
                S.dma("gpsimd", wring[i_].rearrange("p (k n) -> p k n", k=kd_), src_, writes=["w%d" % i_])
            logT = b.alloc("logT", [S_], F32)
            mark_inner = b.off
            xts = [b.alloc("xt0", [D_], F32), b.alloc("xt1", [D_], F32)]
            xnb = [b.alloc("xn0", [D_], BF16), b.alloc("xn1", [D_], BF16)]
            xnf2 = [b.alloc("xnf0", [D_], F32), b.alloc("xnf1", [D_], F32)]
            junk = b.alloc("njunk", [D_], BF16)
            ssq2 = [b.alloc("ssq0", [1], F32), b.alloc("ssq1", [1], F32)]
            rstd2 = [b.alloc("rstd0", [1], F32), b.alloc("rstd1", [1], F32)]
            hTf = b.alloc("hTf", [16, 128], F32)
            wrA = b.alloc("wrA", [16, 48], F32)
            wrB = b.alloc("wrB", [16, 48], F32)
            load_gain(ffn_norm[layer])
            S.op("vector", lambda e: e.memset(wrA, 0.0), writes=["wr"])
            S.op("vector", lambda e: e.memset(wrB, 0.0), writes=["wr"])
            S.dma("sync", wrA[:, :, 0:16], router_w[layer].rearrange("(k p) e -> p k e", p=128), writes=["wr"])
            S.dma("sync", wrB[:, :, 32:48], router_w[layer].rearrange("(k p) e -> p k e", p=128), writes=["wr"])
            S.op("vector", lambda e: e.memset(logT[0:48, 0:1024], -30000.0), writes=["logT"])

            def m_load(ti):
                S.dma("sync", xts[ti % 2], X[ti * 128:(ti + 1) * 128, :], writes=["xt%d" % (ti % 2)])

            def m_early(ti):
                i = ti % 2
                rows = slice(ti * 128, (ti + 1) * 128)
                xt, xn, xnf, ssq, rstd = xts[i], xnb[i], xnf2[i], ssq2[i], rstd2[i]
                S.op("scalar", lambda e: e.activation(out=junk, in_=xt, func=AF.Square, accum_out=ssq),
                     reads=["xt%d" % i], writes=["njunk", "ssq%d" % i])
                S.op("scalar", lambda e: e.activation(out=rstd, in_=ssq, func=AF.Sqrt, scale=1.0 / D_, bias=eps),
                     reads=["ssq%d" % i], writes=["rstd%d" % i])
                S.op("vector", lambda e: e.reciprocal(out=rstd, in_=rstd), reads=["rstd%d" % i], writes=["rstd%d" % i])
                S.op("vector", lambda e: e.scalar_tensor_tensor(out=xnf, in0=xt, scalar=rstd, in1=gb, op0=ALU.mult, op1=ALU.mult),
                     reads=["xt%d" % i, "rstd%d" % i, "gb"], writes=["xnf%d" % i])
                S.op("gpsimd", lambda e: e.tensor_copy(out=xn, in_=xnf), reads=["xnf%d" % i], writes=["xn%d" % i])
                S.dma("sync", hmoe_d[rows, :], xn, reads=["xn%d" % i], writes=["hmoe_d"])

            def m_late(ti):
                i = ti % 2
                xnf = xnf2[i]
                for q4 in range(4):
                    bank = 2 + q4
                    for k in range(4):
                        kk = q4 * 4 + k
                        S.op("tensor", lambda e, k=k, kk=kk, bank=bank: e.transpose(
                            out=b.ps(bank)[:, k * 128:(k + 1) * 128], in_=xnf[:, kk * 128:(kk + 1) * 128], identity=identf),
                             reads=["xnf%d" % i], writes=["ps%d" % bank])
                    dst = hTf[:, q4 * 4:(q4 + 1) * 4, :]
                    if q4 % 2 == 0:
                        S.op("vector", lambda e, dst=dst, bank=bank: e.tensor_copy(
                            out=dst, in_=b.ps(bank).rearrange("p (a b) -> p a b", a=4)), reads=["ps%d" % bank], writes=["hTf%d" % q4])
                    else:
                        S.op("scalar", lambda e, dst=dst, bank=bank: e.copy(
                            out=dst, in_=b.ps(bank).rearrange("p (a b) -> p a b", a=4)), reads=["ps%d" % bank], writes=["hTf%d" % q4])
                wsel = wrA if ti < 8 else wrB
                for k in range(16):
                    S.op("tensor", lambda e, k=k, wsel=wsel: e.matmul(b.ps(6)[0:48, 0:128], lhsT=wsel[:, k, :], rhs=hTf[:, k, :],
                                                                     start=(k == 0), stop=(k == 15)), reads=["wr", "hTf%d" % (k // 4)], writes=["ps6"])
                p0 = 0 if ti < 8 else 32
                tcol = (ti % 8) * 128
                S.op("vector", lambda e, p0=p0, tcol=tcol: e.tensor_copy(out=logT[p0:p0 + 16, tcol:tcol + 128], in_=b.ps(6)[p0:p0 + 16, 0:128]),
                     reads=["ps6"], writes=["logT"])

            m_load(0)
            for ti in range(S_ // 128 + 1):
                if ti + 1 < S_ // 128:
                    m_load(ti + 1)
                if ti < S_ // 128:
                    m_early(ti)
                if ti >= 1:
                    m_late(ti - 1)
            S.barrier()
            b.off = mark_inner
            HS = S_ // 2
            aff = b.alloc("aff", [HS], F32)
            work = b.alloc("work", [HS], F32)
            vals = b.alloc("vals", [CAP], F32)
            idxu = b.alloc("idxu", [CAP], U32)
            idxf = b.alloc("idxf", [CAP], F32)
            ob = b.alloc("ob", [48], F32)
            offs = b.alloc("offs", [1], F32)
            Jm = b.alloc("Jm", [128], F32)
            VT = b.alloc("VT", [2, 48], F32)
            IT = b.alloc("IT", [2, 48], F32)
            BrV = b.alloc("BrV", [2, NE], F32)
            BrI = b.alloc("BrI", [2, NE], F32)
            msk = b.alloc("msk", [2, NE], F32)
            dif = b.alloc("dif", [2, NE], F32)
            gateT = b.alloc("gateT", [2, NE], F32)
            idxT = b.alloc("idxT", [2, NE], I32)
            S.op("gpsimd", lambda e: e.memset(ob[0:48, :], 0.0), writes=["ob"])
            S.op("gpsimd", lambda e: e.memset(ob[0:32, 0:32], 1.0), writes=["ob"])
            S.op("gpsimd", lambda e: e.memset(ob[32:48, 32:48], 1.0), writes=["ob"])
            S.op("gpsimd", lambda e: e.memset(offs[0:48, :], 0.0), writes=["offs"])
            S.op("gpsimd", lambda e: e.memset(offs[32:48, :], float(HS)), writes=["offs"])
            S.op("gpsimd", lambda e: e.memset(Jm, 0.0), writes=["Jm"])
            S.op("gpsimd", lambda e: e.affine_select(out=Jm, in_=Jm, pattern=[[1, 128]], compare_op=ALU.not_equal, fill=1.0,
                                                     base=-127, channel_multiplier=1), reads=["Jm"], writes=["Jm"])
            S.op("scalar", lambda e: e.activation(out=aff[0:48, :], in_=logT[0:48, 0:HS], func=AF.Exp), reads=["logT"], writes=["aff"])
            for tb in range(2):
                S.op("tensor", lambda e, tb=tb: e.matmul(b.ps(2)[0:48, :], lhsT=ob[0:48, 0:48], rhs=aff[0:48, tb * 512:(tb + 1) * 512],
                                                         start=True, stop=True), reads=["aff", "ob"], writes=["ps2"])
                S.op("vector", lambda e, tb=tb: e.reciprocal(out=work[0:48, tb * 512:(tb + 1) * 512], in_=b.ps(2)[0:48, :]),
                     reads=["ps2"], writes=["work"])
            S.op("vector", lambda e: e.tensor_tensor(out=aff[0:48, :], in0=aff[0:48, :], in1=work[0:48, :], op=ALU.mult),
                 reads=["aff", "work"], writes=["aff"])
            S.op("vector", lambda e: e.tensor_copy(out=work[0:48, :], in_=aff[0:48, :]), reads=["aff"], writes=["work"])
            for r in range(CAP // 8):
                S.op("vector", lambda e, r=r: e.max(out=vals[0:48, r * 8:(r + 1) * 8], in_=work[0:48, :]), reads=["work"], writes=["vals"])
                S.op("vector", lambda e, r=r: e.max_index(out=idxu[0:48, r * 8:(r + 1) * 8], in_max=vals[0:48, r * 8:(r + 1) * 8],
                                                          in_values=work[0:48, :]), reads=["work", "vals"], writes=["idxu"])
                S.op("vector", lambda e, r=r: e.match_replace(out=work[0:48, :], in_to_replace=vals[0:48, r * 8:(r + 1) * 8],
                                                              in_values=work[0:48, :], imm_value=-1.0),
                     reads=["work", "vals", "idxu"], writes=["work"])
            S.op("vector", lambda e: e.tensor_copy(out=idxf[0:48, :], in_=idxu[0:48, :]), reads=["idxu"], writes=["idxf"])
            S.op("vector", lambda e: e.tensor_scalar(out=idxf[0:48, :], in0=idxf[0:48, :], scalar1=0.0, scalar2=float(HS - 1),
                                                     op0=ALU.max, op1=ALU.min), reads=["idxf"], writes=["idxf"])
            S.op("vector", lambda e: e.tensor_scalar(out=idxf[0:48, :], in0=idxf[0:48, :], scalar1=offs[0:48, :], scalar2=None,
                                                     op0=ALU.add), reads=["idxf", "offs"], writes=["idxf"])
            for cc in range(2):
                S.op("tensor", lambda e, cc=cc: e.transpose(out=b.ps(3)[:, 0:48], in_=vals[0:48, cc * 128:(cc + 1) * 128],
                                                            identity=identf[0:48, 0:48]), reads=["vals"], writes=["ps3"])
                S.op("vector", lambda e, cc=cc: e.tensor_copy(out=VT[:, cc, :], in_=b.ps(3)[:, 0:48]), reads=["ps3"], writes=["VT"])
                S.op("tensor", lambda e, cc=cc: e.transpose(out=b.ps(4)[:, 0:48], in_=idxf[0:48, cc * 128:(cc + 1) * 128],
                                                            identity=identf[0:48, 0:48]), reads=["idxf"], writes=["ps4"])
                S.op("vector", lambda e, cc=cc: e.tensor_copy(out=IT[:, cc, :], in_=b.ps(4)[:, 0:48]), reads=["ps4"], writes=["IT"])
            for cc in range(2):
                S.op("tensor", lambda e, cc=cc: e.matmul(b.ps(3)[:, 0:16], lhsT=Jm, rhs=VT[:, 1 - cc, 32:48], start=True, stop=True),
                     reads=["Jm", "VT"], writes=["ps3"])
                S.op("vector", lambda e, cc=cc: e.tensor_copy(out=BrV[:, cc, :], in_=b.ps(3)[:, 0:16]), reads=["ps3"], writes=["BrV"])
                S.op("tensor", lambda e, cc=cc: e.matmul(b.ps(4)[:, 0:16], lhsT=Jm, rhs=IT[:, 1 - cc, 32:48], start=True, stop=True),
                     reads=["Jm", "IT"], writes=["ps4"])
                S.op("vector", lambda e, cc=cc: e.tensor_copy(out=BrI[:, cc, :], in_=b.ps(4)[:, 0:16]), reads=["ps4"], writes=["BrI"])
            S.op("vector", lambda e: e.tensor_tensor(out=gateT, in0=VT[:, :, 0:16], in1=BrV, op=ALU.max), reads=["VT", "BrV"], writes=["gateT"])
            S.op("vector", lambda e: e.tensor_tensor(out=msk, in0=VT[:, :, 0:16], in1=BrV, op=ALU.is_gt), reads=["VT", "BrV"], writes=["msk"])
            S.op("vector", lambda e: e.tensor_tensor(out=dif, in0=IT[:, :, 0:16], in1=BrI, op=ALU.subtract), reads=["IT", "BrI"], writes=["dif"])
            S.op("vector", lambda e: e.tensor_tensor(out=dif, in0=dif, in1=msk, op=ALU.mult), reads=["dif", "msk"], writes=["dif"])
            S.op("vector", lambda e: e.tensor_tensor(out=dif, in0=dif, in1=BrI, op=ALU.add), reads=["dif", "BrI"], writes=["dif"])
            S.op("vector", lambda e: e.tensor_copy(out=idxT, in_=dif), reads=["dif"], writes=["idxT"])
            if stage == 30:
                return
            xs = [b.alloc("xs%d" % i, [D_], BF16) for i in range(4)]
            xsT = [b.alloc("xsT0", [16, CAP], BF16), b.alloc("xsT1", [16, CAP], BF16)]
            gT = b.alloc("gT", [8, CAP], BF16)
            sa = [b.alloc("sa0", [CAP], F32), b.alloc("sa1", [CAP], F32)]
            ysb = [b.alloc("ysb0", [D_], F32), b.alloc("ysb1", [D_], F32)]

            def piece_src(ex, i):
                if i in (0, 2):
                    fh = i // 2
                    return ew_gate[layer, ex][:, fh * 512:(fh + 1) * 512].rearrange("(k p) n -> p k n", p=128), 16
                if i in (1, 3):
                    fh = i // 2
                    return ew_up[layer, ex][:, fh * 512:(fh + 1) * 512].rearrange("(k p) n -> p k n", p=128), 16
                nh = i - 4
                return ew_down[layer, ex][:, nh * 1024:(nh + 1) * 1024].rearrange("(k p) n -> p k n", p=128), 8

            def load_piece(ex, i):
                src, kd = piece_src(ex, i)
                S.dma("gpsimd", wring[i].rearrange("p (k n) -> p k n", k=kd), src, writes=["w%d" % i])

            def piece(i, kd):
                return wring[i].rearrange("p (k n) -> p k n", k=kd), "w%d" % i

            def gather(ex):
                for cc in range(2):
                    xi = (2 * ex + cc) % 4
                    S.op("gpsimd", lambda e, xi=xi, cc=cc, ex=ex: e.indirect_dma_start(
                        out=xs[xi], out_offset=None, in_=hmoe_d[:, :],
                        in_offset=bass.IndirectOffsetOnAxis(ap=idxT[:, cc, ex:ex + 1], axis=0)),
                         reads=["hmoe_d", "idxT"], writes=["xs%d" % xi], dma=True)

            def transposes(ex):
                xT = xsT[ex % 2]
                xTk = "xsT%d" % (ex % 2)
                for cc in range(2):
                    xi = (2 * ex + cc) % 4
                    xsb = xs[xi]
                    xk = "xs%d" % xi
                    for half in range(2):
                        pb = b.psb(half)
                        for k in range(8):
                            kk = half * 8 + k
                            S.op("tensor", lambda e, k=k, kk=kk, pb=pb, xsb=xsb: e.transpose(
                                out=pb[:, k, :], in_=xsb[:, kk * 128:(kk + 1) * 128], identity=identb),
                                 reads=[xk], writes=["ps%d" % half])
                        dst = xT[:, half * 8:(half + 1) * 8, cc * 128:(cc + 1) * 128]
                        if half == 0:
                            S.op("vector", lambda e, dst=dst, pb=pb: e.tensor_copy(out=dst, in_=pb), reads=["ps0"], writes=[xTk])
                        else:
                            S.op("scalar", lambda e, dst=dst, pb=pb: e.copy(out=dst, in_=pb), reads=["ps1"], writes=[xTk])

            hcn = [0]

            def hidden(ex, fh):
                xT = xsT[ex % 2]
                xTk = "xsT%d" % (ex % 2)
                wg, wgk = piece(2 * fh, 16)
                wu, wuk = piece(2 * fh + 1, 16)
                for ft in range(4):
                    hc = hcn[0]
                    hcn[0] += 1
                    bg = 2 + 2 * (hc % 2)
                    bu = bg + 1
                    si = hc % 2
                    for k in range(16):
                        S.op("tensor", lambda e, k=k, ft=ft, bg=bg, wg=wg, xT=xT: e.matmul(
                            b.ps(bg)[:, 0:256], lhsT=wg[:, k, ft * 128:(ft + 1) * 128], rhs=xT[:, k, :],
                            start=(k == 0), stop=(k == 15)), reads=[wgk, xTk], writes=["ps%d" % bg])
                    for k in range(16):
                        S.op("tensor", lambda e, k=k, ft=ft, bu=bu, wu=wu, xT=xT: e.matmul(
                            b.ps(bu)[:, 0:256], lhsT=wu[:, k, ft * 128:(ft + 1) * 128], rhs=xT[:, k, :],
                            start=(k == 0), stop=(k == 15)), reads=[wuk, xTk], writes=["ps%d" % bu])
                    S.op("scalar", lambda e, bg=bg, si=si: e.activation(out=sa[si], in_=b.ps(bg)[:, 0:256], func=AF.Silu),
                         reads=["ps%d" % bg], writes=["sa%d" % si])
                    S.op("vector", lambda e, bu=bu, si=si, fh=fh, ft=ft: e.tensor_tensor(
                        out=gT[:, fh * 4 + ft, :], in0=b.ps(bu)[:, 0:256], in1=sa[si], op=ALU.mult),
                         reads=["ps%d" % bu, "sa%d" % si], writes=["gT"])

            dcn = [0]

            def down(ex, nh):
                wdv, wdk = piece(4 + nh, 8)
                for cc in range(2):
                    for nbk in range(2):
                        bank = 6 + (dcn[0] % 2)
                        dcn[0] += 1
                        for ft in range(8):
                            S.op("tensor", lambda e, ft=ft, cc=cc, nbk=nbk, bank=bank, wdv=wdv: e.matmul(
                                b.ps(bank), lhsT=gT[:, ft, cc * 128:(cc + 1) * 128], rhs=wdv[:, ft, nbk * 512:(nbk + 1) * 512],
                                start=(ft == 0), stop=(ft == 7)), reads=["gT", wdk], writes=["ps%d" % bank])
                        dst = ysb[cc][:, nh * 1024 + nbk * 512: nh * 1024 + (nbk + 1) * 512]
                        if nbk == 0:
                            S.op("vector", lambda e, dst=dst, bank=bank, cc=cc, ex=ex: e.tensor_scalar(
                                out=dst, in0=b.ps(bank), scalar1=gateT[:, cc, ex:ex + 1], scalar2=None, op0=ALU.mult),
                                 reads=["ps%d" % bank, "gateT"], writes=["ysb%d" % cc])
                        else:
                            S.op("scalar", lambda e, dst=dst, bank=bank, cc=cc, ex=ex: e.activation(
                                out=dst, in_=b.ps(bank), func=AF.Copy, scale=gateT[:, cc, ex:ex + 1]),
                                 reads=["ps%d" % bank, "gateT"], writes=["ysb%d" % cc])

            def scatter(ex):
                for cc in range(2):
                    S.op("gpsimd", lambda e, cc=cc, ex=ex: e.indirect_dma_start(
                        out=X[:, :], out_offset=bass.IndirectOffsetOnAxis(ap=idxT[:, cc, ex:ex + 1], axis=0),
                        in_=ysb[cc], in_offset=None, compute_op=ALU.add),
                         reads=["ysb%d" % cc, "idxT"], writes=["Xmoe"], dma=True)

            gather(0)
            for ex in range(NE):
                nxt = ex + 1 < NE
                if nxt:
                    gather(ex + 1)
                transposes(ex)
                hidden(ex, 0)
                if nxt:
                    load_piece(ex + 1, 0)
                    load_piece(ex + 1, 1)
                hidden(ex, 1)
                if nxt:
                    load_piece(ex + 1, 2)
                    load_piece(ex + 1, 3)
                down(ex, 0)
                if nxt:
                    load_piece(ex + 1, 4)
                down(ex, 1)
                if nxt:
                    load_piece(ex + 1, 5)
                scatter(ex)

        def phase_pool(X_src, X_dst):
            b.phase()
            global wring
            wring = [b.alloc("w%d" % i, [2048], BF16) for i in range(4)]
            hT = b.alloc("hT", [16, S_], BF16)
            xa = b.alloc("xa", [16, D_], BF16)
            strips = b.alloc("strips", [4, 6 * 512], BF16)
            edges = b.alloc("edges", [4, 2 * 128], BF16)
            scale_b = [b.alloc("scale_b0", [512], F32), b.alloc("scale_b1", [512], F32)]
            load_gain(mix_norm[1])
            wts = [wring[g].rearrange("p (k n) -> p k n", k=4) for g in range(4)]

            def load_consts():
                S.dma("gpsimd", strips.rearrange("p w (k n) -> p w k n", k=6), c_band.rearrange("w k p n -> p w k n"), writes=["strips"])
                S.dma("gpsimd", edges.rearrange("p w (k n) -> p w k n", k=2), c_bedge.rearrange("w k p n -> p w k n"), writes=["edges"])
                for g in range(4):
                    S.dma("gpsimd", wts[g][:, 0:4, :], pool_w[g].rearrange("(k p) n -> p k n", p=128), writes=["w%d" % g])
                for g in range(4):
                    sbg = scale_b[g % 2]
                    S.dma("gpsimd", sbg, pool_scale[g * 512:(g + 1) * 512].partition_broadcast(128), writes=["scale_b%d" % (g % 2)])
                    for k in range(4):
                        S.op("gpsimd", lambda e, g=g, k=k, sbg=sbg: e.tensor_tensor(out=wts[g][:, k, :], in0=wts[g][:, k, :], in1=sbg, op=ALU.mult),
                             reads=["w%d" % g, "scale_b%d" % (g % 2)], writes=["w%d" % g])
            xts = [b.alloc("xt0", [D_], F32), b.alloc("xt1", [D_], F32)]
            ssq2 = [b.alloc("ssq0", [1], F32), b.alloc("ssq1", [1], F32)]
            rstd2 = [b.alloc("rstd0", [1], F32), b.alloc("rstd1", [1], F32)]
            NT = S_ // 128
            S.dma("sync", xts[0], X_src[0:128, :], writes=["xt0"])
            for ti in range(NT):
                i = ti % 2
                xt, ssq, rstd = xts[i], ssq2[i], rstd2[i]
                if ti + 1 < NT:
                    S.dma("sync", xts[1 - i], X_src[(ti + 1) * 128:(ti + 2) * 128, :], writes=["xt%d" % (1 - i)])
                S.op("scalar", lambda e, xt=xt, ssq=ssq, ti=ti: e.activation(out=xa[:, ti, :], in_=xt, func=AF.Square, accum_out=ssq),
                     reads=["xt%d" % i], writes=["xa%d" % ti, "ssq%d" % i])
                S.op("scalar", lambda e, ssq=ssq, rstd=rstd: e.activation(out=rstd, in_=ssq, func=AF.Sqrt, scale=1.0 / D_, bias=eps),
                     reads=["ssq%d" % i], writes=["rstd%d" % i])
                S.op("vector", lambda e, rstd=rstd: e.reciprocal(out=rstd, in_=rstd), reads=["rstd%d" % i], writes=["rstd%d" % i])
                S.op("vector", lambda e, xt=xt, rstd=rstd, ti=ti: e.scalar_tensor_tensor(out=xa[:, ti, :], in0=xt, scalar=rstd, in1=gb,
                                                                                        op0=ALU.mult, op1=ALU.mult),
                     reads=["xt%d" % i, "rstd%d" % i, "gb"], writes=["xa%d" % ti])
                if ti == 2:
                    load_consts()
            sv = strips.rearrange("p w (k n) -> p w k n", k=6)
            ev = edges.rearrange("p w (k n) -> p w k n", k=2)
            pc = 0
            for J in range(4):
                for ch in range(16):
                    wi = ch // 4
                    bank = pc % 8
                    pc += 1
                    pk = "ps%d" % bank
                    for pos in range(4):
                        Sx = 4 * J + pos
                        kidx = pos
                        if Sx == 0:
                            kidx = 4
                        elif Sx == NT - 1:
                            kidx = 5
                        S.op("tensor", lambda e, Sx=Sx, ch=ch, wi=wi, kidx=kidx, bank=bank, pos=pos: e.matmul(
                            b.ps(bank), lhsT=xa[:, Sx, ch * 128:(ch + 1) * 128], rhs=sv[:, wi, kidx, :],
                            start=(pos == 0), stop=False), reads=["xa%d" % Sx, "strips"], writes=[pk])
                    last_full = True
                    if J > 0:
                        S.op("tensor", lambda e, J=J, ch=ch, wi=wi, bank=bank: e.matmul(
                            b.ps(bank)[:, 0:128], lhsT=xa[:, 4 * J - 1, ch * 128:(ch + 1) * 128], rhs=ev[:, wi, 0, :],
                            start=False, stop=False), reads=["xa%d" % (4 * J - 1), "edges"], writes=[pk])
                    if J < 3:
                        S.op("tensor", lambda e, J=J, ch=ch, wi=wi, bank=bank: e.matmul(
                            b.ps(bank)[:, 384:512], lhsT=xa[:, 4 * J + 4, ch * 128:(ch + 1) * 128], rhs=ev[:, wi, 1, :],
                            start=False, stop=False), reads=["xa%d" % (4 * J + 4), "edges"], writes=[pk])
                    dst = hT[:, ch, J * 512:(J + 1) * 512]
                    if pc % 2 == 0:
                        S.op("vector", lambda e, dst=dst, bank=bank: e.tensor_copy(out=dst, in_=b.ps(bank)), reads=[pk], writes=["hTc%d_%d" % (ch, J)])
                    else:
                        S.op("scalar", lambda e, dst=dst, bank=bank: e.copy(out=dst, in_=b.ps(bank)), reads=[pk], writes=["hTc%d_%d" % (ch, J)])
            for ti in range(S_ // 128):
                i = ti % 2
                r = xts[i]
                rk = "xt%d" % i
                rows = slice(ti * 128, (ti + 1) * 128)
                S.dma("sync", r, X_src[rows, :], writes=[rk])
                for g in range(4):
                    bank = (ti * 4 + g) % 8
                    for k in range(4):
                        S.op("tensor", lambda e, k=k, ti=ti, g=g, bank=bank: e.matmul(
                            b.ps(bank), lhsT=hT[:, g * 4 + k, ti * 128:(ti + 1) * 128], rhs=wts[g][:, k, :],
                            start=(k == 0), stop=(k == 3)), reads=["hTc%d_%d" % (g * 4 + k, ti // 4), "w%d" % g], writes=["ps%d" % bank])
                    S.op("vector", lambda e, g=g, bank=bank, r=r: e.tensor_tensor(out=r[:, g * 512:(g + 1) * 512], in0=b.ps(bank),
                                                                               in1=r[:, g * 512:(g + 1) * 512], op=ALU.add),
                         reads=["ps%d" % bank, rk], writes=[rk])
                S.dma("sync", X_dst[rows, :], r, reads=[rk], writes=["Xdst"])

        def phase_final(X_src):
            b.phase()
            xts = [b.alloc("xt0", [D_], F32), b.alloc("xt1", [D_], F32)]
            junk = b.alloc("njunk", [D_], BF16)
            ssq2 = [b.alloc("ssq0", [1], F32), b.alloc("ssq1", [1], F32)]
            rstd2 = [b.alloc("rstd0", [1], F32), b.alloc("rstd1", [1], F32)]
            yo = [b.alloc("yo0", [D_], F32), b.alloc("yo1", [D_], F32)]
            load_gain(final_norm)
            for ti in range(S_ // 128):
                rows = slice(ti * 128, (ti + 1) * 128)
                i = ti % 2
                xt, ssq, rstd, y = xts[i], ssq2[i], rstd2[i], yo[i]
                if ti == 0:
                    S.dma("sync", xt, X_src[rows, :], writes=["xt%d" % i])
                if ti + 1 < S_ // 128:
                    S.dma("sync", xts[1 - i], X_src[(ti + 1) * 128:(ti + 2) * 128, :], writes=["xt%d" % (1 - i)])
                S.op("scalar", lambda e, xt=xt, ssq=ssq: e.activation(out=junk, in_=xt, func=AF.Square, accum_out=ssq),
                     reads=["xt%d" % i], writes=["njunk", "ssq%d" % i])
                S.op("scalar", lambda e, ssq=ssq, rstd=rstd: e.activation(out=rstd, in_=ssq, func=AF.Sqrt, scale=1.0 / D_, bias=eps),
                     reads=["ssq%d" % i], writes=["rstd%d" % i])
                S.op("vector", lambda e, rstd=rstd: e.reciprocal(out=rstd, in_=rstd), reads=["rstd%d" % i], writes=["rstd%d" % i])
                S.op("vector", lambda e, xt=xt, y=y, rstd=rstd: e.scalar_tensor_tensor(out=y, in0=xt, scalar=rstd, in1=gb, op0=ALU.mult, op1=ALU.mult),
                     reads=["xt%d" % i, "rstd%d" % i, "gb"], writes=["yo%d" % i])
                S.dma("sync", y_out[rows, :], y, reads=["yo%d" % i], writes=["y"])

        def copy_out(X_src):
            b.phase()
            t = [b.alloc("co0", [D_], F32), b.alloc("co1", [D_], F32)]
            for ti in range(S_ // 128):
                rows = slice(ti * 128, (ti + 1) * 128)
                S.dma("sync", t[ti % 2], X_src[rows, :], writes=["co%d" % (ti % 2)])
                S.dma("sync", y_out[rows, :], t[ti % 2], reads=["co%d" % (ti % 2)], writes=["y"])

        def run():
            phase_attn_proj()
            phase_attention()
            phase_fourier()
            phase_outproj(w_out, x_in, XB)
            if stage == 1:
                return copy_out(XB)
            phase_cross(0, XB, XA)
            if stage == 2:
                return copy_out(XA)
            phase_moe(0, XA)
            if stage == 30:
                return
            if stage == 3:
                return copy_out(XA)
            phase_pool(XA, XB)
            if stage == 4:
                return copy_out(XB)
            phase_cross(1, XB, XA)
            if stage == 5:
                return copy_out(XA)
            phase_moe(1, XA)
            if stage == 6:
                return copy_out(XA)
            phase_final(XA)

        run()
        counts = S.finalize()
        print("instr counts", counts, flush=True)
    return nc


wring = []


def host_constants():
    S = S_
    rows = S // 64
    row_idx = np.repeat(np.arange(rows), 64).astype(np.float32)
    col_idx = np.tile(np.arange(64), rows).astype(np.float32)
    inv_freq = (1.0 / (10000.0 ** (np.arange(0, 64, 2, dtype=np.float32) / 64))).astype(np.float32)
    ang = np.concatenate([row_idx[:, None] * inv_freq[None, :], col_idx[:, None] * inv_freq[None, :]], axis=-1)
    cos = np.cos(ang).astype(np.float32)
    sin = np.sin(ang).astype(np.float32)
    c_rope = np.stack([np.tile(cos, (1, 4)), np.tile(sin, (1, 4))]).astype(np.float32)
    n = np.arange(S, dtype=np.int64)
    ph = (np.outer(n, n) % S).astype(np.float64) * (2 * np.pi / S)
    c_dftS = np.stack([np.cos(ph), np.sin(ph)]).astype(np.float32)
    c = np.arange(128, dtype=np.int64)
    pc = (np.outer(c, c) % 128).astype(np.float64) * (2 * np.pi / 128)
    c_dftC = np.stack([np.cos(pc) / 512.0, -np.sin(pc) / 512.0]).astype(np.float32)
    t = np.arange(S)
    bands = []
    bedges = []
    for w in (2, 4, 8, 16):
        lo = np.clip(t - w // 2, 0, S)
        hi = np.clip(t + w - w // 2, 0, S)
        M = np.zeros((S, S), np.float32)
        for tt_ in range(S):
            M[lo[tt_]:hi[tt_], tt_] = 1.0 / float(hi[tt_] - lo[tt_])
        M[np.arange(S), np.arange(S)] -= 1.0
        st = [M[(4 + p) * 128:(5 + p) * 128, 512:1024] for p in range(4)]
        st.append(M[0:128, 0:512])
        st.append(M[S - 128:S, S - 512:S])
        bands.append(np.stack(st))
        bedges.append(np.stack([M[3 * 128:4 * 128, 512:640], M[8 * 128:9 * 128, 7 * 128:8 * 128]]))
    c_band = np.stack(bands).astype(np.float32)
    c_bedge = np.stack(bedges).astype(np.float32)
    return {"c_rope": c_rope, "c_dftS": c_dftS, "c_dftC": c_dftC, "c_band": c_band, "c_bedge": c_bedge}


def make_in_maps(inputs, n_cores=8):
    g = {k: np.ascontiguousarray(np.asarray(v), dtype=np.float32) for k, v in inputs.items()}
    shared = {
        "mix_norm": g["mix_norm"], "attn_w_in": g["attn_w_in"][0], "q_gain": g["q_gain"][0], "k_gain": g["k_gain"][0],
        "fourier_w": g["fourier_w"][0], "attn_w_out": g["attn_w_out"][0], "pool_w": g["pool_w"][0],
        "pool_scale": g["pool_scale"][0], "cross_norm": g["cross_norm"], "mem_norm": g["mem_norm"],
        "cross_w_q": g["cross_w_q"], "cross_w_k": g["cross_w_k"], "cross_w_v": g["cross_w_v"], "cross_w_o": g["cross_w_o"],
        "ffn_norm": g["ffn_norm"], "router_w": g["router_w"], "expert_w_gate": g["expert_w_gate"],
        "expert_w_up": g["expert_w_up"], "expert_w_down": g["expert_w_down"], "final_norm": g["final_norm"],
    }
    shared.update(host_constants())
    maps = []
    for c in range(n_cores):
        m = dict(shared)
        m["x"] = g["x"][c % 4]
        m["mem"] = g["mem"][c % 4]
        maps.append(m)
    return maps


def kernel(**inputs):
    nc = build_program()
    maps = make_in_maps(inputs, 4)
    res = run_bass_kernel_spmd(nc, maps, core_ids=list(range(4)))
    out = np.stack([np.asarray(res.results[c]["y"], dtype=np.float32) for c in range(4)], axis=0)
    return out
```

```python
from contextlib import ExitStack
import numpy as np
import concourse.bass as bass
import concourse.mybir as mybir
from concourse.bass_utils import run_bass_kernel_spmd

F32 = mybir.dt.float32
BF16 = mybir.dt.bfloat16
I32 = mybir.dt.int32
U32 = mybir.dt.uint32
U8 = mybir.dt.uint8
ALU = mybir.AluOpType
AF = mybir.ActivationFunctionType
AX = mybir.AxisListType

S_ = 2048
D_ = 2048
NE = 16
CAP = 256
ENGS = ["sync", "scalar", "gpsimd", "vector", "tensor"]
DMA_K = 8


class Op:
    __slots__ = ("eng", "fn", "deps", "dma", "idx", "needed", "sig", "waits", "dma_j")

    def __init__(self, eng, fn, deps, dma):
        self.eng = eng
        self.fn = fn
        self.deps = deps
        self.dma = dma
        self.needed = False
        self.sig = None
        self.waits = []
        self.dma_j = None


class Sched:
    def __init__(self, nc):
        self.nc = nc
        self.ops = []
        self.last_w = {}
        self.readers = {}
        self.dma_count = {e: 0 for e in ENGS}
        self.dma_ops = {e: [] for e in ENGS}
        self.last_on = {e: None for e in ENGS}
        self.last_comp = {e: None for e in ENGS}
        self.fence = set()
        self.fenced = {e: True for e in ENGS}

    def barrier(self):
        f = set()
        for e in ENGS:
            if self.last_on[e] is not None:
                f.add(self.last_on[e])
            if self.last_comp[e] is not None:
                f.add(self.last_comp[e])
            n = self.dma_count[e]
            for j in range(max(0, n - DMA_K), n):
                f.add(self.dma_ops[e][j].idx)
        self.fence = f
        self.fenced = {e: False for e in ENGS}
        self.last_w = {}
        self.readers = {}

    def op(self, eng, fn, reads=(), writes=(), dma=False):
        pr = [r for r in reads if r.startswith("ps")]
        if pr:
            reads = [r for r in reads if not r.startswith("ps")]
            writes = list(writes) + pr
        deps = set()
        if not self.fenced[eng]:
            deps |= self.fence
            self.fenced[eng] = True
        for r in reads:
            w = self.last_w.get(r)
            if w is not None:
                deps.add(w)
        for w_ in writes:
            w = self.last_w.get(w_)
            if w is not None:
                deps.add(w)
            for r in self.readers.get(w_, ()):
                deps.add(r)
        o = Op(eng, fn, deps, dma)
        o.idx = len(self.ops)
        if dma:
            j = self.dma_count[eng]
            o.dma_j = j
            self.dma_count[eng] += 1
            if j >= DMA_K:
                deps.add(self.dma_ops[eng][j - DMA_K].idx)
            self.dma_ops[eng].append(o)
        deps.discard(o.idx)
        self.ops.append(o)
        self.last_on[eng] = o.idx
        if not dma:
            self.last_comp[eng] = o.idx
        for r in reads:
            self.readers.setdefault(r, []).append(o.idx)
        for w_ in writes:
            self.last_w[w_] = o.idx
            self.readers[w_] = []
        return o

    def dma(self, eng, out, in_, reads=(), writes=(), **kw):
        return self.op(eng, lambda e: e.dma_start(out=out, in_=in_, **kw), reads, writes, dma=True)

    def finalize(self):
        nc = self.nc
        ops = self.ops
        tail = []
        for e in ENGS:
            n = self.dma_count[e]
            for j in range(max(0, n - DMA_K), n):
                tail.append(self.dma_ops[e][j].idx)
        fin = Op("sync", None, set(tail), False)
        fin.idx = len(ops)
        ops.append(fin)
        for o in ops:
            for d in o.deps:
                p = ops[d]
                if p.eng == "tensor" and o.eng == "tensor" and not p.dma and not o.dma:
                    continue
                p.needed = True
        with ExitStack() as st:
            csem = {e: st.enter_context(nc.semaphore("c_" + e)) for e in ENGS}
            dsem = {e: [st.enter_context(nc.semaphore("d_%s%d" % (e, k))) for k in range(DMA_K)]
                    for e in ENGS if self.dma_count[e] > 0}
            cnt = {e: 0 for e in ENGS}
            for o in ops:
                if o.dma:
                    k = o.dma_j % DMA_K
                    o.sig = (dsem[o.eng][k], 16, 16 * (o.dma_j // DMA_K + 1))
                elif o.needed:
                    cnt[o.eng] += 1
                    o.sig = (csem[o.eng], 1, cnt[o.eng])
            seen = {e: {} for e in ENGS}
            for o in ops:
                need = {}
                for d in o.deps:
                    p = ops[d]
                    if p.sig is None:
                        continue
                    if p.eng == "tensor" and o.eng == "tensor" and not p.dma and not o.dma:
                        continue
                    sem, _, val = p.sig
                    key = id(sem)
                    if key not in need or need[key][1] < val:
                        need[key] = (sem, val)
                sn = seen[o.eng]
                for key, (sem, val) in need.items():
                    if sn.get(key, 0) >= val:
                        continue
                    sn[key] = val
                    o.waits.append((sem, val))
            by_eng = {e: [o for o in ops if o.eng == e] for e in ENGS}

            def emit(name, e):
                for o in by_eng[name]:
                    for sem, val in o.waits:
                        e.wait_ge(sem, val)
                    if o.fn is None:
                        continue
                    ins = o.fn(e)
                    if o.sig is not None:
                        ins.then_inc(o.sig[0], o.sig[1])

            with nc.Block() as block:
                @block.sync
                def _(e):
                    emit("sync", e)

                @block.scalar
                def _(e):
                    emit("scalar", e)

                @block.gpsimd
                def _(e):
                    emit("gpsimd", e)

                @block.vector
                def _(e):
                    emit("vector", e)

                @block.tensor
                def _(e):
                    emit("tensor", e)
        return {e: len(by_eng[e]) for e in ENGS}


ARENA = 207 * 1024


class B:
    def __init__(self, nc, st):
        self.nc = nc
        self.S = Sched(nc)
        self.arena = st.enter_context(nc.sbuf_tensor("arena", [128, ARENA], U8))
        self.off = 0
        self.mark = 0
        self.psf = [st.enter_context(nc.psum_tensor("ps%d" % i, [128, 512], F32)) for i in range(8)]
        self.wslot = 0
        self.uid = 0

    def alloc(self, name, free_shape, dt):
        n = 1
        for s in free_shape:
            n *= s
        nb = n * (2 if dt == BF16 else 4)
        nb = (nb + 63) // 64 * 64
        assert self.off + nb <= ARENA, (name, self.off, nb)
        v = self.arena[:, self.off:self.off + nb].bitcast(dt)
        self.off += nb
        nn = 1
        for s in free_shape:
            nn *= s
        v = v[:, 0:nn]
        if len(free_shape) == 2:
            v = v.rearrange("p (a b) -> p a b", a=free_shape[0])
        elif len(free_shape) == 3:
            v = v.rearrange("p (a b c) -> p a b c", a=free_shape[0], b=free_shape[1])
        return v

    def phase(self):
        self.S.barrier()
        self.off = self.mark

    def ps(self, i):
        return self.psf[i][:]

    def psb(self, i):
        return self.psf[i][:].bitcast(BF16).rearrange("p (a b) -> p a b", a=8)


def build_program(stage=99):
    nc = bass.Bass("TRN2", target_bir_lowering=False)

    def din(name, shape, dt=F32):
        return nc.dram_tensor(name, list(shape), dt, kind="ExternalInput").ap()

    def dscr(name, shape, dt):
        return nc.dram_tensor(name, list(shape), dt, kind="Internal").ap()

    x_in = din("x", [S_, D_])
    mem_in = din("mem", [256, D_])
    mix_norm = din("mix_norm", [2, D_])
    w_in = din("attn_w_in", [D_, 3072])
    q_gain = din("q_gain", [128])
    k_gain = din("k_gain", [128])
    fourier_w = din("fourier_w", [4, 128, 128])
    w_out = din("attn_w_out", [D_, D_])
    pool_w = din("pool_w", [4, 512, 512])
    pool_scale = din("pool_scale", [D_])
    cross_norm = din("cross_norm", [2, D_])
    mem_norm = din("mem_norm", [2, D_])
    cw_q = din("cross_w_q", [2, D_, D_])
    cw_k = din("cross_w_k", [2, D_, D_])
    cw_v = din("cross_w_v", [2, D_, D_])
    cw_o = din("cross_w_o", [2, D_, D_])
    ffn_norm = din("ffn_norm", [2, D_])
    router_w = din("router_w", [2, D_, NE])
    ew_gate = din("expert_w_gate", [2, NE, D_, 1024])
    ew_up = din("expert_w_up", [2, NE, D_, 1024])
    ew_down = din("expert_w_down", [2, NE, 1024, D_])
    final_norm = din("final_norm", [D_])
    c_rope = din("c_rope", [2, S_, 256])
    c_dftS = din("c_dftS", [2, S_, S_])
    c_dftC = din("c_dftC", [2, 128, 128])
    c_band = din("c_band", [4, 6, 128, 512])
    c_bedge = din("c_bedge", [4, 2, 128, 128])
    y_out = nc.dram_tensor("y", [S_, D_], F32, kind="ExternalOutput").ap()

    XA = dscr("XA", [S_, D_], F32)
    XB = dscr("XB", [S_, D_], F32)
    qT_d = dscr("qT_d", [12, 128, S_], BF16)
    kT_d = dscr("kT_d", [4, 128, S_], BF16)
    v_d = dscr("v_d", [S_, 512], BF16)
    fT_d = dscr("fT_d", [4, 128, S_], BF16)
    mixT_d = dscr("mixT_d", [16, 128, S_], BF16)
    hmoe_d = dscr("hmoe_d", [S_, D_], BF16)

    with ExitStack() as st:
        b = B(nc, st)
        S = b.S
        identf = b.alloc("identf", [128], F32)
        identb = b.alloc("identb", [128], BF16)
        onesb = b.alloc("onesb", [128], BF16)
        onesf = b.alloc("onesf", [128], F32)
        eps = b.alloc("eps", [1], F32)
        gb = b.alloc("gb", [D_], F32)
        S.op("gpsimd", lambda e: e.memset(identf, 0.0), writes=["identf"])
        S.op("gpsimd", lambda e: e.affine_select(out=identf, in_=identf, pattern=[[-1, 128]],
                                                 compare_op=ALU.not_equal, fill=1.0, base=0,
                                                 channel_multiplier=1), reads=["identf"], writes=["identf"])
        S.op("vector", lambda e: e.tensor_copy(out=identb, in_=identf), reads=["identf"], writes=["identb"])
        S.op("vector", lambda e: e.memset(onesb, 1.0), writes=["onesb"])
        S.op("vector", lambda e: e.memset(onesf, 1.0), writes=["onesf"])
        S.op("vector", lambda e: e.memset(eps, 1e-6), writes=["eps"])
        b.mark = b.off
        CONST = ["identf", "identb", "onesb", "onesf", "eps"]

        def keep_consts():
            pass

        def load_gain(gain_ap):
            S.dma("sync", gb, gain_ap.partition_broadcast(128), writes=["gb"])

        def norm_tile(src_rows, ti, xts, xn, ssq, rstd, out_fp32=None):
            xt = xts[ti % 2]
            xk = "xt%d" % (ti % 2)
            S.dma("sync", xt, src_rows, writes=[xk])
            S.op("scalar", lambda e: e.activation(out=xn, in_=xt, func=AF.Square, accum_out=ssq),
                 reads=[xk], writes=["xn", "ssq"])
            S.op("scalar", lambda e: e.activation(out=rstd, in_=ssq, func=AF.Sqrt, scale=1.0 / D_, bias=eps),
                 reads=["ssq"], writes=["rstd"])
            S.op("vector", lambda e: e.reciprocal(out=rstd, in_=rstd), reads=["rstd"], writes=["rstd"])
            if out_fp32 is not None:
                S.op("vector", lambda e: e.scalar_tensor_tensor(out=out_fp32, in0=xt, scalar=rstd, in1=gb,
                                                                op0=ALU.mult, op1=ALU.mult),
                     reads=[xk, "rstd", "gb"], writes=["xnf"])
                S.op("gpsimd", lambda e: e.tensor_copy(out=xn, in_=out_fp32), reads=["xnf"], writes=["xn"])
            else:
                S.op("vector", lambda e: e.scalar_tensor_tensor(out=xn, in0=xt, scalar=rstd, in1=gb,
                                                                op0=ALU.mult, op1=ALU.mult),
                     reads=[xk, "rstd", "gb"], writes=["xn"])

        def transpose_to(hT, col0, xn, kc=16):
            for half in range(kc // 8):
                pb = b.psb(half)
                pk = "ps%d" % half
                for k in range(8):
                    kk = half * 8 + k
                    S.op("tensor", lambda e, k=k, kk=kk, pb=pb: e.transpose(out=pb[:, k, :], in_=xn[:, kk * 128:(kk + 1) * 128],
                                                                           identity=identb),
                         reads=["xn"], writes=[pk])
                eng = "vector" if half == 0 else "scalar"
                dst = hT[:, half * 8:(half + 1) * 8, col0:col0 + 128]
                if eng == "vector":
                    S.op("vector", lambda e, dst=dst, pb=pb: e.tensor_copy(out=dst, in_=pb), reads=[pk], writes=["hT"])
                else:
                    S.op("scalar", lambda e, dst=dst, pb=pb: e.copy(out=dst, in_=pb), reads=[pk], writes=["hT"])

        def norm_loop(rows_of, n_tiles, hT, hkey="hT", junk=None):
            xts = [b.alloc("xt0", [D_], F32), b.alloc("xt1", [D_], F32)]
            xn2 = [b.alloc("xn0", [D_], BF16), b.alloc("xn1", [D_], BF16)]
            if junk is None:
                junk = b.alloc("njunk", [D_], BF16)
            ssq2 = [b.alloc("ssq0", [1], F32), b.alloc("ssq1", [1], F32)]
            rstd2 = [b.alloc("rstd0", [1], F32), b.alloc("rstd1", [1], F32)]

            def load(ti):
                S.dma("sync", xts[ti % 2], rows_of(ti), writes=["xt%d" % (ti % 2)])

            def early(ti):
                i = ti % 2
                xt, xn, ssq, rstd = xts[i], xn2[i], ssq2[i], rstd2[i]
                S.op("scalar", lambda e: e.activation(out=junk, in_=xt, func=AF.Square, accum_out=ssq),
                     reads=["xt%d" % i], writes=["njunk", "ssq%d" % i])
                S.op("scalar", lambda e: e.activation(out=rstd, in_=ssq, func=AF.Sqrt, scale=1.0 / D_, bias=eps),
                     reads=["ssq%d" % i], writes=["rstd%d" % i])
                S.op("vector", lambda e: e.reciprocal(out=rstd, in_=rstd), reads=["rstd%d" % i], writes=["rstd%d" % i])
                S.op("vector", lambda e: e.scalar_tensor_tensor(out=xn, in0=xt, scalar=rstd, in1=gb, op0=ALU.mult, op1=ALU.mult),
                     reads=["xt%d" % i, "rstd%d" % i, "gb"], writes=["xn%d" % i])

            def late(ti):
                i = ti % 2
                xn = xn2[i]
                for half in range(2):
                    pb = b.psb(half)
                    pk = "ps%d" % half
                    for k in range(8):
                        kk = half * 8 + k
                        S.op("tensor", lambda e, k=k, kk=kk, pb=pb: e.transpose(out=pb[:, k, :], in_=xn[:, kk * 128:(kk + 1) * 128],
                                                                               identity=identb),
                             reads=["xn%d" % i], writes=[pk])
                    dst = hT[:, half * 8:(half + 1) * 8, ti * 128:(ti + 1) * 128]
                    if half == 0:
                        S.op("vector", lambda e, dst=dst, pb=pb: e.tensor_copy(out=dst, in_=pb), reads=[pk], writes=[hkey])
                    else:
                        S.op("scalar", lambda e, dst=dst, pb=pb: e.copy(out=dst, in_=pb), reads=[pk], writes=[hkey])

            load(0)
            for ti in range(n_tiles + 1):
                if ti + 1 < n_tiles:
                    load(ti + 1)
                if ti < n_tiles:
                    early(ti)
                if ti >= 1:
                    late(ti - 1)

        def norm_phase_alloc():
            xts = [b.alloc("xt0", [D_], F32), b.alloc("xt1", [D_], F32)]
            xn = b.alloc("xn", [D_], BF16)
            ssq = b.alloc("ssq", [1], F32)
            rstd = b.alloc("rstd", [1], F32)
            return xts, xn, ssq, rstd

        def wpiece(src_ap, kdim=16):
            slot = b.wslot % len(wring)
            b.wslot += 1
            wt = wring[slot].rearrange("p (k n) -> p k n", k=kdim)
            S.dma("gpsimd", wt, src_ap, writes=["w%d" % slot])
            return wt, "w%d" % slot

        def linear_tok(hT, T, w_src, n_cols, epilogue, kc=16, banks=(2, 3, 4, 5, 6, 7), hkey="hT"):
            cnt = 0
            nblk = n_cols // 512
            nxt = wpiece(w_src(0).rearrange("(k p) n -> p k n", p=128))
            for nb in range(nblk):
                wt, wk = nxt
                if nb + 1 < nblk:
                    nxt = wpiece(w_src(nb + 1).rearrange("(k p) n -> p k n", p=128))
                for ti in range(T // 128):
                    bank = banks[cnt % len(banks)]
                    cnt += 1
                    pk = "ps%d" % bank
                    for k in range(kc):
                        S.op("tensor", lambda e, k=k, ti=ti, bank=bank, wt=wt: e.matmul(
                            b.ps(bank), lhsT=hT[:, k, ti * 128:(ti + 1) * 128], rhs=wt[:, k, :],
                            start=(k == 0), stop=(k == kc - 1)), reads=[hkey, wk], writes=[pk])
                    epilogue(nb, ti, b.ps(bank), pk)

        def linear_feat(hT, T, w_src, n_cols, epilogue, kc=16, banks=(2, 3, 4, 5, 6, 7), hkey="hT"):
            cnt = 0
            nblk = n_cols // 512
            nxt = wpiece(w_src(0).rearrange("(k p) n -> p k n", p=128))
            for nb in range(nblk):
                wt, wk = nxt
                if nb + 1 < nblk:
                    nxt = wpiece(w_src(nb + 1).rearrange("(k p) n -> p k n", p=128))
                for nt in range(4):
                    for tb in range(T // 512):
                        bank = banks[cnt % len(banks)]
                        cnt += 1
                        pk = "ps%d" % bank
                        for k in range(kc):
                            S.op("tensor", lambda e, k=k, nt=nt, tb=tb, bank=bank, wt=wt: e.matmul(
                                b.ps(bank), lhsT=wt[:, k, nt * 128:(nt + 1) * 128],
                                rhs=hT[:, k, tb * 512:(tb + 1) * 512],
                                start=(k == 0), stop=(k == kc - 1)), reads=[hkey, wk], writes=[pk])
                        epilogue(nb * 4 + nt, tb, b.ps(bank), pk)

        TG = 1024
        NTG = S_ // TG

        def make_resid_epilogue(X_src, X_dst, t_base, order, scale_b=None):
            NRX = 3
            rx = [b.alloc("rx%d" % i, [512], F32) for i in range(NRX)]
            ctr = [0]
            issued = [0]
            sc_tmp = b.alloc("sc_tmp", [512], F32) if scale_b is not None else None

            def issue_loads(upto):
                while issued[0] < min(upto, len(order)):
                    k = issued[0]
                    nb_, ti_ = order[k]
                    rows = slice(t_base + ti_ * 128, t_base + (ti_ + 1) * 128)
                    cols = slice(nb_ * 512, (nb_ + 1) * 512)
                    S.dma("sync", rx[k % NRX], X_src[rows, cols], writes=["rx%d" % (k % NRX)])
                    issued[0] += 1

            def ep(nb, ti, ps, pk):
                k = ctr[0]
                ctr[0] += 1
                assert order[k] == (nb, ti), (order[k], nb, ti)
                issue_loads(k + 2)
                i = k % NRX
                r = rx[i]
                rk = "rx%d" % i
                rows = slice(t_base + ti * 128, t_base + (ti + 1) * 128)
                cols = slice(nb * 512, (nb + 1) * 512)
                if scale_b is not None:
                    S.op("vector", lambda e: e.tensor_tensor(out=sc_tmp, in0=ps, in1=scale_b[:, cols], op=ALU.mult),
                         reads=[pk, "scale_b"], writes=["sc_tmp"])
                    S.op("gpsimd", lambda e: e.tensor_tensor(out=r, in0=sc_tmp, in1=r, op=ALU.add),
                         reads=["sc_tmp", rk], writes=[rk])
                else:
                    S.op("vector", lambda e: e.tensor_tensor(out=r, in0=ps, in1=r, op=ALU.add),
                         reads=[pk, rk], writes=[rk])
                S.dma("sync", X_dst[rows, cols], r, reads=[rk], writes=["Xdst"])
            return ep

        def phase_attn_proj():
            for tg in range(NTG):
                b.phase()
                global wring
                wring = [b.alloc("w%d" % i, [8192], BF16) for i in range(3)]
                hT = b.alloc("hT", [16, TG], BF16)
                load_gain(mix_norm[0])
                norm_loop(lambda ti: x_in[tg * TG + ti * 128: tg * TG + (ti + 1) * 128, :], TG // 128, hT)
                NB3 = 3
                stage = [b.alloc("stg0", [4, TG], BF16), b.alloc("stg1", [4, TG], BF16)]
                junk = [b.alloc("junk%d" % i, [512], BF16) for i in range(NB3)]
                xg = [b.alloc("xg%d" % i, [512], F32) for i in range(NB3)]
                ro = [b.alloc("ro%d" % i, [512], F32) for i in range(NB3)]
                tt = [[b.alloc("tt%d_%d" % (i, j), [256], F32) for j in range(4)] for i in range(NB3)]
                qr = [b.alloc("qr%d" % i, [512], BF16) for i in range(NB3)]
                ssq4 = [b.alloc("ssq4_%d" % i, [4], F32) for i in range(NB3)]
                rs4 = [b.alloc("rs4_%d" % i, [4], F32) for i in range(NB3)]
                gq4 = b.alloc("gq4", [512], F32)
                gk4 = b.alloc("gk4", [512], F32)
                cs = [b.alloc("cs0", [2, 256], F32), b.alloc("cs1", [2, 256], F32)]
                vst = [b.alloc("vst0", [512], BF16), b.alloc("vst1", [512], BF16)]
                for h in range(4):
                    S.dma("sync", gq4[:, h * 128:(h + 1) * 128], q_gain.partition_broadcast(128), writes=["gq4"])
                    S.dma("sync", gk4[:, h * 128:(h + 1) * 128], k_gain.partition_broadcast(128), writes=["gk4"])
                cctr = [0]
                stB = {}
                stC = {}

                def load_cs(n):
                    ti_ = n % (TG // 128)
                    rows = slice(tg * TG + ti_ * 128, tg * TG + (ti_ + 1) * 128)
                    S.dma("sync", cs[n % 2][:, 0, :], c_rope[0, rows, :], writes=["cs%d" % (n % 2)])
                    S.dma("sync", cs[n % 2][:, 1, :], c_rope[1, rows, :], writes=["cs%d" % (n % 2)])

                def run_stage(d, n):
                    f = d.pop(n, None)
                    if f is not None:
                        f()

                def flush():
                    n = cctr[0]
                    run_stage(stB, n - 1)
                    run_stage(stC, n - 2)
                    run_stage(stC, n - 1)

                def qk_epilogue(nb, ti, ps, pk):
                    gg, ggk = (gq4, "gq4") if nb < 3 else (gk4, "gk4")
                    n = cctr[0]
                    cctr[0] += 1
                    eb = n % NB3
                    c2 = cs[n % 2]
                    ck = "cs%d" % (n % 2)
                    if n == 0:
                        load_cs(0)
                    if n + 1 < 4 * (TG // 128):
                        load_cs(n + 1)
                    jk, sq, rs = junk[eb], ssq4[eb], rs4[eb]
                    x_, r_, q_ = xg[eb], ro[eb], qr[eb]
                    t1, t2, t3, t4 = tt[eb]
                    K = lambda nm: "%s%d" % (nm, eb)
                    for h in range(4):
                        S.op("scalar", lambda e, h=h: e.activation(out=jk[:, h * 128:(h + 1) * 128],
                                                                   in_=ps[:, h * 128:(h + 1) * 128], func=AF.Square,
                                                                   accum_out=sq[:, h:h + 1]),
                             reads=[pk], writes=[K("junk"), K("ssq4")])
                    S.op("scalar", lambda e: e.activation(out=rs, in_=sq, func=AF.Sqrt, scale=1.0 / 128, bias=eps),
                         reads=[K("ssq4")], writes=[K("rs4")])
                    S.op("vector", lambda e: e.tensor_tensor(out=x_, in0=ps, in1=gg, op=ALU.mult), reads=[pk, ggk], writes=[K("xg")])
                    xv = x_.rearrange("p (x two) -> p x two", two=2)
                    rv = r_.rearrange("p (x two) -> p x two", two=2)
                    x1 = xv[:, :, 0]
                    x2 = xv[:, :, 1]
                    cc = c2[:, 0, :]
                    ss = c2[:, 1, :]
                    S.op("gpsimd", lambda e: e.tensor_tensor(out=t2, in0=x2, in1=ss, op=ALU.mult), reads=[K("xg"), ck], writes=[K("t2")])
                    S.op("gpsimd", lambda e: e.tensor_tensor(out=t3, in0=x1, in1=ss, op=ALU.mult), reads=[K("xg"), ck], writes=[K("t3")])
                    S.op("gpsimd", lambda e: e.tensor_tensor(out=t4, in0=x2, in1=cc, op=ALU.mult), reads=[K("xg"), ck], writes=[K("t4")])
                    S.op("vector", lambda e: e.tensor_tensor(out=t1, in0=x1, in1=cc, op=ALU.mult), reads=[K("xg"), ck], writes=[K("t1")])

                    def stage_b():
                        S.op("vector", lambda e: e.reciprocal(out=rs, in_=rs), reads=[K("rs4")], writes=[K("rs4")])
                        S.op("vector", lambda e: e.tensor_tensor(out=rv[:, :, 0], in0=t1, in1=t2, op=ALU.subtract),
                             reads=[K("t1"), K("t2")], writes=[K("ro")])
                        S.op("vector", lambda e: e.tensor_tensor(out=rv[:, :, 1], in0=t3, in1=t4, op=ALU.add),
                             reads=[K("t3"), K("t4")], writes=[K("ro")])
                        for h in range(4):
                            S.op("vector", lambda e, h=h: e.tensor_scalar(out=q_[:, h * 128:(h + 1) * 128], in0=r_[:, h * 128:(h + 1) * 128],
                                                                         scalar1=rs[:, h:h + 1], scalar2=None, op0=ALU.mult),
                                 reads=[K("ro"), K("rs4")], writes=[K("qr%d" % h)])

                    pb = b.psb(n % 2)
                    pbk = "ps%d" % (n % 2)
                    sg = stage[nb % 2]

                    def stage_c():
                        for h in range(4):
                            S.op("tensor", lambda e, h=h: e.transpose(out=pb[:, h, :], in_=q_[:, h * 128:(h + 1) * 128], identity=identb),
                                 reads=[K("qr%d" % h)], writes=[pbk])
                        S.op("scalar", lambda e: e.copy(out=sg[:, :, ti * 128:(ti + 1) * 128], in_=pb[:, 0:4, :]),
                             reads=[pbk], writes=["stg%d" % (nb % 2)])
                        if ti == TG // 128 - 1:
                            cols = slice(tg * TG, (tg + 1) * TG)
                            if nb < 3:
                                dst = qT_d[nb * 4:(nb + 1) * 4, :, cols]
                            else:
                                dst = kT_d[:, :, cols]
                            S.dma("sync", dst.rearrange("h p t -> p h t"), sg, reads=["stg%d" % (nb % 2)], writes=["qkT_d"])

                    stB[n] = stage_b
                    stC[n] = stage_c
                    run_stage(stB, n - 1)
                    run_stage(stC, n - 2)

                def w_src(nb):
                    return w_in[:, nb * 512:(nb + 1) * 512]

                linear_tok(hT, TG, w_src, 4 * 512, qk_epilogue)
                flush()

                def v_epilogue(nb, ti, ps, pk):
                    vb = vst[ti % 2]
                    S.op("scalar", lambda e: e.copy(out=vb, in_=ps), reads=[pk], writes=["vst%d" % (ti % 2)])
                    S.dma("sync", v_d[tg * TG + ti * 128: tg * TG + (ti + 1) * 128, :], vb, reads=["vst%d" % (ti % 2)], writes=["v_d"])

                linear_tok(hT, TG, lambda nb: w_in[:, 2048:2560], 512, v_epilogue)

                fst = [b.alloc("fst0", [512], BF16), b.alloc("fst1", [512], BF16)]
                fctr = [0]

                def f_epilogue(nt, tb, ps, pk):
                    i = fctr[0] % 2
                    fctr[0] += 1
                    S.op("vector", lambda e: e.tensor_copy(out=fst[i], in_=ps), reads=[pk], writes=["fst%d" % i])
                    S.dma("sync", fT_d[nt, :, tg * TG + tb * 512: tg * TG + (tb + 1) * 512], fst[i],
                          reads=["fst%d" % i], writes=["fT_d"])

                linear_feat(hT, TG, lambda nb: w_in[:, 2560:3072], 512, f_epilogue)

        def phase_attention():
            b.phase()
            kT = b.alloc("kT", [4, S_], BF16)
            V = b.alloc("V", [16, 512], BF16)
            qT = [b.alloc("qT0", [S_], BF16), b.alloc("qT1", [S_], BF16)]
            eT = [b.alloc("eT%d" % i, [512], BF16) for i in range(4)]
            rsum = [b.alloc("rsum0", [512], F32), b.alloc("rsum1", [512], F32)]
            ost = [b.alloc("ost0", [512], BF16), b.alloc("ost1", [512], BF16)]
            S.dma("sync", kT, kT_d.rearrange("h p t -> p h t"), reads=["qkT_d"], writes=["kT"])
            S.dma("sync", V, v_d.rearrange("(j p) d -> p j d", p=128), reads=["v_d"], writes=["V"])
            scale = 128 ** -0.5
            items = [(h, qb, st_) for h in range(12) for qb in range(4) for st_ in range(16)]

            def load_q(h):
                S.dma("sync", qT[h % 2], qT_d[h], reads=["qkT_d"], writes=["qT%d" % (h % 2)])

            def emit_sc(i):
                h, qb, st_ = items[i]
                g = h // 3
                q = qT[h % 2]
                scb = 2 + (i % 2)
                S.op("tensor", lambda e: e.matmul(
                    b.ps(scb), lhsT=kT[:, g, st_ * 128:(st_ + 1) * 128], rhs=q[:, qb * 512:(qb + 1) * 512],
                    start=True, stop=True), reads=["kT", "qT%d" % (h % 2)], writes=["ps%d" % scb])

            load_q(0)
            emit_sc(0)
            for i, (h, qb, st_) in enumerate(items):
                g = h // 3
                blk = i // 16
                ob = 4 + (blk % 2)
                sb_ = 6 + (blk % 2)
                scb = 2 + (i % 2)
                ei = i % 4
                if qb == 0 and st_ == 0 and h + 1 < 12:
                    load_q(h + 1)
                if i + 1 < len(items):
                    emit_sc(i + 1)
                S.op("scalar", lambda e, scb=scb, ei=ei: e.activation(out=eT[ei], in_=b.ps(scb), func=AF.Exp, scale=scale),
                     reads=["ps%d" % scb], writes=["eT%d" % ei])
                S.op("tensor", lambda e, st_=st_, ob=ob, ei=ei, g=g: e.matmul(
                    b.ps(ob), lhsT=V[:, st_, g * 128:(g + 1) * 128], rhs=eT[ei], start=(st_ == 0), stop=(st_ == 15)),
                     reads=["V", "eT%d" % ei], writes=["ps%d" % ob])
                S.op("tensor", lambda e, st_=st_, sb_=sb_, ei=ei: e.matmul(
                    b.ps(sb_), lhsT=onesb, rhs=eT[ei], start=(st_ == 0), stop=(st_ == 15)),
                     reads=["eT%d" % ei], writes=["ps%d" % sb_])
                if st_ == 15:
                    rs = rsum[blk % 2]
                    rk = "rsum%d" % (blk % 2)
                    o = ost[blk % 2]
                    okk = "ost%d" % (blk % 2)
                    S.op("vector", lambda e, sb_=sb_, rs=rs: e.reciprocal(out=rs, in_=b.ps(sb_)), reads=["ps%d" % sb_], writes=[rk])
                    S.op("vector", lambda e, ob=ob, o=o, rs=rs: e.tensor_tensor(out=o, in0=b.ps(ob), in1=rs, op=ALU.mult),
                         reads=["ps%d" % ob, rk], writes=[okk])
                    S.dma("sync", mixT_d[h, :, qb * 512:(qb + 1) * 512], o, reads=[okk], writes=["mixT_d"])

        def phase_fourier():
            b.phase()
            global wring
            wring = [b.alloc("w%d" % i, [8192], BF16) for i in range(4)]
            dC = b.alloc("dC", [2, 128], F32)
            fw = b.alloc("fw", [4, 128], F32)
            AB = b.alloc("AB", [4, 256], BF16)
            fT = b.alloc("fT", [4, S_], BF16)
            P12 = b.alloc("P12", [16, 4 * 256], BF16)
            ost = [b.alloc("ost0", [512], BF16), b.alloc("ost1", [512], BF16)]
            S.dma("sync", dC, c_dftC.rearrange("a p c -> p a c"), writes=["dC"])
            S.dma("sync", fw, fourier_w.rearrange("g p c -> p g c"), writes=["fw"])
            S.dma("sync", fT, fT_d.rearrange("g p t -> p g t"), reads=["fT_d"], writes=["fT"])
            for g in range(4):
                for a in range(2):
                    S.op("tensor", lambda e, g=g, a=a: e.matmul(b.ps(2)[:, a * 128:(a + 1) * 128], lhsT=dC[:, a, :], rhs=fw[:, g, :],
                                                                start=True, stop=True), reads=["dC", "fw"], writes=["ps2"])
                S.op("vector", lambda e, g=g: e.tensor_copy(out=AB[:, g, :], in_=b.ps(2)[:, 0:256]), reads=["ps2"], writes=["AB"])
            for j in range(16):
                for half in range(2):
                    bank = 3 + half
                    for gg in range(2):
                        g = half * 2 + gg
                        S.op("tensor", lambda e, g=g, gg=gg, j=j, bank=bank: e.matmul(
                            b.ps(bank)[:, gg * 256:(gg + 1) * 256], lhsT=fT[:, g, j * 128:(j + 1) * 128], rhs=AB[:, g, :],
                            start=True, stop=True), reads=["fT", "AB"], writes=["ps%d" % bank])
                    eng = "vector" if half == 0 else "scalar"
                    dst = P12[:, j, half * 512:(half + 1) * 512]
                    if eng == "vector":
                        S.op("vector", lambda e, dst=dst, bank=bank: e.tensor_copy(out=dst, in_=b.ps(bank)), reads=["ps%d" % bank], writes=["P12"])
                    else:
                        S.op("scalar", lambda e, dst=dst, bank=bank: e.copy(out=dst, in_=b.ps(bank)), reads=["ps%d" % bank], writes=["P12"])
            oc = 0
            for tb in range(4):
                cw, ck = wpiece(c_dftS[0][:, tb * 512:(tb + 1) * 512].rearrange("(k p) n -> p k n", p=128))
                sw, sk = wpiece(c_dftS[1][:, tb * 512:(tb + 1) * 512].rearrange("(k p) n -> p k n", p=128))
                for g in range(4):
                    bank = 5 + (oc % 2)
                    for j in range(16):
                        S.op("tensor", lambda e, g=g, j=j, bank=bank, cw=cw: e.matmul(
                            b.ps(bank), lhsT=P12[:, j, g * 256:g * 256 + 128], rhs=cw[:, j, :], start=(j == 0), stop=False),
                             reads=["P12", ck], writes=["ps%d" % bank])
                    for j in range(16):
                        S.op("tensor", lambda e, g=g, j=j, bank=bank, sw=sw: e.matmul(
                            b.ps(bank), lhsT=P12[:, j, g * 256 + 128:g * 256 + 256], rhs=sw[:, j, :], start=False, stop=(j == 15)),
                             reads=["P12", sk], writes=["ps%d" % bank])
                    o = ost[oc % 2]
                    okk = "ost%d" % (oc % 2)
                    oc += 1
                    S.op("vector", lambda e, o=o, bank=bank: e.tensor_copy(out=o, in_=b.ps(bank)), reads=["ps%d" % bank], writes=[okk])
                    S.dma("sync", mixT_d[12 + g, :, tb * 512:(tb + 1) * 512], o, reads=[okk], writes=["mixT_d"])

        def phase_outproj(W, X_src, X_dst):
            for tg in range(NTG):
                b.phase()
                global wring
                wring = [b.alloc("w%d" % i, [8192], BF16) for i in range(4)]
                hT = b.alloc("hT", [16, TG], BF16)
                S.dma("sync", hT, mixT_d[:, :, tg * TG:(tg + 1) * TG].rearrange("k p t -> p k t"), reads=["mixT_d"], writes=["hT"])
                ep = make_resid_epilogue(X_src, X_dst, tg * TG, [(nb, ti) for nb in range(4) for ti in range(TG // 128)])
                linear_tok(hT, TG, lambda nb: W[:, nb * 512:(nb + 1) * 512], D_, ep)

        def phase_cross(layer, X_src, X_dst):
            b.phase()
            global wring
            Aq = b.alloc("Aq", [16, 4 * 256], BF16)
            VW = b.alloc("VW", [2, 4 * D_], BF16)
            mark_main = b.off
            wring = [b.alloc("w%d" % i, [8192], BF16) for i in range(3)]
            kTm = b.alloc("kTm", [16, 256], BF16)
            vTm = b.alloc("vTm", [16, 256], BF16)
            memT = b.alloc("hT", [16, 256], BF16)
            WqT = [b.alloc("WqT0", [4, D_], BF16), b.alloc("WqT1", [4, D_], BF16)]
            load_gain(mem_norm[layer])
            norm_loop(lambda ti: mem_in[ti * 128:(ti + 1) * 128, :], 2, memT)
            cnt = 0
            for Wm, dstT, dkey in ((cw_k, kTm, "kTm"), (cw_v, vTm, "vTm")):
                nxt = wpiece(Wm[layer][:, 0:512].rearrange("(k p) n -> p k n", p=128))
                for nb in range(4):
                    wt, wk = nxt
                    if nb + 1 < 4:
                        nxt = wpiece(Wm[layer][:, (nb + 1) * 512:(nb + 2) * 512].rearrange("(k p) n -> p k n", p=128))
                    for nt in range(4):
                        bank = 2 + (cnt % 4)
                        cnt += 1
                        for k in range(16):
                            S.op("tensor", lambda e, k=k, nt=nt, bank=bank, wt=wt: e.matmul(
                                b.ps(bank)[:, 0:256], lhsT=wt[:, k, nt * 128:(nt + 1) * 128], rhs=memT[:, k, :],
                                start=(k == 0), stop=(k == 15)), reads=["hT", wk], writes=["ps%d" % bank])
                        dst = dstT[:, nb * 4 + nt, :]
                        if cnt % 2 == 0:
                            S.op("vector", lambda e, dst=dst, bank=bank: e.tensor_copy(out=dst, in_=b.ps(bank)[:, 0:256]), reads=["ps%d" % bank], writes=[dkey])
                        else:
                            S.op("scalar", lambda e, dst=dst, bank=bank: e.copy(out=dst, in_=b.ps(bank)[:, 0:256]), reads=["ps%d" % bank], writes=[dkey])
            nxt = wpiece(cw_q[layer][:, 0:512].rearrange("(k p) n -> p k n", p=128))
            for h in range(4):
                wt, wk = nxt
                if h + 1 < 4:
                    nxt = wpiece(cw_q[layer][:, (h + 1) * 512:(h + 2) * 512].rearrange("(k p) n -> p k n", p=128))
                WT = WqT[h % 2]
                wtk = "WqT%d" % (h % 2)
                tcn = 0
                for c in range(4):
                    for k0 in (0, 8):
                        half = tcn % 2
                        tcn += 1
                        pb = b.psb(half)
                        for k in range(8):
                            S.op("tensor", lambda e, k=k, k0=k0, c=c, pb=pb, wt=wt: e.transpose(
                                out=pb[:, k, :], in_=wt[:, k0 + k, c * 128:(c + 1) * 128], identity=identb),
                                 reads=[wk], writes=["ps%d" % half])
                        dst = WT[:, c, k0 * 128:(k0 + 8) * 128].rearrange("p (a b) -> p a b", a=8)
                        if half == 0:
                            S.op("vector", lambda e, dst=dst, pb=pb: e.tensor_copy(out=dst, in_=pb), reads=["ps0"], writes=[wtk])
                        else:
                            S.op("scalar", lambda e, dst=dst, pb=pb: e.copy(out=dst, in_=pb), reads=["ps1"], writes=[wtk])
                for k in range(16):
                    bank = 2 + (cnt % 4)
                    cnt += 1
                    for c in range(4):
                        S.op("tensor", lambda e, k=k, c=c, h=h, bank=bank, WT=WT: e.matmul(
                            b.ps(bank)[:, 0:256], lhsT=WT[:, c, k * 128:(k + 1) * 128], rhs=kTm[:, h * 4 + c, :],
                            start=(c == 0), stop=(c == 3)), reads=[wtk, "kTm"], writes=["ps%d" % bank])
                    dst = Aq[:, k, h * 256:(h + 1) * 256]
                    if k % 2 == 0:
                        S.op("vector", lambda e, dst=dst, bank=bank: e.tensor_copy(out=dst, in_=b.ps(bank)[:, 0:256]), reads=["ps%d" % bank], writes=["Aq"])
                    else:
                        S.op("scalar", lambda e, dst=dst, bank=bank: e.copy(out=dst, in_=b.ps(bank)[:, 0:256]), reads=["ps%d" % bank], writes=["Aq"])
            nxt = wpiece(cw_o[layer][:, 0:512].rearrange("(k p) n -> p k n", p=128))
            for nb in range(4):
                wt, wk = nxt
                if nb + 1 < 4:
                    nxt = wpiece(cw_o[layer][:, (nb + 1) * 512:(nb + 2) * 512].rearrange("(k p) n -> p k n", p=128))
                for h in range(4):
                    for mt in range(2):
                        bank = 2 + (cnt % 4)
                        cnt += 1
                        for c in range(4):
                            S.op("tensor", lambda e, c=c, h=h, mt=mt, bank=bank, wt=wt: e.matmul(
                                b.ps(bank), lhsT=vTm[:, h * 4 + c, mt * 128:(mt + 1) * 128], rhs=wt[:, h * 4 + c, :],
                                start=(c == 0), stop=(c == 3)), reads=["vTm", wk], writes=["ps%d" % bank])
                        dst = VW[:, mt, h * D_ + nb * 512: h * D_ + (nb + 1) * 512]
                        if cnt % 2 == 0:
                            S.op("vector", lambda e, dst=dst, bank=bank: e.tensor_copy(out=dst, in_=b.ps(bank)), reads=["ps%d" % bank], writes=["VW"])
                        else:
                            S.op("scalar", lambda e, dst=dst, bank=bank: e.copy(out=dst, in_=b.ps(bank)), reads=["ps%d" % bank], writes=["VW"])
            scale = 512 ** -0.5
            for tg in range(NTG):
                S.barrier()
                b.off = mark_main
                hT = b.alloc("hTq", [16, TG], BF16)
                eTs = [b.alloc("eTs0", [8, 512], BF16), b.alloc("eTs1", [8, 512], BF16)]
                rsum2 = [b.alloc("rsum_a", [512], F32), b.alloc("rsum_b", [512], F32)]
                load_gain(cross_norm[layer])
                norm_loop(lambda ti: X_src[tg * TG + ti * 128: tg * TG + (ti + 1) * 128, :], TG // 128, hT)
                NTB = TG // 512
                order = [(nb, tb * 4 + t4) for tb in range(NTB) for t4 in range(4) for nb in range(4)]
                ep = make_resid_epilogue(X_src, X_dst, tg * TG, order)
                scn = [0]
                rcn = [0]

                def sums_norm(tb, h):
                    et = eTs[tb % 2]
                    ek = "eTs%d_%d" % (tb % 2, h)
                    rs = rsum2[rcn[0] % 2]
                    rk = "rsum%d" % (rcn[0] % 2)
                    rcn[0] += 1
                    for mt in range(2):
                        S.op("tensor", lambda e, mt=mt: e.matmul(b.ps(3), lhsT=onesb, rhs=et[:, h * 2 + mt, :], start=(mt == 0), stop=(mt == 1)),
                             reads=[ek], writes=["ps3"])
                    S.op("vector", lambda e: e.reciprocal(out=rs, in_=b.ps(3)), reads=["ps3"], writes=[rk])
                    for mt in range(2):
                        S.op("vector", lambda e, mt=mt: e.tensor_tensor(out=et[:, h * 2 + mt, :], in0=et[:, h * 2 + mt, :], in1=rs, op=ALU.mult),
                             reads=[ek, rk], writes=[ek])

                def stage_scores(tb):
                    et = eTs[tb % 2]
                    for h in range(4):
                        ek = "eTs%d_%d" % (tb % 2, h)
                        for mt in range(2):
                            bank = scn[0] % 3
                            scn[0] += 1
                            for k in range(16):
                                S.op("tensor", lambda e, k=k, h=h, mt=mt, bank=bank: e.matmul(
                                    b.ps(bank), lhsT=Aq[:, k, h * 256 + mt * 128: h * 256 + (mt + 1) * 128],
                                    rhs=hT[:, k, tb * 512:(tb + 1) * 512], start=(k == 0), stop=(k == 15)),
                                     reads=["Aq", "hT"], writes=["ps%d" % bank])
                            S.op("scalar", lambda e, h=h, mt=mt, bank=bank: e.activation(out=et[:, h * 2 + mt, :], in_=b.ps(bank), func=AF.Exp, scale=scale),
                                 reads=["ps%d" % bank], writes=[ek])
                        if h >= 1:
                            sums_norm(tb, h - 1)
                    sums_norm(tb, 3)

                ocn = [0]

                def stage_out(tb):
                    et = eTs[tb % 2]
                    for t4 in range(4):
                        for nb in range(4):
                            bank = 4 + (ocn[0] % 4)
                            ocn[0] += 1
                            i = 0
                            for h in range(4):
                                for mt in range(2):
                                    S.op("tensor", lambda e, h=h, mt=mt, t4=t4, nb=nb, bank=bank, i=i: e.matmul(
                                        b.ps(bank), lhsT=et[:, h * 2 + mt, t4 * 128:(t4 + 1) * 128],
                                        rhs=VW[:, mt, h * D_ + nb * 512: h * D_ + (nb + 1) * 512], start=(i == 0), stop=(i == 7)),
                                         reads=["eTs%d_%d" % (tb % 2, h), "VW"], writes=["ps%d" % bank])
                                    i += 1
                            ep(nb, tb * 4 + t4, b.ps(bank), "ps%d" % bank)

                stage_scores(0)
                for tb in range(NTB):
                    if tb + 1 < NTB:
                        stage_scores(tb + 1)
                    stage_out(tb)

        def phase_moe(layer, X):
            b.phase()
            global wring
            wring = [b.alloc("w%d" % i, [8192], BF16) for i in range(6)]
            for i_ in range(6):
                src_, kd_ = ((ew_gate if i_ in (0, 2) else ew_up)[layer, 0][:, (i_ // 2) * 512:(i_ // 2 + 1) * 512].rearrange("(k p) n -> p k n", p=128), 16) \
                    if i_ < 4 else (ew_down[layer, 0][:, (i_ - 4) * 1024:(i_ - 3) * 1024].rearrange("(k p) n -> p k n", p=128), 8)
                S.dma("gpsimd", wring[i_].rearrange("p (k n) -> p k n", k=kd_), src_, writes=["w%d" % i_])
            logT = b.alloc("logT", [S_], F32)
            mark_inner = b.off
            xts = [b.alloc("xt0", [D_], F32), b.alloc("xt1", [D_], F32)]
            xnb = [b.alloc("xn0", [D_], BF16), b.alloc("xn1", [D_], BF16)]
            xnf2 = [b.alloc("xnf0", [D_], F32), b.alloc("xnf1", [D_], F32)]
            junk = b.alloc("njunk", [D_], BF16)
            ssq2 = [b.alloc("ssq0", [1], F32), b.alloc("ssq1", [1], F32)]
            rstd2 = [b.alloc("rstd0", [1], F32), b.alloc("rstd1", [1], F32)]
            hTf = b.alloc("hTf", [16, 128], F32)
            wrA = b.alloc("wrA", [16, 48], F32)
            wrB = b.alloc("wrB", [16, 48], F32)
            load_gain(ffn_norm[layer])
            S.op("vector", lambda e: e.memset(wrA, 0.0), writes=["wr"])
            S.op("vector", lambda e: e.memset(wrB, 0.0), writes=["wr"])
            S.dma("sync", wrA[:, :, 0:16], router_w[layer].rearrange("(k p) e -> p k e", p=128), writes=["wr"])
            S.dma("sync", wrB[:, :, 32:48], router_w[layer].rearrange("(k p) e -> p k e", p=128), writes=["wr"])
            S.op("vector", lambda e: e.memset(logT[0:48, 0:1024], -30000.0), writes=["logT"])

            def m_load(ti):
                S.dma("sync", xts[ti % 2], X[ti * 128:(ti + 1) * 128, :], writes=["xt%d" % (ti % 2)])

            def m_early(ti):
                i = ti % 2
                rows = slice(ti * 128, (ti + 1) * 128)
                xt, xn, xnf, ssq, rstd = xts[i], xnb[i], xnf2[i], ssq2[i], rstd2[i]
                S.op("scalar", lambda e: e.activation(out=junk, in_=xt, func=AF.Square, accum_out=ssq),
                     reads=["xt%d" % i], writes=["njunk", "ssq%d" % i])
                S.op("scalar", lambda e: e.activation(out=rstd, in_=ssq, func=AF.Sqrt, scale=1.0 / D_, bias=eps),
                     reads=["ssq%d" % i], writes=["rstd%d" % i])
                S.op("vector", lambda e: e.reciprocal(out=rstd, in_=rstd), reads=["rstd%d" % i], writes=["rstd%d" % i])
                S.op("vector", lambda e: e.scalar_tensor_tensor(out=xnf, in0=xt, scalar=rstd, in1=gb, op0=ALU.mult, op1=ALU.mult),
                     reads=["xt%d" % i, "rstd%d" % i, "gb"], writes=["xnf%d" % i])
                S.op("gpsimd", lambda e: e.tensor_copy(out=xn, in_=xnf), reads=["xnf%d" % i], writes=["xn%d" % i])
                S.dma("sync", hmoe_d[rows, :], xn, reads=["xn%d" % i], writes=["hmoe_d"])

            def m_late(ti):
                i = ti % 2
                xnf = xnf2[i]
                for q4 in range(4):
                    bank = 2 + q4
                    for k in range(4):
                        kk = q4 * 4 + k
                        S.op("tensor", lambda e, k=k, kk=kk, bank=bank: e.transpose(
                            out=b.ps(bank)[:, k * 128:(k + 1) * 128], in_=xnf[:, kk * 128:(kk + 1) * 128], identity=identf),
                             reads=["xnf%d" % i], writes=["ps%d" % bank])
                    dst = hTf[:, q4 * 4:(q4 + 1) * 4, :]
                    if q4 % 2 == 0:
                        S.op("vector", lambda e, dst=dst, bank=bank: e.tensor_copy(
                            out=dst, in_=b.ps(bank).rearrange("p (a b) -> p a b", a=4)), reads=["ps%d" % bank], writes=["hTf%d" % q4])
                    else:
                        S.op("scalar", lambda e, dst=dst, bank=bank: e.copy(
                            out=dst, in_=b.ps(bank).rearrange("p (a b) -> p a b", a=4)), reads=["ps%d" % bank], writes=["hTf%d" % q4])
                wsel = wrA if ti < 8 else wrB
                for k in range(16):
                    S.op("tensor", lambda e, k=k, wsel=wsel: e.matmul(b.ps(6)[0:48, 0:128], lhsT=wsel[:, k, :], rhs=hTf[:, k, :],
                                                                     start=(k == 0), stop=(k == 15)), reads=["wr", "hTf%d" % (k // 4)], writes=["ps6"])
                p0 = 0 if ti < 8 else 32
                tcol = (ti % 8) * 128
                S.op("vector", lambda e, p0=p0, tcol=tcol: e.tensor_copy(out=logT[p0:p0 + 16, tcol:tcol + 128], in_=b.ps(6)[p0:p0 + 16, 0:128]),
                     reads=["ps6"], writes=["logT"])

            m_load(0)
            for ti in range(S_ // 128 + 1):
                if ti + 1 < S_ // 128:
                    m_load(ti + 1)
                if ti < S_ // 128:
                    m_early(ti)
                if ti >= 1:
                    m_late(ti - 1)
            S.barrier()
            b.off = mark_inner
            HS = S_ // 2
            aff = b.alloc("aff", [HS], F32)
            work = b.alloc("work", [HS], F32)
            vals = b.alloc("vals", [CAP], F32)
            idxu = b.alloc("idxu", [CAP], U32)
            idxf = b.alloc("idxf", [CAP], F32)
            ob = b.alloc("ob", [48], F32)
            offs = b.alloc("offs", [1], F32)
            Jm = b.alloc("Jm", [128], F32)
            VT = b.alloc("VT", [2, 48], F32)
            IT = b.alloc("IT", [2, 48], F32)
            BrV = b.alloc("BrV", [2, NE], F32)
            BrI = b.alloc("BrI", [2, NE], F32)
            msk = b.alloc("msk", [2, NE], F32)
            dif = b.alloc("dif", [2, NE], F32)
            gateT = b.alloc("gateT", [2, NE], F32)
            idxT = b.alloc("idxT", [2, NE], I32)
            S.op("gpsimd", lambda e: e.memset(ob[0:48, :], 0.0), writes=["ob"])
            S.op("gpsimd", lambda e: e.memset(ob[0:32, 0:32], 1.0), writes=["ob"])
            S.op("gpsimd", lambda e: e.memset(ob[32:48, 32:48], 1.0), writes=["ob"])
            S.op("gpsimd", lambda e: e.memset(offs[0:48, :], 0.0), writes=["offs"])
            S.op("gpsimd", lambda e: e.memset(offs[32:48, :], float(HS)), writes=["offs"])
            S.op("gpsimd", lambda e: e.memset(Jm, 0.0), writes=["Jm"])
            S.op("gpsimd", lambda e: e.affine_select(out=Jm, in_=Jm, pattern=[[1, 128]], compare_op=ALU.not_equal, fill=1.0,
                                                     base=-127, channel_multiplier=1), reads=["Jm"], writes=["Jm"])
            S.op("scalar", lambda e: e.activation(out=aff[0:48, :], in_=logT[0:48, 0:HS], func=AF.Exp), reads=["logT"], writes=["aff"])
            for tb in range(2):
                S.op("tensor", lambda e, tb=tb: e.matmul(b.ps(2)[0:48, :], lhsT=ob[0:48, 0:48], rhs=aff[0:48, tb * 512:(tb + 1) * 512],
                                                         start=True, stop=True), reads=["aff", "ob"], writes=["ps2"])
                S.op("vector", lambda e, tb=tb: e.reciprocal(out=work[0:48, tb * 512:(tb + 1) * 512], in_=b.ps(2)[0:48, :]),
                     reads=["ps2"], writes=["work"])
            S.op("vector", lambda e: e.tensor_tensor(out=aff[0:48, :], in0=aff[0:48, :], in1=work[0:48, :], op=ALU.mult),
                 reads=["aff", "work"], writes=["aff"])
            S.op("vector", lambda e: e.tensor_copy(out=work[0:48, :], in_=aff[0:48, :]), reads=["aff"], writes=["work"])
            for r in range(CAP // 8):
                S.op("vector", lambda e, r=r: e.max(out=vals[0:48, r * 8:(r + 1) * 8], in_=work[0:48, :]), reads=["work"], writes=["vals"])
                S.op("vector", lambda e, r=r: e.max_index(out=idxu[0:48, r * 8:(r + 1) * 8], in_max=vals[0:48, r * 8:(r + 1) * 8],
                                                          in_values=work[0:48, :]), reads=["work", "vals"], writes=["idxu"])
                S.op("vector", lambda e, r=r: e.match_replace(out=work[0:48, :], in_to_replace=vals[0:48, r * 8:(r + 1) * 8],
                                                              in_values=work[0:48, :], imm_value=-1.0),
                     reads=["work", "vals", "idxu"], writes=["work"])
            S.op("vector", lambda e: e.tensor_copy(out=idxf[0:48, :], in_=idxu[0:48, :]), reads=["idxu"], writes=["idxf"])
            S.op("vector", lambda e: e.tensor_scalar(out=idxf[0:48, :], in0=idxf[0:48, :], scalar1=0.0, scalar2=float(HS - 1),
                                                     op0=ALU.max, op1=ALU.min), reads=["idxf"], writes=["idxf"])
            S.op("vector", lambda e: e.tensor_scalar(out=idxf[0:48, :], in0=idxf[0:48, :], scalar1=offs[0:48, :], scalar2=None,
                                                     op0=ALU.add), reads=["idxf", "offs"], writes=["idxf"])
            for cc in range(2):
                S.op("tensor", lambda e, cc=cc: e.transpose(out=b.ps(3)[:, 0:48], in_=vals[0:48, cc * 128:(cc + 1) * 128],
                                                            identity=identf[0:48, 0:48]), reads=["vals"], writes=["ps3"])
                S.op("vector", lambda e, cc=cc: e.tensor_copy(out=VT[:, cc, :], in_=b.ps(3)[:, 0:48]), reads=["ps3"], writes=["VT"])
                S.op("tensor", lambda e, cc=cc: e.transpose(out=b.ps(4)[:, 0:48], in_=idxf[0:48, cc * 128:(cc + 1) * 128],
                                                            identity=identf[0:48, 0:48]), reads=["idxf"], writes=["ps4"])
                S.op("vector", lambda e, cc=cc: e.tensor_copy(out=IT[:, cc, :], in_=b.ps(4)[:, 0:48]), reads=["ps4"], writes=["IT"])
            for cc in range(2):
                S.op("tensor", lambda e, cc=cc: e.matmul(b.ps(3)[:, 0:16], lhsT=Jm, rhs=VT[:, 1 - cc, 32:48], start=True, stop=True),
                     reads=["Jm", "VT"], writes=["ps3"])
                S.op("vector", lambda e, cc=cc: e.tensor_copy(out=BrV[:, cc, :], in_=b.ps(3)[:, 0:16]), reads=["ps3"], writes=["BrV"])
                S.op("tensor", lambda e, cc=cc: e.matmul(b.ps(4)[:, 0:16], lhsT=Jm, rhs=IT[:, 1 - cc, 32:48], start=True, stop=True),
                     reads=["Jm", "IT"], writes=["ps4"])
                S.op("vector", lambda e, cc=cc: e.tensor_copy(out=BrI[:, cc, :], in_=b.ps(4)[:, 0:16]), reads=["ps4"], writes=["BrI"])
            S.op("vector", lambda e: e.tensor_tensor(out=gateT, in0=VT[:, :, 0:16], in1=BrV, op=ALU.max), reads=["VT", "BrV"], writes=["gateT"])
            S.op("vector", lambda e: e.tensor_tensor(out=msk, in0=VT[:, :, 0:16], in1=BrV, op=ALU.is_gt), reads=["VT", "BrV"], writes=["msk"])
            S.op("vector", lambda e: e.tensor_tensor(out=dif, in0=IT[:, :, 0:16], in1=BrI, op=ALU.subtract), reads=["IT", "BrI"], writes=["dif"])
            S.op("vector", lambda e: e.tensor_tensor(out=dif, in0=dif, in1=msk, op=ALU.mult), reads=["dif", "msk"], writes=["dif"])
            S.op("vector", lambda e: e.tensor_tensor(out=dif, in0=dif, in1=BrI, op=ALU.add), reads=["dif", "BrI"], writes=["dif"])
            S.op("vector", lambda e: e.tensor_copy(out=idxT, in_=dif), reads=["dif"], writes=["idxT"])
            if stage == 30:
                return
            xs = [b.alloc("xs%d" % i, [D_], BF16) for i in range(4)]
            xsT = [b.alloc("xsT0", [16, CAP], BF16), b.alloc("xsT1", [16, CAP], BF16)]
            gT = b.alloc("gT", [8, CAP], BF16)
            sa = [b.alloc("sa0", [CAP], F32), b.alloc("sa1", [CAP], F32)]
            ysb = [b.alloc("ysb0", [D_], F32), b.alloc("ysb1", [D_], F32)]

            def piece_src(ex, i):
                if i in (0, 2):
                    fh = i // 2
                    return ew_gate[layer, ex][:, fh * 512:(fh + 1) * 512].rearrange("(k p) n -> p k n", p=128), 16
                if i in (1, 3):
                    fh = i // 2
                    return ew_up[layer, ex][:, fh * 512:(fh + 1) * 512].rearrange("(k p) n -> p k n", p=128), 16
                nh = i - 4
                return ew_down[layer, ex][:, nh * 1024:(nh + 1) * 1024].rearrange("(k p) n -> p k n", p=128), 8

            def load_piece(ex, i):
                src, kd = piece_src(ex, i)
                S.dma("gpsimd", wring[i].rearrange("p (k n) -> p k n", k=kd), src, writes=["w%d" % i])

            def piece(i, kd):
                return wring[i].rearrange("p (k n) -> p k n", k=kd), "w%d" % i

            def gather(ex):
                for cc in range(2):
                    xi = (2 * ex + cc) % 4
                    S.op("gpsimd", lambda e, xi=xi, cc=cc, ex=ex: e.indirect_dma_start(
                        out=xs[xi], out_offset=None, in_=hmoe_d[:, :],
                        in_offset=bass.IndirectOffsetOnAxis(ap=idxT[:, cc, ex:ex + 1], axis=0)),
                         reads=["hmoe_d", "idxT"], writes=["xs%d" % xi], dma=True)

            def transposes(ex):
                xT = xsT[ex % 2]
                xTk = "xsT%d" % (ex % 2)
                for cc in range(2):
                    xi = (2 * ex + cc) % 4
                    xsb = xs[xi]
                    xk = "xs%d" % xi
                    for half in range(2):
                        pb = b.psb(half)
                        for k in range(8):
                            kk = half * 8 + k
                            S.op("tensor", lambda e, k=k, kk=kk, pb=pb, xsb=xsb: e.transpose(
                                out=pb[:, k, :], in_=xsb[:, kk * 128:(kk + 1) * 128], identity=identb),
                                 reads=[xk], writes=["ps%d" % half])
                        dst = xT[:, half * 8:(half + 1) * 8, cc * 128:(cc + 1) * 128]
                        if half == 0:
                            S.op("vector", lambda e, dst=dst, pb=pb: e.tensor_copy(out=dst, in_=pb), reads=["ps0"], writes=[xTk])
                        else:
                            S.op("scalar", lambda e, dst=dst, pb=pb: e.copy(out=dst, in_=pb), reads=["ps1"], writes=[xTk])

            hcn = [0]

            def hidden(ex, fh):
                xT = xsT[ex % 2]
                xTk = "xsT%d" % (ex % 2)
                wg, wgk = piece(2 * fh, 16)
                wu, wuk = piece(2 * fh + 1, 16)
                for ft in range(4):
                    hc = hcn[0]
                    hcn[0] += 1
                    bg = 2 + 2 * (hc % 2)
                    bu = bg + 1
                    si = hc % 2
                    for k in range(16):
                        S.op("tensor", lambda e, k=k, ft=ft, bg=bg, wg=wg, xT=xT: e.matmul(
                            b.ps(bg)[:, 0:256], lhsT=wg[:, k, ft * 128:(ft + 1) * 128], rhs=xT[:, k, :],
                            start=(k == 0), stop=(k == 15)), reads=[wgk, xTk], writes=["ps%d" % bg])
                    for k in range(16):
                        S.op("tensor", lambda e, k=k, ft=ft, bu=bu, wu=wu, xT=xT: e.matmul(
                            b.ps(bu)[:, 0:256], lhsT=wu[:, k, ft * 128:(ft + 1) * 128], rhs=xT[:, k, :],
                            start=(k == 0), stop=(k == 15)), reads=[wuk, xTk], writes=["ps%d" % bu])
                    S.op("scalar", lambda e, bg=bg, si=si: e.activation(out=sa[si], in_=b.ps(bg)[:, 0:256], func=AF.Silu),
                         reads=["ps%d" % bg], writes=["sa%d" % si])
                    S.op("vector", lambda e, bu=bu, si=si, fh=fh, ft=ft: e.tensor_tensor(
                        out=gT[:, fh * 4 + ft, :], in0=b.ps(bu)[:, 0:256], in1=sa[si], op=ALU.mult),
                         reads=["ps%d" % bu, "sa%d" % si], writes=["gT"])

            dcn = [0]

            def down(ex, nh):
                wdv, wdk = piece(4 + nh, 8)
                for cc in range(2):
                    for nbk in range(2):
                        bank = 6 + (dcn[0] % 2)
                        dcn[0] += 1
                        for ft in range(8):
                            S.op("tensor", lambda e, ft=ft, cc=cc, nbk=nbk, bank=bank, wdv=wdv: e.matmul(
                                b.ps(bank), lhsT=gT[:, ft, cc * 128:(cc + 1) * 128], rhs=wdv[:, ft, nbk * 512:(nbk + 1) * 512],
                                start=(ft == 0), stop=(ft == 7)), reads=["gT", wdk], writes=["ps%d" % bank])
                        dst = ysb[cc][:, nh * 1024 + nbk * 512: nh * 1024 + (nbk + 1) * 512]
                        if nbk == 0:
                            S.op("vector", lambda e, dst=dst, bank=bank, cc=cc, ex=ex: e.tensor_scalar(
                                out=dst, in0=b.ps(bank), scalar1=gateT[:, cc, ex:ex + 1], scalar2=None, op0=ALU.mult),
                                 reads=["ps%d" % bank, "gateT"], writes=["ysb%d" % cc])
                        else:
                            S.op("scalar", lambda e, dst=dst, bank=bank, cc=cc, ex=ex: e.activation(
                                out=dst, in_=b.ps(bank), func=AF.Copy, scale=gateT[:, cc, ex:ex + 1]),
                                 reads=["ps%d" % bank, "gateT"], writes=["ysb%d" % cc])

            def scatter(ex):
                for cc in range(2):
                    S.op("gpsimd", lambda e, cc=cc, ex=ex: e.indirect_dma_start(
                        out=X[:, :], out_offset=bass.IndirectOffsetOnAxis(ap=idxT[:, cc, ex:ex + 1], axis=0),
                        in_=ysb[cc], in_offset=None, compute_op=ALU.add),
                         reads=["ysb%d" % cc, "idxT"], writes=["Xmoe"], dma=True)

            gather(0)
            for ex in range(NE):
                nxt = ex + 1 < NE
                if nxt:
                    gather(ex + 1)
                transposes(ex)
                hidden(ex, 0)
                if nxt:
                    load_piece(ex + 1, 0)
                    load_piece(ex + 1, 1)
                hidden(ex, 1)
                if nxt:
                    load_piece(ex + 1, 2)
                    load_piece(ex + 1, 3)
                down(ex, 0)
                if nxt:
                    load_piece(ex + 1, 4)
                down(ex, 1)
                if nxt:
                    load_piece(ex + 1, 5)
                scatter(ex)

        def phase_pool(X_src, X_dst):
            b.phase()
            global wring
            wring = [b.alloc("w%d" % i, [2048], BF16) for i in range(4)]
            hT = b.alloc("hT", [16, S_], BF16)
            xa = b.alloc("xa", [16, D_], BF16)
            strips = b.alloc("strips", [4, 6 * 512], BF16)
            edges = b.alloc("edges", [4, 2 * 128], BF16)
            scale_b = [b.alloc("scale_b0", [512], F32), b.alloc("scale_b1", [512], F32)]
            load_gain(mix_norm[1])
            wts = [wring[g].rearrange("p (k n) -> p k n", k=4) for g in range(4)]

            def load_consts():
                S.dma("gpsimd", strips.rearrange("p w (k n) -> p w k n", k=6), c_band.rearrange("w k p n -> p w k n"), writes=["strips"])
                S.dma("gpsimd", edges.rearrange("p w (k n) -> p w k n", k=2), c_bedge.rearrange("w k p n -> p w k n"), writes=["edges"])
                for g in range(4):
                    S.dma("gpsimd", wts[g][:, 0:4, :], pool_w[g].rearrange("(k p) n -> p k n", p=128), writes=["w%d" % g])
                for g in range(4):
                    sbg = scale_b[g % 2]
                    S.dma("gpsimd", sbg, pool_scale[g * 512:(g + 1) * 512].partition_broadcast(128), writes=["scale_b%d" % (g % 2)])
                    for k in range(4):
                        S.op("gpsimd", lambda e, g=g, k=k, sbg=sbg: e.tensor_tensor(out=wts[g][:, k, :], in0=wts[g][:, k, :], in1=sbg, op=ALU.mult),
                             reads=["w%d" % g, "scale_b%d" % (g % 2)], writes=["w%d" % g])
            xts = [b.alloc("xt0", [D_], F32), b.alloc("xt1", [D_], F32)]
            ssq2 = [b.alloc("ssq0", [1], F32), b.alloc("ssq1", [1], F32)]
            rstd2 = [b.alloc("rstd0", [1], F32), b.alloc("rstd1", [1], F32)]
            NT = S_ // 128
            S.dma("sync", xts[0], X_src[0:128, :], writes=["xt0"])
            for ti in range(NT):
                i = ti % 2
                xt, ssq, rstd = xts[i], ssq2[i], rstd2[i]
                if ti + 1 < NT:
                    S.dma("sync", xts[1 - i], X_src[(ti + 1) * 128:(ti + 2) * 128, :], writes=["xt%d" % (1 - i)])
                S.op("scalar", lambda e, xt=xt, ssq=ssq, ti=ti: e.activation(out=xa[:, ti, :], in_=xt, func=AF.Square, accum_out=ssq),
                     reads=["xt%d" % i], writes=["xa%d" % ti, "ssq%d" % i])
                S.op("scalar", lambda e, ssq=ssq, rstd=rstd: e.activation(out=rstd, in_=ssq, func=AF.Sqrt, scale=1.0 / D_, bias=eps),
                     reads=["ssq%d" % i], writes=["rstd%d" % i])
                S.op("vector", lambda e, rstd=rstd: e.reciprocal(out=rstd, in_=rstd), reads=["rstd%d" % i], writes=["rstd%d" % i])
                S.op("vector", lambda e, xt=xt, rstd=rstd, ti=ti: e.scalar_tensor_tensor(out=xa[:, ti, :], in0=xt, scalar=rstd, in1=gb,
                                                                                        op0=ALU.mult, op1=ALU.mult),
                     reads=["xt%d" % i, "rstd%d" % i, "gb"], writes=["xa%d" % ti])
                if ti == 2:
                    load_consts()
            sv = strips.rearrange("p w (k n) -> p w k n", k=6)
            ev = edges.rearrange("p w (k n) -> p w k n", k=2)
            pc = 0
            for J in range(4):
                for ch in range(16):
                    wi = ch // 4
                    bank = pc % 8
                    pc += 1
                    pk = "ps%d" % bank
                    for pos in range(4):
                        Sx = 4 * J + pos
                        kidx = pos
                        if Sx == 0:
                            kidx = 4
                        elif Sx == NT - 1:
                            kidx = 5
                        S.op("tensor", lambda e, Sx=Sx, ch=ch, wi=wi, kidx=kidx, bank=bank, pos=pos: e.matmul(
                            b.ps(bank), lhsT=xa[:, Sx, ch * 128:(ch + 1) * 128], rhs=sv[:, wi, kidx, :],
                            start=(pos == 0), stop=False), reads=["xa%d" % Sx, "strips"], writes=[pk])
                    last_full = True
                    if J > 0:
                        S.op("tensor", lambda e, J=J, ch=ch, wi=wi, bank=bank: e.matmul(
                            b.ps(bank)[:, 0:128], lhsT=xa[:, 4 * J - 1, ch * 128:(ch + 1) * 128], rhs=ev[:, wi, 0, :],
                            start=False, stop=False), reads=["xa%d" % (4 * J - 1), "edges"], writes=[pk])
                    if J < 3:
                        S.op("tensor", lambda e, J=J, ch=ch, wi=wi, bank=bank: e.matmul(
                            b.ps(bank)[:, 384:512], lhsT=xa[:, 4 * J + 4, ch * 128:(ch + 1) * 128], rhs=ev[:, wi, 1, :],
                            start=False, stop=False), reads=["xa%d" % (4 * J + 4), "edges"], writes=[pk])
                    dst = hT[:, ch, J * 512:(J + 1) * 512]
                    if pc % 2 == 0:
                        S.op("vector", lambda e, dst=dst, bank=bank: e.tensor_copy(out=dst, in_=b.ps(bank)), reads=[pk], writes=["hTc%d_%d" % (ch, J)])
                    else:
                        S.op("scalar", lambda e, dst=dst, bank=bank: e.copy(out=dst, in_=b.ps(bank)), reads=[pk], writes=["hTc%d_%d" % (ch, J)])
            for ti in range(S_ // 128):
                i = ti % 2
                r = xts[i]
                rk = "xt%d" % i
                rows = slice(ti * 128, (ti + 1) * 128)
                S.dma("sync", r, X_src[rows, :], writes=[rk])
                for g in range(4):
                    bank = (ti * 4 + g) % 8
                    for k in range(4):
                        S.op("tensor", lambda e, k=k, ti=ti, g=g, bank=bank: e.matmul(
                            b.ps(bank), lhsT=hT[:, g * 4 + k, ti * 128:(ti + 1) * 128], rhs=wts[g][:, k, :],
                            start=(k == 0), stop=(k == 3)), reads=["hTc%d_%d" % (g * 4 + k, ti // 4), "w%d" % g], writes=["ps%d" % bank])
                    S.op("vector", lambda e, g=g, bank=bank, r=r: e.tensor_tensor(out=r[:, g * 512:(g + 1) * 512], in0=b.ps(bank),
                                                                               in1=r[:, g * 512:(g + 1) * 512], op=ALU.add),
                         reads=["ps%d" % bank, rk], writes=[rk])
                S.dma("sync", X_dst[rows, :], r, reads=[rk], writes=["Xdst"])

        def phase_final(X_src):
            b.phase()
            xts = [b.alloc("xt0", [D_], F32), b.alloc("xt1", [D_], F32)]
            junk = b.alloc("njunk", [D_], BF16)
            ssq2 = [b.alloc("ssq0", [1], F32), b.alloc("ssq1", [1], F32)]
            rstd2 = [b.alloc("rstd0", [1], F32), b.alloc("rstd1", [1], F32)]
            yo = [b.alloc("yo0", [D_], F32), b.alloc("yo1", [D_], F32)]
            load_gain(final_norm)
            for ti in range(S_ // 128):
                rows = slice(ti * 128, (ti + 1) * 128)
                i = ti % 2
                xt, ssq, rstd, y = xts[i], ssq2[i], rstd2[i], yo[i]
                if ti == 0:
                    S.dma("sync", xt, X_src[rows, :], writes=["xt%d" % i])
                if ti + 1 < S_ // 128:
                    S.dma("sync", xts[1 - i], X_src[(ti + 1) * 128:(ti + 2) * 128, :], writes=["xt%d" % (1 - i)])
                S.op("scalar", lambda e, xt=xt, ssq=ssq: e.activation(out=junk, in_=xt, func=AF.Square, accum_out=ssq),
                     reads=["xt%d" % i], writes=["njunk", "ssq%d" % i])
                S.op("scalar", lambda e, ssq=ssq, rstd=rstd: e.activation(out=rstd, in_=ssq, func=AF.Sqrt, scale=1.0 / D_, bias=eps),
                     reads=["ssq%d" % i], writes=["rstd%d" % i])
                S.op("vector", lambda e, rstd=rstd: e.reciprocal(out=rstd, in_=rstd), reads=["rstd%d" % i], writes=["rstd%d" % i])
                S.op("vector", lambda e, xt=xt, y=y, rstd=rstd: e.scalar_tensor_tensor(out=y, in0=xt, scalar=rstd, in1=gb, op0=ALU.mult, op1=ALU.mult),
                     reads=["xt%d" % i, "rstd%d" % i, "gb"], writes=["yo%d" % i])
                S.dma("sync", y_out[rows, :], y, reads=["yo%d" % i], writes=["y"])

        def copy_out(X_src):
            b.phase()
            t = [b.alloc("co0", [D_], F32), b.alloc("co1", [D_], F32)]
            for ti in range(S_ // 128):
                rows = slice(ti * 128, (ti + 1) * 128)
                S.dma("sync", t[ti % 2], X_src[rows, :], writes=["co%d" % (ti % 2)])
                S.dma("sync", y_out[rows, :], t[ti % 2], reads=["co%d" % (ti % 2)], writes=["y"])

        def run():
            phase_attn_proj()
            phase_attention()
            phase_fourier()
            phase_outproj(w_out, x_in, XB)
            if stage == 1:
                return copy_out(XB)
            phase_cross(0, XB, XA)
            if stage == 2:
                return copy_out(XA)
            phase_moe(0, XA)
            if stage == 30:
                return
            if stage == 3:
                return copy_out(XA)
            phase_pool(XA, XB)
            if stage == 4:
                return copy_out(XB)
            phase_cross(1, XB, XA)
            if stage == 5:
                return copy_out(XA)
            phase_moe(1, XA)
            if stage == 6:
                return copy_out(XA)
            phase_final(XA)

        run()
        counts = S.finalize()
        print("instr counts", counts, flush=True)
    return nc


wring = []


def host_constants():
    S = S_
    rows = S // 64
    row_idx = np.repeat(np.arange(rows), 64).astype(np.float32)
    col_idx = np.tile(np.arange(64), rows).astype(np.float32)
    inv_freq = (1.0 / (10000.0 ** (np.arange(0, 64, 2, dtype=np.float32) / 64))).astype(np.float32)
    ang = np.concatenate([row_idx[:, None] * inv_freq[None, :], col_idx[:, None] * inv_freq[None, :]], axis=-1)
    cos = np.cos(ang).astype(np.float32)
    sin = np.sin(ang).astype(np.float32)
    c_rope = np.stack([np.tile(cos, (1, 4)), np.tile(sin, (1, 4))]).astype(np.float32)
    n = np.arange(S, dtype=np.int64)
    ph = (np.outer(n, n) % S).astype(np.float64) * (2 * np.pi / S)
    c_dftS = np.stack([np.cos(ph), np.sin(ph)]).astype(np.float32)
    c = np.arange(128, dtype=np.int64)
    pc = (np.outer(c, c) % 128).astype(np.float64) * (2 * np.pi / 128)
    c_dftC = np.stack([np.cos(pc) / 512.0, -np.sin(pc) / 512.0]).astype(np.float32)
    t = np.arange(S)
    bands = []
    bedges = []
    for w in (2, 4, 8, 16):
        lo = np.clip(t - w // 2, 0, S)
        hi = np.clip(t + w - w // 2, 0, S)
        M = np.zeros((S, S), np.float32)
        for tt_ in range(S):
            M[lo[tt_]:hi[tt_], tt_] = 1.0 / float(hi[tt_] - lo[tt_])
        M[np.arange(S), np.arange(S)] -= 1.0
        st = [M[(4 + p) * 128:(5 + p) * 128, 512:1024] for p in range(4)]
        st.append(M[0:128, 0:512])
        st.append(M[S - 128:S, S - 512:S])
        bands.append(np.stack(st))
        bedges.append(np.stack([M[3 * 128:4 * 128, 512:640], M[8 * 128:9 * 128, 7 * 128:8 * 128]]))
    c_band = np.stack(bands).astype(np.float32)
    c_bedge = np.stack(bedges).astype(np.float32)
    return {"c_rope": c_rope, "c_dftS": c_dftS, "c_dftC": c_dftC, "c_band": c_band, "c_bedge": c_bedge}


def make_in_maps(inputs, n_cores=8):
    g = {k: np.ascontiguousarray(np.asarray(v), dtype=np.float32) for k, v in inputs.items()}
    shared = {
        "mix_norm": g["mix_norm"], "attn_w_in": g["attn_w_in"][0], "q_gain": g["q_gain"][0], "k_gain": g["k_gain"][0],
        "fourier_w": g["fourier_w"][0], "attn_w_out": g["attn_w_out"][0], "pool_w": g["pool_w"][0],
        "pool_scale": g["pool_scale"][0], "cross_norm": g["cross_norm"], "mem_norm": g["mem_norm"],
        "cross_w_q": g["cross_w_q"], "cross_w_k": g["cross_w_k"], "cross_w_v": g["cross_w_v"], "cross_w_o": g["cross_w_o"],
        "ffn_norm": g["ffn_norm"], "router_w": g["router_w"], "expert_w_gate": g["expert_w_gate"],
        "expert_w_up": g["expert_w_up"], "expert_w_down": g["expert_w_down"], "final_norm": g["final_norm"],
    }
    shared.update(host_constants())
    maps = []
    for c in range(n_cores):
        m = dict(shared)
        m["x"] = g["x"][c % 4]
        m["mem"] = g["mem"][c % 4]
        maps.append(m)
    return maps


def kernel(**inputs):
    nc = build_program()
    maps = make_in_maps(inputs, 4)
    res = run_bass_kernel_spmd(nc, maps, core_ids=list(range(4)))
    out = np.stack([np.asarray(res.results[c]["y"], dtype=np.float32) for c in range(4)], axis=0)
    return out
```

```python
from contextlib import ExitStack
import numpy as np
import concourse.bass as bass
import concourse.mybir as mybir
from concourse.bass_utils import run_bass_kernel_spmd

F32 = mybir.dt.float32
BF16 = mybir.dt.bfloat16
I32 = mybir.dt.int32
U32 = mybir.dt.uint32
U8 = mybir.dt.uint8
ALU = mybir.AluOpType
AF = mybir.ActivationFunctionType
AX = mybir.AxisListType

S_ = 2048
D_ = 2048
NE = 16
CAP = 256
ENGS = ["sync", "scalar", "gpsimd", "vector", "tensor"]
DMA_K = 8


class Op:
    __slots__ = ("eng", "fn", "deps", "dma", "idx", "needed", "sig", "waits", "dma_j")

    def __init__(self, eng, fn, deps, dma):
        self.eng = eng
        self.fn = fn
        self.deps = deps
        self.dma = dma
        self.needed = False
        self.sig = None
        self.waits = []
        self.dma_j = None


class Sched:
    def __init__(self, nc):
        self.nc = nc
        self.ops = []
        self.last_w = {}
        self.readers = {}
        self.dma_count = {e: 0 for e in ENGS}
        self.dma_ops = {e: [] for e in ENGS}
        self.last_on = {e: None for e in ENGS}
        self.last_comp = {e: None for e in ENGS}
        self.fence = set()
        self.fenced = {e: True for e in ENGS}

    def barrier(self):
        f = set()
        for e in ENGS:
            if self.last_on[e] is not None:
                f.add(self.last_on[e])
            if self.last_comp[e] is not None:
                f.add(self.last_comp[e])
            n = self.dma_count[e]
            for j in range(max(0, n - DMA_K), n):
                f.add(self.dma_ops[e][j].idx)
        self.fence = f
        self.fenced = {e: False for e in ENGS}
        self.last_w = {}
        self.readers = {}

    def op(self, eng, fn, reads=(), writes=(), dma=False):
        pr = [r for r in reads if r.startswith("ps")]
        if pr:
            reads = [r for r in reads if not r.startswith("ps")]
            writes = list(writes) + pr
        deps = set()
        if not self.fenced[eng]:
            deps |= self.fence
            self.fenced[eng] = True
        for r in reads:
            w = self.last_w.get(r)
            if w is not None:
                deps.add(w)
        for w_ in writes:
            w = self.last_w.get(w_)
            if w is not None:
                deps.add(w)
            for r in self.readers.get(w_, ()):
                deps.add(r)
        o = Op(eng, fn, deps, dma)
        o.idx = len(self.ops)
        if dma:
            j = self.dma_count[eng]
            o.dma_j = j
            self.dma_count[eng] += 1
            if j >= DMA_K:
                deps.add(self.dma_ops[eng][j - DMA_K].idx)
            self.dma_ops[eng].append(o)
        deps.discard(o.idx)
        self.ops.append(o)
        self.last_on[eng] = o.idx
        if not dma:
            self.last_comp[eng] = o.idx
        for r in reads:
            self.readers.setdefault(r, []).append(o.idx)
        for w_ in writes:
            self.last_w[w_] = o.idx
            self.readers[w_] = []
        return o

    def dma(self, eng, out, in_, reads=(), writes=(), **kw):
        return self.op(eng, lambda e: e.dma_start(out=out, in_=in_, **kw), reads, writes, dma=True)

    def finalize(self):
        nc = self.nc
        ops = self.ops
        tail = []
        for e in ENGS:
            n = self.dma_count[e]
            for j in range(max(0, n - DMA_K), n):
                tail.append(self.dma_ops[e][j].idx)
        fin = Op("sync", None, set(tail), False)
        fin.idx = len(ops)
        ops.append(fin)
        for o in ops:
            for d in o.deps:
                p = ops[d]
                if p.eng == "tensor" and o.eng == "tensor" and not p.dma and not o.dma:
                    continue
                p.needed = True
        with ExitStack() as st:
            csem = {e: st.enter_context(nc.semaphore("c_" + e)) for e in ENGS}
            dsem = {e: [st.enter_context(nc.semaphore("d_%s%d" % (e, k))) for k in range(DMA_K)]
                    for e in ENGS if self.dma_count[e] > 0}
            cnt = {e: 0 for e in ENGS}
            for o in ops:
                if o.dma:
                    k = o.dma_j % DMA_K
                    o.sig = (dsem[o.eng][k], 16, 16 * (o.dma_j // DMA_K + 1))
                elif o.needed:
                    cnt[o.eng] += 1
                    o.sig = (csem[o.eng], 1, cnt[o.eng])
            seen = {e: {} for e in ENGS}
            for o in ops:
                need = {}
                for d in o.deps:
                    p = ops[d]
                    if p.sig is None:
                        continue
                    if p.eng == "tensor" and o.eng == "tensor" and not p.dma and not o.dma:
                        continue
                    sem, _, val = p.sig
                    key = id(sem)
                    if key not in need or need[key][1] < val:
                        need[key] = (sem, val)
                sn = seen[o.eng]
                for key, (sem, val) in need.items():
                    if sn.get(key, 0) >= val:
                        continue
                    sn[key] = val
                    o.waits.append((sem, val))
            by_eng = {e: [o for o in ops if o.eng == e] for e in ENGS}

            def emit(name, e):
                for o in by_eng[name]:
                    for sem, val in o.waits:
                        e.wait_ge(sem, val)
                    if o.fn is None:
                        continue
                    ins = o.fn(e)
                    if o.sig is not None:
                        ins.then_inc(o.sig[0], o.sig[1])

            with nc.Block() as block:
                @block.sync
                def _(e):
                    emit("sync", e)

                @block.scalar
                def _(e):
                    emit("scalar", e)

                @block.gpsimd
                def _(e):
                    emit("gpsimd", e)

                @block.vector
                def _(e):
                    emit("vector", e)

                @block.tensor
                def _(e):
                    emit("tensor", e)
        return {e: len(by_eng[e]) for e in ENGS}


ARENA = 207 * 1024


class B:
    def __init__(self, nc, st):
        self.nc = nc
        self.S = Sched(nc)
        self.arena = st.enter_context(nc.sbuf_tensor("arena", [128, ARENA], U8))
        self.off = 0
        self.mark = 0
        self.psf = [st.enter_context(nc.psum_tensor("ps%d" % i, [128, 512], F32)) for i in range(8)]
        self.wslot = 0
        self.uid = 0

    def alloc(self, name, free_shape, dt):
        n = 1
        for s in free_shape:
            n *= s
        nb = n * (2 if dt == BF16 else 4)
        nb = (nb + 63) // 64 * 64
        assert self.off + nb <= ARENA, (name, self.off, nb)
        v = self.arena[:, self.off:self.off + nb].bitcast(dt)
        self.off += nb
        nn = 1
        for s in free_shape:
            nn *= s
        v = v[:, 0:nn]
        if len(free_shape) == 2:
            v = v.rearrange("p (a b) -> p a b", a=free_shape[0])
        elif len(free_shape) == 3:
            v = v.rearrange("p (a b c) -> p a b c", a=free_shape[0], b=free_shape[1])
        return v

    def phase(self):
        self.S.barrier()
        self.off = self.mark

    def ps(self, i):
        return self.psf[i][:]

    def psb(self, i):
        return self.psf[i][:].bitcast(BF16).rearrange("p (a b) -> p a b", a=8)


def build_program(stage=99):
    nc = bass.Bass("TRN2", target_bir_lowering=False)

    def din(name, shape, dt=F32):
        return nc.dram_tensor(name, list(shape), dt, kind="ExternalInput").ap()

    def dscr(name, shape, dt):
        return nc.dram_tensor(name, list(shape), dt, kind="Internal").ap()

    x_in = din("x", [S_, D_])
    mem_in = din("mem", [256, D_])
    mix_norm = din("mix_norm", [2, D_])
    w_in = din("attn_w_in", [D_, 3072])
    q_gain = din("q_gain", [128])
    k_gain = din("k_gain", [128])
    fourier_w = din("fourier_w", [4, 128, 128])
    w_out = din("attn_w_out", [D_, D_])
    pool_w = din("pool_w", [4, 512, 512])
    pool_scale = din("pool_scale", [D_])
    cross_norm = din("cross_norm", [2, D_])
    mem_norm = din("mem_norm", [2, D_])
    cw_q = din("cross_w_q", [2, D_, D_])
    cw_k = din("cross_w_k", [2, D_, D_])
    cw_v = din("cross_w_v", [2, D_, D_])
    cw_o = din("cross_w_o", [2, D_, D_])
    ffn_norm = din("ffn_norm", [2, D_])
    router_w = din("router_w", [2, D_, NE])
    ew_gate = din("expert_w_gate", [2, NE, D_, 1024])
    ew_up = din("expert_w_up", [2, NE, D_, 1024])
    ew_down = din("expert_w_down", [2, NE, 1024, D_])
    final_norm = din("final_norm", [D_])
    c_rope = din("c_rope", [2, S_, 256])
    c_dftS = din("c_dftS", [2, S_, S_])
    c_dftC = din("c_dftC", [2, 128, 128])
    c_band = din("c_band", [4, 6, 128, 512])
    c_bedge = din("c_bedge", [4, 2, 128, 128])
    y_out = nc.dram_tensor("y", [S_, D_], F32, kind="ExternalOutput").ap()

    XA = dscr("XA", [S_, D_], F32)
    XB = dscr("XB", [S_, D_], F32)
    qT_d = dscr("qT_d", [12, 128, S_], BF16)
    kT_d = dscr("kT_d", [4, 128, S_], BF16)
    v_d = dscr("v_d", [S_, 512], BF16)
    fT_d = dscr("fT_d", [4, 128, S_], BF16)
    mixT_d = dscr("mixT_d", [16, 128, S_], BF16)
    hmoe_d = dscr("hmoe_d", [S_, D_], BF16)

    with ExitStack() as st:
        b = B(nc, st)
        S = b.S
        identf = b.alloc("identf", [128], F32)
        identb = b.alloc("identb", [128], BF16)
        onesb = b.alloc("onesb", [128], BF16)
        onesf = b.alloc("onesf", [128], F32)
        eps = b.alloc("eps", [1], F32)
        gb = b.alloc("gb", [D_], F32)
        S.op("gpsimd", lambda e: e.memset(identf, 0.0), writes=["identf"])
        S.op("gpsimd", lambda e: e.affine_select(out=identf, in_=identf, pattern=[[-1, 128]],
                                                 compare_op=ALU.not_equal, fill=1.0, base=0,
                                                 channel_multiplier=1), reads=["identf"], writes=["identf"])
        S.op("vector", lambda e: e.tensor_copy(out=identb, in_=identf), reads=["identf"], writes=["identb"])
        S.op("vector", lambda e: e.memset(onesb, 1.0), writes=["onesb"])
        S.op("vector", lambda e: e.memset(onesf, 1.0), writes=["onesf"])
        S.op("vector", lambda e: e.memset(eps, 1e-6), writes=["eps"])
        b.mark = b.off
        CONST = ["identf", "identb", "onesb", "onesf", "eps"]

        def keep_consts():
            pass

        def load_gain(gain_ap):
            S.dma("sync", gb, gain_ap.partition_broadcast(128), writes=["gb"])

        def norm_tile(src_rows, ti, xts, xn, ssq, rstd, out_fp32=None):
            xt = xts[ti % 2]
            xk = "xt%d" % (ti % 2)
            S.dma("sync", xt, src_rows, writes=[xk])
            S.op("scalar", lambda e: e.activation(out=xn, in_=xt, func=AF.Square, accum_out=ssq),
                 reads=[xk], writes=["xn", "ssq"])
            S.op("scalar", lambda e: e.activation(out=rstd, in_=ssq, func=AF.Sqrt, scale=1.0 / D_, bias=eps),
                 reads=["ssq"], writes=["rstd"])
            S.op("vector", lambda e: e.reciprocal(out=rstd, in_=rstd), reads=["rstd"], writes=["rstd"])
            if out_fp32 is not None:
                S.op("vector", lambda e: e.scalar_tensor_tensor(out=out_fp32, in0=xt, scalar=rstd, in1=gb,
                                                                op0=ALU.mult, op1=ALU.mult),
                     reads=[xk, "rstd", "gb"], writes=["xnf"])
                S.op("gpsimd", lambda e: e.tensor_copy(out=xn, in_=out_fp32), reads=["xnf"], writes=["xn"])
            else:
                S.op("vector", lambda e: e.scalar_tensor_tensor(out=xn, in0=xt, scalar=rstd, in1=gb,
                                                                op0=ALU.mult, op1=ALU.mult),
                     reads=[xk, "rstd", "gb"], writes=["xn"])

        def transpose_to(hT, col0, xn, kc=16):
            for half in range(kc // 8):
                pb = b.psb(half)
                pk = "ps%d" % half
                for k in range(8):
                    kk = half * 8 + k
                    S.op("tensor", lambda e, k=k, kk=kk, pb=pb: e.transpose(out=pb[:, k, :], in_=xn[:, kk * 128:(kk + 1) * 128],
                                                                           identity=identb),
                         reads=["xn"], writes=[pk])
                eng = "vector" if half == 0 else "scalar"
                dst = hT[:, half * 8:(half + 1) * 8, col0:col0 + 128]
                if eng == "vector":
                    S.op("vector", lambda e, dst=dst, pb=pb: e.tensor_copy(out=dst, in_=pb), reads=[pk], writes=["hT"])
                else:
                    S.op("scalar", lambda e, dst=dst, pb=pb: e.copy(out=dst, in_=pb), reads=[pk], writes=["hT"])

        def norm_loop(rows_of, n_tiles, hT, hkey="hT", junk=None):
            xts = [b.alloc("xt0", [D_], F32), b.alloc("xt1", [D_], F32)]
            xn2 = [b.alloc("xn0", [D_], BF16), b.alloc("xn1", [D_], BF16)]
            if junk is None:
                junk = b.alloc("njunk", [D_], BF16)
            ssq2 = [b.alloc("ssq0", [1], F32), b.alloc("ssq1", [1], F32)]
            rstd2 = [b.alloc("rstd0", [1], F32), b.alloc("rstd1", [1], F32)]

            def load(ti):
                S.dma("sync", xts[ti % 2], rows_of(ti), writes=["xt%d" % (ti % 2)])

            def early(ti):
                i = ti % 2
                xt, xn, ssq, rstd = xts[i], xn2[i], ssq2[i], rstd2[i]
                S.op("scalar", lambda e: e.activation(out=junk, in_=xt, func=AF.Square, accum_out=ssq),
                     reads=["xt%d" % i], writes=["njunk", "ssq%d" % i])
                S.op("scalar", lambda e: e.activation(out=rstd, in_=ssq, func=AF.Sqrt, scale=1.0 / D_, bias=eps),
                     reads=["ssq%d" % i], writes=["rstd%d" % i])
                S.op("vector", lambda e: e.reciprocal(out=rstd, in_=rstd), reads=["rstd%d" % i], writes=["rstd%d" % i])
                S.op("vector", lambda e: e.scalar_tensor_tensor(out=xn, in0=xt, scalar=rstd, in1=gb, op0=ALU.mult, op1=ALU.mult),
                     reads=["xt%d" % i, "rstd%d" % i, "gb"], writes=["xn%d" % i])

            def late(ti):
                i = ti % 2
                xn = xn2[i]
                for half in range(2):
                    pb = b.psb(half)
                    pk = "ps%d" % half
                    for k in range(8):
                        kk = half * 8 + k
                        S.op("tensor", lambda e, k=k, kk=kk, pb=pb: e.transpose(out=pb[:, k, :], in_=xn[:, kk * 128:(kk + 1) * 128],
                                                                               identity=identb),
                             reads=["xn%d" % i], writes=[pk])
                    dst = hT[:, half * 8:(half + 1) * 8, ti * 128:(ti + 1) * 128]
                    if half == 0:
                        S.op("vector", lambda e, dst=dst, pb=pb: e.tensor_copy(out=dst, in_=pb), reads=[pk], writes=[hkey])
                    else:
                        S.op("scalar", lambda e, dst=dst, pb=pb: e.copy(out=dst, in_=pb), reads=[pk], writes=[hkey])

            load(0)
            for ti in range(n_tiles + 1):
                if ti + 1 < n_tiles:
                    load(ti + 1)
                if ti < n_tiles:
                    early(ti)
                if ti >= 1:
                    late(ti - 1)

        def norm_phase_alloc():
            xts = [b.alloc("xt0", [D_], F32), b.alloc("xt1", [D_], F32)]
            xn = b.alloc("xn", [D_], BF16)
            ssq = b.alloc("ssq", [1], F32)
            rstd = b.alloc("rstd", [1], F32)
            return xts, xn, ssq, rstd

        def wpiece(src_ap, kdim=16):
            slot = b.wslot % len(wring)
            b.wslot += 1
            wt = wring[slot].rearrange("p (k n) -> p k n", k=kdim)
            S.dma("gpsimd", wt, src_ap, writes=["w%d" % slot])
            return wt, "w%d" % slot

        def linear_tok(hT, T, w_src, n_cols, epilogue, kc=16, banks=(2, 3, 4, 5, 6, 7), hkey="hT"):
            cnt = 0
            nblk = n_cols // 512
            nxt = wpiece(w_src(0).rearrange("(k p) n -> p k n", p=128))
            for nb in range(nblk):
                wt, wk = nxt
                if nb + 1 < nblk:
                    nxt = wpiece(w_src(nb + 1).rearrange("(k p) n -> p k n", p=128))
                for ti in range(T // 128):
                    bank = banks[cnt % len(banks)]
                    cnt += 1
                    pk = "ps%d" % bank
                    for k in range(kc):
                        S.op("tensor", lambda e, k=k, ti=ti, bank=bank, wt=wt: e.matmul(
                            b.ps(bank), lhsT=hT[:, k, ti * 128:(ti + 1) * 128], rhs=wt[:, k, :],
                            start=(k == 0), stop=(k == kc - 1)), reads=[hkey, wk], writes=[pk])
                    epilogue(nb, ti, b.ps(bank), pk)

        def linear_feat(hT, T, w_src, n_cols, epilogue, kc=16, banks=(2, 3, 4, 5, 6, 7), hkey="hT"):
            cnt = 0
            nblk = n_cols // 512
            nxt = wpiece(w_src(0).rearrange("(k p) n -> p k n", p=128))
            for nb in range(nblk):
                wt, wk = nxt
                if nb + 1 < nblk:
                    nxt = wpiece(w_src(nb + 1).rearrange("(k p) n -> p k n", p=128))
                for nt in range(4):
                    for tb in range(T // 512):
                        bank = banks[cnt % len(banks)]
                        cnt += 1
                        pk = "ps%d" % bank
                        for k in range(kc):
                            S.op("tensor", lambda e, k=k, nt=nt, tb=tb, bank=bank, wt=wt: e.matmul(
                                b.ps(bank), lhsT=wt[:, k, nt * 128:(nt + 1) * 128],
                                rhs=hT[:, k, tb * 512:(tb + 1) * 512],
                                start=(k == 0), stop=(k == kc - 1)), reads=[hkey, wk], writes=[pk])
                        epilogue(nb * 4 + nt, tb, b.ps(bank), pk)

        TG = 1024
        NTG = S_ // TG

        def make_resid_epilogue(X_src, X_dst, t_base, order, scale_b=None):
            NRX = 3
            rx = [b.alloc("rx%d" % i, [512], F32) for i in range(NRX)]
            ctr = [0]
            issued = [0]
            sc_tmp = b.alloc("sc_tmp", [512], F32) if scale_b is not None else None

            def issue_loads(upto):
                while issued[0] < min(upto, len(order)):
                    k = issued[0]
                    nb_, ti_ = order[k]
                    rows = slice(t_base + ti_ * 128, t_base + (ti_ + 1) * 128)
                    cols = slice(nb_ * 512, (nb_ + 1) * 512)
                    S.dma("sync", rx[k % NRX], X_src[rows, cols], writes=["rx%d" % (k % NRX)])
                    issued[0] += 1

            def ep(nb, ti, ps, pk):
                k = ctr[0]
                ctr[0] += 1
                assert order[k] == (nb, ti), (order[k], nb, ti)
                issue_loads(k + 2)
                i = k % NRX
                r = rx[i]
                rk = "rx%d" % i
                rows = slice(t_base + ti * 128, t_base + (ti + 1) * 128)
                cols = slice(nb * 512, (nb + 1) * 512)
                if scale_b is not None:
                    S.op("vector", lambda e: e.tensor_tensor(out=sc_tmp, in0=ps, in1=scale_b[:, cols], op=ALU.mult),
                         reads=[pk, "scale_b"], writes=["sc_tmp"])
                    S.op("gpsimd", lambda e: e.tensor_tensor(out=r, in0=sc_tmp, in1=r, op=ALU.add),
                         reads=["sc_tmp", rk], writes=[rk])
                else:
                    S.op("vector", lambda e: e.tensor_tensor(out=r, in0=ps, in1=r, op=ALU.add),
                         reads=[pk, rk], writes=[rk])
                S.dma("sync", X_dst[rows, cols], r, reads=[rk], writes=["Xdst"])
            return ep

        def phase_attn_proj():
            for tg in range(NTG):
                b.phase()
                global wring
                wring = [b.alloc("w%d" % i, [8192], BF16) for i in range(3)]
                hT = b.alloc("hT", [16, TG], BF16)
                load_gain(mix_norm[0])
                norm_loop(lambda ti: x_in[tg * TG + ti * 128: tg * TG + (ti + 1) * 128, :], TG // 128, hT)
                NB3 = 3
                stage = [b.alloc("stg0", [4, TG], BF16), b.alloc("stg1", [4, TG], BF16)]
                junk = [b.alloc("junk%d" % i, [512], BF16) for i in range(NB3)]
                xg = [b.alloc("xg%d" % i, [512], F32) for i in range(NB3)]
                ro = [b.alloc("ro%d" % i, [512], F32) for i in range(NB3)]
                tt = [[b.alloc("tt%d_%d" % (i, j), [256], F32) for j in range(4)] for i in range(NB3)]
                qr = [b.alloc("qr%d" % i, [512], BF16) for i in range(NB3)]
                ssq4 = [b.alloc("ssq4_%d" % i, [4], F32) for i in range(NB3)]
                rs4 = [b.alloc("rs4_%d" % i, [4], F32) for i in range(NB3)]
                gq4 = b.alloc("gq4", [512], F32)
                gk4 = b.alloc("gk4", [512], F32)
                cs = [b.alloc("cs0", [2, 256], F32), b.alloc("cs1", [2, 256], F32)]
                vst = [b.alloc("vst0", [512], BF16), b.alloc("vst1", [512], BF16)]
                for h in range(4):
                    S.dma("sync", gq4[:, h * 128:(h + 1) * 128], q_gain.partition_broadcast(128), writes=["gq4"])
                    S.dma("sync", gk4[:, h * 128:(h + 1) * 128], k_gain.partition_broadcast(128), writes=["gk4"])
                cctr = [0]
                stB = {}
                stC = {}

                def load_cs(n):
                    ti_ = n % (TG // 128)
                    rows = slice(tg * TG + ti_ * 128, tg * TG + (ti_ + 1) * 128)
                    S.dma("sync", cs[n % 2][:, 0, :], c_rope[0, rows, :], writes=["cs%d" % (n % 2)])
                    S.dma("sync", cs[n % 2][:, 1, :], c_rope[1, rows, :], writes=["cs%d" % (n % 2)])

                def run_stage(d, n):
                    f = d.pop(n, None)
                    if f is not None:
                        f()

                def flush():
                    n = cctr[0]
                    run_stage(stB, n - 1)
                    run_stage(stC, n - 2)
                    run_stage(stC, n - 1)

                def qk_epilogue(nb, ti, ps, pk):
                    gg, ggk = (gq4, "gq4") if nb < 3 else (gk4, "gk4")
                    n = cctr[0]
                    cctr[0] += 1
                    eb = n % NB3
                    c2 = cs[n % 2]
                    ck = "cs%d" % (n % 2)
                    if n == 0:
                        load_cs(0)
                    if n + 1 < 4 * (TG // 128):
                        load_cs(n + 1)
                    jk, sq, rs = junk[eb], ssq4[eb], rs4[eb]
                    x_, r_, q_ = xg[eb], ro[eb], qr[eb]
                    t1, t2, t3, t4 = tt[eb]
                    K = lambda nm: "%s%d" % (nm, eb)
                    for h in range(4):
                        S.op("scalar", lambda e, h=h: e.activation(out=jk[:, h * 128:(h + 1) * 128],
                                                                   in_=ps[:, h * 128:(h + 1) * 128], func=AF.Square,
                                                                   accum_out=sq[:, h:h + 1]),
                             reads=[pk], writes=[K("junk"), K("ssq4")])
                    S.op("scalar", lambda e: e.activation(out=rs, in_=sq, func=AF.Sqrt, scale=1.0 / 128, bias=eps),
                         reads=[K("ssq4")], writes=[K("rs4")])
                    S.op("vector", lambda e: e.tensor_tensor(out=x_, in0=ps, in1=gg, op=ALU.mult), reads=[pk, ggk], writes=[K("xg")])
                    xv = x_.rearrange("p (x two) -> p x two", two=2)
                    rv = r_.rearrange("p (x two) -> p x two", two=2)
                    x1 = xv[:, :, 0]
                    x2 = xv[:, :, 1]
                    cc = c2[:, 0, :]
                    ss = c2[:, 1, :]
                    S.op("gpsimd", lambda e: e.tensor_tensor(out=t2, in0=x2, in1=ss, op=ALU.mult), reads=[K("xg"), ck], writes=[K("t2")])
                    S.op("gpsimd", lambda e: e.tensor_tensor(out=t3, in0=x1, in1=ss, op=ALU.mult), reads=[K("xg"), ck], writes=[K("t3")])
                    S.op("gpsimd", lambda e: e.tensor_tensor(out=t4, in0=x2, in1=cc, op=ALU.mult), reads=[K("xg"), ck], writes=[K("t4")])
                    S.op("vector", lambda e: e.tensor_tensor(out=t1, in0=x1, in1=cc, op=ALU.mult), reads=[K("xg"), ck], writes=[K("t1")])

                    def stage_b():
                        S.op("vector", lambda e: e.reciprocal(out=rs, in_=rs), reads=[K("rs4")], writes=[K("rs4")])
                        S.op("vector", lambda e: e.tensor_tensor(out=rv[:, :, 0], in0=t1, in1=t2, op=ALU.subtract),
                             reads=[K("t1"), K("t2")], writes=[K("ro")])
                        S.op("vector", lambda e: e.tensor_tensor(out=rv[:, :, 1], in0=t3, in1=t4, op=ALU.add),
                             reads=[K("t3"), K("t4")], writes=[K("ro")])
                        for h in range(4):
                            S.op("vector", lambda e, h=h: e.tensor_scalar(out=q_[:, h * 128:(h + 1) * 128], in0=r_[:, h * 128:(h + 1) * 128],
                                                                         scalar1=rs[:, h:h + 1], scalar2=None, op0=ALU.mult),
                                 reads=[K("ro"), K("rs4")], writes=[K("qr%d" % h)])

                    pb = b.psb(n % 2)
                    pbk = "ps%d" % (n % 2)
                    sg = stage[nb % 2]

                    def stage_c():
                        for h in range(4):
                            S.op("tensor", lambda e, h=h: e.transpose(out=pb[:, h, :], in_=q_[:, h * 128:(h + 1) * 128], identity=identb),
                                 reads=[K("qr%d" % h)], writes=[pbk])
                        S.op("scalar", lambda e: e.copy(out=sg[:, :, ti * 128:(ti + 1) * 128], in_=pb[:, 0:4, :]),
                             reads=[pbk], writes=["stg%d" % (nb % 2)])
                        if ti == TG // 128 - 1:
                            cols = slice(tg * TG, (tg + 1) * TG)
                            if nb < 3:
                                dst = qT_d[nb * 4:(nb + 1) * 4, :, cols]
                            else:
                                dst = kT_d[:, :, cols]
                            S.dma("sync", dst.rearrange("h p t -> p h t"), sg, reads=["stg%d" % (nb % 2)], writes=["qkT_d"])

                    stB[n] = stage_b
                    stC[n] = stage_c
                    run_stage(stB, n - 1)
                    run_stage(stC, n - 2)

                def w_src(nb):
                    return w_in[:, nb * 512:(nb + 1) * 512]

                linear_tok(hT, TG, w_src, 4 * 512, qk_epilogue)
                flush()

                def v_epilogue(nb, ti, ps, pk):
                    vb = vst[ti % 2]
                    S.op("scalar", lambda e: e.copy(out=vb, in_=ps), reads=[pk], writes=["vst%d" % (ti % 2)])
                    S.dma("sync", v_d[tg * TG + ti * 128: tg * TG + (ti + 1) * 128, :], vb, reads=["vst%d" % (ti % 2)], writes=["v_d"])

                linear_tok(hT, TG, lambda nb: w_in[:, 2048:2560], 512, v_epilogue)

                fst = [b.alloc("fst0", [512], BF16), b.alloc("fst1", [512], BF16)]
                fctr = [0]

                def f_epilogue(nt, tb, ps, pk):
                    i = fctr[0] % 2
                    fctr[0] += 1
                    S.op("vector", lambda e: e.tensor_copy(out=fst[i], in_=ps), reads=[pk], writes=["fst%d" % i])
                    S.dma("sync", fT_d[nt, :, tg * TG + tb * 512: tg * TG + (tb + 1) * 512], fst[i],
                          reads=["fst%d" % i], writes=["fT_d"])

                linear_feat(hT, TG, lambda nb: w_in[:, 2560:3072], 512, f_epilogue)

        def phase_attention():
            b.phase()
            kT = b.alloc("kT", [4, S_], BF16)
            V = b.alloc("V", [16, 512], BF16)
            qT = [b.alloc("qT0", [S_], BF16), b.alloc("qT1", [S_], BF16)]
            eT = [b.alloc("eT%d" % i, [512], BF16) for i in range(4)]
            rsum = [b.alloc("rsum0", [512], F32), b.alloc("rsum1", [512], F32)]
            ost = [b.alloc("ost0", [512], BF16), b.alloc("ost1", [512], BF16)]
            S.dma("sync", kT, kT_d.rearrange("h p t -> p h t"), reads=["qkT_d"], writes=["kT"])
            S.dma("sync", V, v_d.rearrange("(j p) d -> p j d", p=128), reads=["v_d"], writes=["V"])
            scale = 128 ** -0.5
            items = [(h, qb, st_) for h in range(12) for qb in range(4) for st_ in range(16)]

            def load_q(h):
                S.dma("sync", qT[h % 2], qT_d[h], reads=["qkT_d"], writes=["qT%d" % (h % 2)])

            def emit_sc(i):
                h, qb, st_ = items[i]
                g = h // 3
                q = qT[h % 2]
                scb = 2 + (i % 2)
                S.op("tensor", lambda e: e.matmul(
                    b.ps(scb), lhsT=kT[:, g, st_ * 128:(st_ + 1) * 128], rhs=q[:, qb * 512:(qb + 1) * 512],
                    start=True, stop=True), reads=["kT", "qT%d" % (h % 2)], writes=["ps%d" % scb])

            load_q(0)
            emit_sc(0)
            for i, (h, qb, st_) in enumerate(items):
                g = h // 3
                blk = i // 16
                ob = 4 + (blk % 2)
                sb_ = 6 + (blk % 2)
                scb = 2 + (i % 2)
                ei = i % 4
                if qb == 0 and st_ == 0 and h + 1 < 12:
                    load_q(h + 1)
                if i + 1 < len(items):
                    emit_sc(i + 1)
                S.op("scalar", lambda e, scb=scb, ei=ei: e.activation(out=eT[ei], in_=b.ps(scb), func=AF.Exp, scale=scale),
                     reads=["ps%d" % scb], writes=["eT%d" % ei])
                S.op("tensor", lambda e, st_=st_, ob=ob, ei=ei, g=g: e.matmul(
                    b.ps(ob), lhsT=V[:, st_, g * 128:(g + 1) * 128], rhs=eT[ei], start=(st_ == 0), stop=(st_ == 15)),
                     reads=["V", "eT%d" % ei], writes=["ps%d" % ob])
                S.op("tensor", lambda e, st_=st_, sb_=sb_, ei=ei: e.matmul(
                    b.ps(sb_), lhsT=onesb, rhs=eT[ei], start=(st_ == 0), stop=(st_ == 15)),
                     reads=["eT%d" % ei], writes=["ps%d" % sb_])
                if st_ == 15:
                    rs = rsum[blk % 2]
                    rk = "rsum%d" % (blk % 2)
                    o = ost[blk % 2]
                    okk = "ost%d" % (blk % 2)
                    S.op("vector", lambda e, sb_=sb_, rs=rs: e.reciprocal(out=rs, in_=b.ps(sb_)), reads=["ps%d" % sb_], writes=[rk])
                    S.op("vector", lambda e, ob=ob, o=o, rs=rs: e.tensor_tensor(out=o, in0=b.ps(ob), in1=rs, op=ALU.mult),
                         reads=["ps%d" % ob, rk], writes=[okk])
                    S.dma("sync", mixT_d[h, :, qb * 512:(qb + 1) * 512], o, reads=[okk], writes=["mixT_d"])

        def phase_fourier():
            b.phase()
            global wring
            wring = [b.alloc("w%d" % i, [8192], BF16) for i in range(4)]
            dC = b.alloc("dC", [2, 128], F32)
            fw = b.alloc("fw", [4, 128], F32)
            AB = b.alloc("AB", [4, 256], BF16)
            fT = b.alloc("fT", [4, S_], BF16)
            P12 = b.alloc("P12", [16, 4 * 256], BF16)
            ost = [b.alloc("ost0", [512], BF16), b.alloc("ost1", [512], BF16)]
            S.dma("sync", dC, c_dftC.rearrange("a p c -> p a c"), writes=["dC"])
            S.dma("sync", fw, fourier_w.rearrange("g p c -> p g c"), writes=["fw"])
            S.dma("sync", fT, fT_d.rearrange("g p t -> p g t"), reads=["fT_d"], writes=["fT"])
            for g in range(4):
                for a in range(2):
                    S.op("tensor", lambda e, g=g, a=a: e.matmul(b.ps(2)[:, a * 128:(a + 1) * 128], lhsT=dC[:, a, :], rhs=fw[:, g, :],
                                                                start=True, stop=True), reads=["dC", "fw"], writes=["ps2"])
                S.op("vector", lambda e, g=g: e.tensor_copy(out=AB[:, g, :], in_=b.ps(2)[:, 0:256]), reads=["ps2"], writes=["AB"])
            for j in range(16):
                for half in range(2):
                    bank = 3 + half
                    for gg in range(2):
                        g = half * 2 + gg
                        S.op("tensor", lambda e, g=g, gg=gg, j=j, bank=bank: e.matmul(
                            b.ps(bank)[:, gg * 256:(gg + 1) * 256], lhsT=fT[:, g, j * 128:(j + 1) * 128], rhs=AB[:, g, :],
                            start=True, stop=True), reads=["fT", "AB"], writes=["ps%d" % bank])
                    eng = "vector" if half == 0 else "scalar"
                    dst = P12[:, j, half * 512:(half + 1) * 512]
                    if eng == "vector":
                        S.op("vector", lambda e, dst=dst, bank=bank: e.tensor_copy(out=dst, in_=b.ps(bank)), reads=["ps%d" % bank], writes=["P12"])
                    else:
                        S.op("scalar", lambda e, dst=dst, bank=bank: e.copy(out=dst, in_=b.ps(bank)), reads=["ps%d" % bank], writes=["P12"])
            oc = 0
            for tb in range(4):
                cw, ck = wpiece(c_dftS[0][:, tb * 512:(tb + 1) * 512].rearrange("(k p) n -> p k n", p=128))
                sw, sk = wpiece(c_dftS[1][:, tb * 512:(tb + 1) * 512].rearrange("(k p) n -> p k n", p=128))
                for g in range(4):
                    bank = 5 + (oc % 2)
                    for j in range(16):
                        S.op("tensor", lambda e, g=g, j=j, bank=bank, cw=cw: e.matmul(
                            b.ps(bank), lhsT=P12[:, j, g * 256:g * 256 + 128], rhs=cw[:, j, :], start=(j == 0), stop=False),
                             reads=["P12", ck], writes=["ps%d" % bank])
                    for j in range(16):
                        S.op("tensor", lambda e, g=g, j=j, bank=bank, sw=sw: e.matmul(
                            b.ps(bank), lhsT=P12[:, j, g * 256 + 128:g * 256 + 256], rhs=sw[:, j, :], start=False, stop=(j == 15)),
                             reads=["P12", sk], writes=["ps%d" % bank])
                    o = ost[oc % 2]
                    okk = "ost%d" % (oc % 2)
                    oc += 1
                    S.op("vector", lambda e, o=o, bank=bank: e.tensor_copy(out=o, in_=b.ps(bank)), reads=["ps%d" % bank], writes=[okk])
                    S.dma("sync", mixT_d[12 + g, :, tb * 512:(tb + 1) * 512], o, reads=[okk], writes=["mixT_d"])

        def phase_outproj(W, X_src, X_dst):
            for tg in range(NTG):
                b.phase()
                global wring
                wring = [b.alloc("w%d" % i, [8192], BF16) for i in range(4)]
                hT = b.alloc("hT", [16, TG], BF16)
                S.dma("sync", hT, mixT_d[:, :, tg * TG:(tg + 1) * TG].rearrange("k p t -> p k t"), reads=["mixT_d"], writes=["hT"])
                ep = make_resid_epilogue(X_src, X_dst, tg * TG, [(nb, ti) for nb in range(4) for ti in range(TG // 128)])
                linear_tok(hT, TG, lambda nb: W[:, nb * 512:(nb + 1) * 512], D_, ep)

        def phase_cross(layer, X_src, X_dst):
            b.phase()
            global wring
            Aq = b.alloc("Aq", [16, 4 * 256], BF16)
            VW = b.alloc("VW", [2, 4 * D_], BF16)
            mark_main = b.off
            wring = [b.alloc("w%d" % i, [8192], BF16) for i in range(3)]
            kTm = b.alloc("kTm", [16, 256], BF16)
            vTm = b.alloc("vTm", [16, 256], BF16)
            memT = b.alloc("hT", [16, 256], BF16)
            WqT = [b.alloc("WqT0", [4, D_], BF16), b.alloc("WqT1", [4, D_], BF16)]
            load_gain(mem_norm[layer])
            norm_loop(lambda ti: mem_in[ti * 128:(ti + 1) * 128, :], 2, memT)
            cnt = 0
            for Wm, dstT, dkey in ((cw_k, kTm, "kTm"), (cw_v, vTm, "vTm")):
                nxt = wpiece(Wm[layer][:, 0:512].rearrange("(k p) n -> p k n", p=128))
                for nb in range(4):
                    wt, wk = nxt
                    if nb + 1 < 4:
                        nxt = wpiece(Wm[layer][:, (nb + 1) * 512:(nb + 2) * 512].rearrange("(k p) n -> p k n", p=128))
                    for nt in range(4):
                        bank = 2 + (cnt % 4)
                        cnt += 1
                        for k in range(16):
                            S.op("tensor", lambda e, k=k, nt=nt, bank=bank, wt=wt: e.matmul(
                                b.ps(bank)[:, 0:256], lhsT=wt[:, k, nt * 128:(nt + 1) * 128], rhs=memT[:, k, :],
                                start=(k == 0), stop=(k == 15)), reads=["hT", wk], writes=["ps%d" % bank])
                        dst = dstT[:, nb * 4 + nt, :]
                        if cnt % 2 == 0:
                            S.op("vector", lambda e, dst=dst, bank=bank: e.tensor_copy(out=dst, in_=b.ps(bank)[:, 0:256]), reads=["ps%d" % bank], writes=[dkey])
                        else:
                            S.op("scalar", lambda e, dst=dst, bank=bank: e.copy(out=dst, in_=b.ps(bank)[:, 0:256]), reads=["ps%d" % bank], writes=[dkey])
            nxt = wpiece(cw_q[layer][:, 0:512].rearrange("(k p) n -> p k n", p=128))
            for h in range(4):
                wt, wk = nxt
                if h + 1 < 4:
                    nxt = wpiece(cw_q[layer][:, (h + 1) * 512:(h + 2) * 512].rearrange("(k p) n -> p k n", p=128))
                WT = WqT[h % 2]
                wtk = "WqT%d" % (h % 2)
                tcn = 0
                for c in range(4):
                    for k0 in (0, 8):
                        half = tcn % 2
                        tcn += 1
                        pb = b.psb(half)
                        for k in range(8):
                            S.op("tensor", lambda e, k=k, k0=k0, c=c, pb=pb, wt=wt: e.transpose(
                                out=pb[:, k, :], in_=wt[:, k0 + k, c * 128:(c + 1) * 128], identity=identb),
                                 reads=[wk], writes=["ps%d" % half])
                        dst = WT[:, c, k0 * 128:(k0 + 8) * 128].rearrange("p (a b) -> p a b", a=8)
                        if half == 0:
                            S.op("vector", lambda e, dst=dst, pb=pb: e.tensor_copy(out=dst, in_=pb), reads=["ps0"], writes=[wtk])
                        else:
                            S.op("scalar", lambda e, dst=dst, pb=pb: e.copy(out=dst, in_=pb), reads=["ps1"], writes=[wtk])
                for k in range(16):
                    bank = 2 + (cnt % 4)
                    cnt += 1
                    for c in range(4):
                        S.op("tensor", lambda e, k=k, c=c, h=h, bank=bank, WT=WT: e.matmul(
                            b.ps(bank)[:, 0:256], lhsT=WT[:, c, k * 128:(k + 1) * 128], rhs=kTm[:, h * 4 + c, :],
                            start=(c == 0), stop=(c == 3)), reads=[wtk, "kTm"], writes=["ps%d" % bank])
                    dst = Aq[:, k, h * 256:(h + 1) * 256]
                    if k % 2 == 0:
                        S.op("vector", lambda e, dst=dst, bank=bank: e.tensor_copy(out=dst, in_=b.ps(bank)[:, 0:256]), reads=["ps%d" % bank], writes=["Aq"])
                    else:
                        S.op("scalar", lambda e, dst=dst, bank=bank: e.copy(out=dst, in_=b.ps(bank)[:, 0:256]), reads=["ps%d" % bank], writes=["Aq"])
            nxt = wpiece(cw_o[layer][:, 0:512].rearrange("(k p) n -> p k n", p=128))
            for nb in range(4):
                wt, wk = nxt
                if nb + 1 < 4:
                    nxt = wpiece(cw_o[layer][:, (nb + 1) * 512:(nb + 2) * 512].rearrange("(k p) n -> p k n", p=128))
                for h in range(4):
                    for mt in range(2):
                        bank = 2 + (cnt % 4)
                        cnt += 1
                        for c in range(4):
                            S.op("tensor", lambda e, c=c, h=h, mt=mt, bank=bank, wt=wt: e.matmul(
                                b.ps(bank), lhsT=vTm[:, h * 4 + c, mt * 128:(mt + 1) * 128], rhs=wt[:, h * 4 + c, :],
                                start=(c == 0), stop=(c == 3)), reads=["vTm", wk], writes=["ps%d" % bank])
                        dst = VW[:, mt, h * D_ + nb * 512: h * D_ + (nb + 1) * 512]
                        if cnt % 2 == 0:
                            S.op("vector", lambda e, dst=dst, bank=bank: e.tensor_copy(out=dst, in_=b.ps(bank)), reads=["ps%d" % bank], writes=["VW"])
                        else:
                            S.op("scalar", lambda e, dst=dst, bank=bank: e.copy(out=dst, in_=b.ps(bank)), reads=["ps%d" % bank], writes=["VW"])
            scale = 512 ** -0.5
            for tg in range(NTG):
                S.barrier()
                b.off = mark_main
                hT = b.alloc("hTq", [16, TG], BF16)
                eTs = [b.alloc("eTs0", [8, 512], BF16), b.alloc("eTs1", [8, 512], BF16)]
                rsum2 = [b.alloc("rsum_a", [512], F32), b.alloc("rsum_b", [512], F32)]
                load_gain(cross_norm[layer])
                norm_loop(lambda ti: X_src[tg * TG + ti * 128: tg * TG + (ti + 1) * 128, :], TG // 128, hT)
                NTB = TG // 512
                rxs = [b.alloc("rxw%d" % i, [D_], F32) for i in range(3)]

                def load_res(n):
                    rows = slice(tg * TG + n * 128, tg * TG + (n + 1) * 128)
                    S.dma("sync", rxs[n % 3], X_src[rows, :], writes=["rxw%d" % (n % 3)])
                scn = [0]
                rcn = [0]

                def sums_norm(tb, h):
                    et = eTs[tb % 2]
                    ek = "eTs%d_%d" % (tb % 2, h)
                    rs = rsum2[rcn[0] % 2]
                    rk = "rsum%d" % (rcn[0] % 2)
                    rcn[0] += 1
                    for mt in range(2):
                        S.op("tensor", lambda e, mt=mt: e.matmul(b.ps(3), lhsT=onesb, rhs=et[:, h * 2 + mt, :], start=(mt == 0), stop=(mt == 1)),
                             reads=[ek], writes=["ps3"])
                    S.op("vector", lambda e: e.reciprocal(out=rs, in_=b.ps(3)), reads=["ps3"], writes=[rk])
                    for mt in range(2):
                        S.op("vector", lambda e, mt=mt: e.tensor_tensor(out=et[:, h * 2 + mt, :], in0=et[:, h * 2 + mt, :], in1=rs, op=ALU.mult),
                             reads=[ek, rk], writes=[ek])

                def stage_scores(tb):
                    et = eTs[tb % 2]
                    for h in range(4):
                        ek = "eTs%d_%d" % (tb % 2, h)
                        for mt in range(2):
                            bank = scn[0] % 3
                            scn[0] += 1
                            for k in range(16):
                                S.op("tensor", lambda e, k=k, h=h, mt=mt, bank=bank: e.matmul(
                                    b.ps(bank), lhsT=Aq[:, k, h * 256 + mt * 128: h * 256 + (mt + 1) * 128],
                                    rhs=hT[:, k, tb * 512:(tb + 1) * 512], start=(k == 0), stop=(k == 15)),
                                     reads=["Aq", "hT"], writes=["ps%d" % bank])
                            S.op("scalar", lambda e, h=h, mt=mt, bank=bank: e.activation(out=et[:, h * 2 + mt, :], in_=b.ps(bank), func=AF.Exp, scale=scale),
                                 reads=["ps%d" % bank], writes=[ek])
                        if h >= 1:
                            sums_norm(tb, h - 1)
                    sums_norm(tb, 3)

                ocn = [0]

                def stage_out(tb):
                    et = eTs[tb % 2]
                    for t4 in range(4):
                        n = tb * 4 + t4
                        if n == 0:
                            load_res(0)
                        if n + 1 < TG // 128:
                            load_res(n + 1)
                        r = rxs[n % 3]
                        rk = "rxw%d" % (n % 3)
                        for nb in range(4):
                            bank = 4 + (ocn[0] % 4)
                            ocn[0] += 1
                            i = 0
                            for h in range(4):
                                for mt in range(2):
                                    S.op("tensor", lambda e, h=h, mt=mt, t4=t4, nb=nb, bank=bank, i=i: e.matmul(
                                        b.ps(bank), lhsT=et[:, h * 2 + mt, t4 * 128:(t4 + 1) * 128],
                                        rhs=VW[:, mt, h * D_ + nb * 512: h * D_ + (nb + 1) * 512], start=(i == 0), stop=(i == 7)),
                                         reads=["eTs%d_%d" % (tb % 2, h), "VW"], writes=["ps%d" % bank])
                                    i += 1
                            S.op("vector", lambda e, nb=nb, bank=bank, r=r: e.tensor_tensor(out=r[:, nb * 512:(nb + 1) * 512], in0=b.ps(bank),
                                                                                         in1=r[:, nb * 512:(nb + 1) * 512], op=ALU.add),
                                 reads=["ps%d" % bank, rk], writes=[rk])
                        S.dma("sync", X_dst[tg * TG + n * 128: tg * TG + (n + 1) * 128, :], r, reads=[rk], writes=["Xdst"])

                stage_scores(0)
                for tb in range(NTB):
                    if tb + 1 < NTB:
                        stage_scores(tb + 1)
                    stage_out(tb)

        def phase_moe(layer, X):
            b.phase()
            global wring
            wring = [b.alloc("w%d" % i, [8192], BF16) for i in range(6)]
            for i_ in range(6):
                src_, kd_ = ((ew_gate if i_ in (0, 2) else ew_up)[layer, 0][:, (i_ // 2) * 512:(i_ // 2 + 1) * 512].rearrange("(k p) n -> p k n", p=128), 16) \
                    if i_ < 4 else (ew_down[layer, 0][:, (i_ - 4) * 1024:(i_ - 3) * 1024].rearrange("(k p) n -> p k n", p=128), 8)
                S.dma("gpsimd", wring[i_].rearrange("p (k n) -> p k n", k=kd_), src_, writes=["w%d" % i_])
            logT = b.alloc("logT", [S_], F32)
            mark_inner = b.off
            xts = [b.alloc("xt0", [D_], F32), b.alloc("xt1", [D_], F32)]
            xnb = [b.alloc("xn0", [D_], BF16), b.alloc("xn1", [D_], BF16)]
            xnf2 = [b.alloc("xnf0", [D_], F32), b.alloc("xnf1", [D_], F32)]
            junk = b.alloc("njunk", [D_], BF16)
            ssq2 = [b.alloc("ssq0", [1], F32), b.alloc("ssq1", [1], F32)]
            rstd2 = [b.alloc("rstd0", [1], F32), b.alloc("rstd1", [1], F32)]
            hTf = b.alloc("hTf", [16, 128], F32)
            wrA = b.alloc("wrA", [16, 48], F32)
            wrB = b.alloc("wrB", [16, 48], F32)
            load_gain(ffn_norm[layer])
            S.op("vector", lambda e: e.memset(wrA, 0.0), writes=["wr"])
            S.op("vector", lambda e: e.memset(wrB, 0.0), writes=["wr"])
            S.dma("sync", wrA[:, :, 0:16], router_w[layer].rearrange("(k p) e -> p k e", p=128), writes=["wr"])
            S.dma("sync", wrB[:, :, 32:48], router_w[layer].rearrange("(k p) e -> p k e", p=128), writes=["wr"])
            S.op("vector", lambda e: e.memset(logT[0:48, 0:1024], -30000.0), writes=["logT"])

            def m_load(ti):
                S.dma("sync", xts[ti % 2], X[ti * 128:(ti + 1) * 128, :], writes=["xt%d" % (ti % 2)])

            def m_early(ti):
                i = ti % 2
                rows = slice(ti * 128, (ti + 1) * 128)
                xt, xn, xnf, ssq, rstd = xts[i], xnb[i], xnf2[i], ssq2[i], rstd2[i]
                S.op("scalar", lambda e: e.activation(out=junk, in_=xt, func=AF.Square, accum_out=ssq),
                     reads=["xt%d" % i], writes=["njunk", "ssq%d" % i])
                S.op("scalar", lambda e: e.activation(out=rstd, in_=ssq, func=AF.Sqrt, scale=1.0 / D_, bias=eps),
                     reads=["ssq%d" % i], writes=["rstd%d" % i])
                S.op("vector", lambda e: e.reciprocal(out=rstd, in_=rstd), reads=["rstd%d" % i], writes=["rstd%d" % i])
                S.op("vector", lambda e: e.scalar_tensor_tensor(out=xnf, in0=xt, scalar=rstd, in1=gb, op0=ALU.mult, op1=ALU.mult),
                     reads=["xt%d" % i, "rstd%d" % i, "gb"], writes=["xnf%d" % i])
                S.op("gpsimd", lambda e: e.tensor_copy(out=xn, in_=xnf), reads=["xnf%d" % i], writes=["xn%d" % i])
                S.dma("sync", hmoe_d[rows, :], xn, reads=["xn%d" % i], writes=["hmoe_d"])

            def m_late(ti):
                i = ti % 2
                xnf = xnf2[i]
                for q4 in range(4):
                    bank = 2 + q4
                    for k in range(4):
                        kk = q4 * 4 + k
                        S.op("tensor", lambda e, k=k, kk=kk, bank=bank: e.transpose(
                            out=b.ps(bank)[:, k * 128:(k + 1) * 128], in_=xnf[:, kk * 128:(kk + 1) * 128], identity=identf),
                             reads=["xnf%d" % i], writes=["ps%d" % bank])
                    dst = hTf[:, q4 * 4:(q4 + 1) * 4, :]
                    if q4 % 2 == 0:
                        S.op("vector", lambda e, dst=dst, bank=bank: e.tensor_copy(
                            out=dst, in_=b.ps(bank).rearrange("p (a b) -> p a b", a=4)), reads=["ps%d" % bank], writes=["hTf%d" % q4])
                    else:
                        S.op("scalar", lambda e, dst=dst, bank=bank: e.copy(
                            out=dst, in_=b.ps(bank).rearrange("p (a b) -> p a b", a=4)), reads=["ps%d" % bank], writes=["hTf%d" % q4])
                wsel = wrA if ti < 8 else wrB
                for k in range(16):
                    S.op("tensor", lambda e, k=k, wsel=wsel: e.matmul(b.ps(6)[0:48, 0:128], lhsT=wsel[:, k, :], rhs=hTf[:, k, :],
                                                                     start=(k == 0), stop=(k == 15)), reads=["wr", "hTf%d" % (k // 4)], writes=["ps6"])
                p0 = 0 if ti < 8 else 32
                tcol = (ti % 8) * 128
                S.op("vector", lambda e, p0=p0, tcol=tcol: e.tensor_copy(out=logT[p0:p0 + 16, tcol:tcol + 128], in_=b.ps(6)[p0:p0 + 16, 0:128]),
                     reads=["ps6"], writes=["logT"])

            m_load(0)
            for ti in range(S_ // 128 + 1):
                if ti + 1 < S_ // 128:
                    m_load(ti + 1)
                if ti < S_ // 128:
                    m_early(ti)
                if ti >= 1:
                    m_late(ti - 1)
            S.barrier()
            b.off = mark_inner
            HS = S_ // 2
            aff = b.alloc("aff", [HS], F32)
            work = b.alloc("work", [HS], F32)
            vals = b.alloc("vals", [CAP], F32)
            idxu = b.alloc("idxu", [CAP], U32)
            idxf = b.alloc("idxf", [CAP], F32)
            ob = b.alloc("ob", [48], F32)
            offs = b.alloc("offs", [1], F32)
            Jm = b.alloc("Jm", [128], F32)
            VT = b.alloc("VT", [2, 48], F32)
            IT = b.alloc("IT", [2, 48], F32)
            BrV = b.alloc("BrV", [2, NE], F32)
            BrI = b.alloc("BrI", [2, NE], F32)
            msk = b.alloc("msk", [2, NE], F32)
            dif = b.alloc("dif", [2, NE], F32)
            gateT = b.alloc("gateT", [2, NE], F32)
            idxT = b.alloc("idxT", [2, NE], I32)
            S.op("gpsimd", lambda e: e.memset(ob[0:48, :], 0.0), writes=["ob"])
            S.op("gpsimd", lambda e: e.memset(ob[0:32, 0:32], 1.0), writes=["ob"])
            S.op("gpsimd", lambda e: e.memset(ob[32:48, 32:48], 1.0), writes=["ob"])
            S.op("gpsimd", lambda e: e.memset(offs[0:48, :], 0.0), writes=["offs"])
            S.op("gpsimd", lambda e: e.memset(offs[32:48, :], float(HS)), writes=["offs"])
            S.op("gpsimd", lambda e: e.memset(Jm, 0.0), writes=["Jm"])
            S.op("gpsimd", lambda e: e.affine_select(out=Jm, in_=Jm, pattern=[[1, 128]], compare_op=ALU.not_equal, fill=1.0,
                                                     base=-127, channel_multiplier=1), reads=["Jm"], writes=["Jm"])
            S.op("scalar", lambda e: e.activation(out=aff[0:48, :], in_=logT[0:48, 0:HS], func=AF.Exp), reads=["logT"], writes=["aff"])
            for tb in range(2):
                S.op("tensor", lambda e, tb=tb: e.matmul(b.ps(2)[0:48, :], lhsT=ob[0:48, 0:48], rhs=aff[0:48, tb * 512:(tb + 1) * 512],
                                                         start=True, stop=True), reads=["aff", "ob"], writes=["ps2"])
                S.op("vector", lambda e, tb=tb: e.reciprocal(out=work[0:48, tb * 512:(tb + 1) * 512], in_=b.ps(2)[0:48, :]),
                     reads=["ps2"], writes=["work"])
            S.op("vector", lambda e: e.tensor_tensor(out=aff[0:48, :], in0=aff[0:48, :], in1=work[0:48, :], op=ALU.mult),
                 reads=["aff", "work"], writes=["aff"])
            S.op("vector", lambda e: e.tensor_copy(out=work[0:48, :], in_=aff[0:48, :]), reads=["aff"], writes=["work"])
            for r in range(CAP // 8):
                S.op("vector", lambda e, r=r: e.max(out=vals[0:48, r * 8:(r + 1) * 8], in_=work[0:48, :]), reads=["work"], writes=["vals"])
                S.op("vector", lambda e, r=r: e.max_index(out=idxu[0:48, r * 8:(r + 1) * 8], in_max=vals[0:48, r * 8:(r + 1) * 8],
                                                          in_values=work[0:48, :]), reads=["work", "vals"], writes=["idxu"])
                S.op("vector", lambda e, r=r: e.match_replace(out=work[0:48, :], in_to_replace=vals[0:48, r * 8:(r + 1) * 8],
                                                              in_values=work[0:48, :], imm_value=-1.0),
                     reads=["work", "vals", "idxu"], writes=["work"])
            S.op("vector", lambda e: e.tensor_copy(out=idxf[0:48, :], in_=idxu[0:48, :]), reads=["idxu"], writes=["idxf"])
            S.op("vector", lambda e: e.tensor_scalar(out=idxf[0:48, :], in0=idxf[0:48, :], scalar1=0.0, scalar2=float(HS - 1),
                                                     op0=ALU.max, op1=ALU.min), reads=["idxf"], writes=["idxf"])
            S.op("vector", lambda e: e.tensor_scalar(out=idxf[0:48, :], in0=idxf[0:48, :], scalar1=offs[0:48, :], scalar2=None,
                                                     op0=ALU.add), reads=["idxf", "offs"], writes=["idxf"])
            for cc in range(2):
                S.op("tensor", lambda e, cc=cc: e.transpose(out=b.ps(3)[:, 0:48], in_=vals[0:48, cc * 128:(cc + 1) * 128],
                                                            identity=identf[0:48, 0:48]), reads=["vals"], writes=["ps3"])
                S.op("vector", lambda e, cc=cc: e.tensor_copy(out=VT[:, cc, :], in_=b.ps(3)[:, 0:48]), reads=["ps3"], writes=["VT"])
                S.op("tensor", lambda e, cc=cc: e.transpose(out=b.ps(4)[:, 0:48], in_=idxf[0:48, cc * 128:(cc + 1) * 128],
                                                            identity=identf[0:48, 0:48]), reads=["idxf"], writes=["ps4"])
                S.op("vector", lambda e, cc=cc: e.tensor_copy(out=IT[:, cc, :], in_=b.ps(4)[:, 0:48]), reads=["ps4"], writes=["IT"])
            for cc in range(2):
                S.op("tensor", lambda e, cc=cc: e.matmul(b.ps(3)[:, 0:16], lhsT=Jm, rhs=VT[:, 1 - cc, 32:48], start=True, stop=True),
                     reads=["Jm", "VT"], writes=["ps3"])
                S.op("vector", lambda e, cc=cc: e.tensor_copy(out=BrV[:, cc, :], in_=b.ps(3)[:, 0:16]), reads=["ps3"], writes=["BrV"])
                S.op("tensor", lambda e, cc=cc: e.matmul(b.ps(4)[:, 0:16], lhsT=Jm, rhs=IT[:, 1 - cc, 32:48], start=True, stop=True),
                     reads=["Jm", "IT"], writes=["ps4"])
                S.op("vector", lambda e, cc=cc: e.tensor_copy(out=BrI[:, cc, :], in_=b.ps(4)[:, 0:16]), reads=["ps4"], writes=["BrI"])
            S.op("vector", lambda e: e.tensor_tensor(out=gateT, in0=VT[:, :, 0:16], in1=BrV, op=ALU.max), reads=["VT", "BrV"], writes=["gateT"])
            S.op("vector", lambda e: e.tensor_tensor(out=msk, in0=VT[:, :, 0:16], in1=BrV, op=ALU.is_gt), reads=["VT", "BrV"], writes=["msk"])
            S.op("vector", lambda e: e.tensor_tensor(out=dif, in0=IT[:, :, 0:16], in1=BrI, op=ALU.subtract), reads=["IT", "BrI"], writes=["dif"])
            S.op("vector", lambda e: e.tensor_tensor(out=dif, in0=dif, in1=msk, op=ALU.mult), reads=["dif", "msk"], writes=["dif"])
            S.op("vector", lambda e: e.tensor_tensor(out=dif, in0=dif, in1=BrI, op=ALU.add), reads=["dif", "BrI"], writes=["dif"])
            S.op("vector", lambda e: e.tensor_copy(out=idxT, in_=dif), reads=["dif"], writes=["idxT"])
            if stage == 30:
                return
            xs = [b.alloc("xs%d" % i, [D_], BF16) for i in range(4)]
            xsT = [b.alloc("xsT0", [16, CAP], BF16), b.alloc("xsT1", [16, CAP], BF16)]
            gT = b.alloc("gT", [8, CAP], BF16)
            sa = [b.alloc("sa0", [CAP], F32), b.alloc("sa1", [CAP], F32)]
            ysb = [b.alloc("ysb0", [D_], F32), b.alloc("ysb1", [D_], F32)]

            def piece_src(ex, i):
                if i in (0, 2):
                    fh = i // 2
                    return ew_gate[layer, ex][:, fh * 512:(fh + 1) * 512].rearrange("(k p) n -> p k n", p=128), 16
                if i in (1, 3):
                    fh = i // 2
                    return ew_up[layer, ex][:, fh * 512:(fh + 1) * 512].rearrange("(k p) n -> p k n", p=128), 16
                nh = i - 4
                return ew_down[layer, ex][:, nh * 1024:(nh + 1) * 1024].rearrange("(k p) n -> p k n", p=128), 8

            def load_piece(ex, i):
                src, kd = piece_src(ex, i)
                S.dma("gpsimd", wring[i].rearrange("p (k n) -> p k n", k=kd), src, writes=["w%d" % i])

            def piece(i, kd):
                return wring[i].rearrange("p (k n) -> p k n", k=kd), "w%d" % i

            def gather(ex):
                for cc in range(2):
                    xi = (2 * ex + cc) % 4
                    S.op("gpsimd", lambda e, xi=xi, cc=cc, ex=ex: e.indirect_dma_start(
                        out=xs[xi], out_offset=None, in_=hmoe_d[:, :],
                        in_offset=bass.IndirectOffsetOnAxis(ap=idxT[:, cc, ex:ex + 1], axis=0)),
                         reads=["hmoe_d", "idxT"], writes=["xs%d" % xi], dma=True)

            def transposes(ex):
                xT = xsT[ex % 2]
                xTk = "xsT%d" % (ex % 2)
                for cc in range(2):
                    xi = (2 * ex + cc) % 4
                    xsb = xs[xi]
                    xk = "xs%d" % xi
                    for half in range(2):
                        pb = b.psb(half)
                        for k in range(8):
                            kk = half * 8 + k
                            S.op("tensor", lambda e, k=k, kk=kk, pb=pb, xsb=xsb: e.transpose(
                                out=pb[:, k, :], in_=xsb[:, kk * 128:(kk + 1) * 128], identity=identb),
                                 reads=[xk], writes=["ps%d" % half])
                        dst = xT[:, half * 8:(half + 1) * 8, cc * 128:(cc + 1) * 128]
                        if half == 0:
                            S.op("vector", lambda e, dst=dst, pb=pb: e.tensor_copy(out=dst, in_=pb), reads=["ps0"], writes=[xTk])
                        else:
                            S.op("scalar", lambda e, dst=dst, pb=pb: e.copy(out=dst, in_=pb), reads=["ps1"], writes=[xTk])

            hcn = [0]

            def hidden(ex, fh):
                xT = xsT[ex % 2]
                xTk = "xsT%d" % (ex % 2)
                wg, wgk = piece(2 * fh, 16)
                wu, wuk = piece(2 * fh + 1, 16)
                for ft in range(4):
                    hc = hcn[0]
                    hcn[0] += 1
                    bg = 2 + 2 * (hc % 2)
                    bu = bg + 1
                    si = hc % 2
                    for k in range(16):
                        S.op("tensor", lambda e, k=k, ft=ft, bg=bg, wg=wg, xT=xT: e.matmul(
                            b.ps(bg)[:, 0:256], lhsT=wg[:, k, ft * 128:(ft + 1) * 128], rhs=xT[:, k, :],
                            start=(k == 0), stop=(k == 15)), reads=[wgk, xTk], writes=["ps%d" % bg])
                    for k in range(16):
                        S.op("tensor", lambda e, k=k, ft=ft, bu=bu, wu=wu, xT=xT: e.matmul(
                            b.ps(bu)[:, 0:256], lhsT=wu[:, k, ft * 128:(ft + 1) * 128], rhs=xT[:, k, :],
                            start=(k == 0), stop=(k == 15)), reads=[wuk, xTk], writes=["ps%d" % bu])
                    S.op("scalar", lambda e, bg=bg, si=si: e.activation(out=sa[si], in_=b.ps(bg)[:, 0:256], func=AF.Silu),
                         reads=["ps%d" % bg], writes=["sa%d" % si])
                    S.op("vector", lambda e, bu=bu, si=si, fh=fh, ft=ft: e.tensor_tensor(
                        out=gT[:, fh * 4 + ft, :], in0=b.ps(bu)[:, 0:256], in1=sa[si], op=ALU.mult),
                         reads=["ps%d" % bu, "sa%d" % si], writes=["gT"])

            dcn = [0]

            def down(ex, nh):
                wdv, wdk = piece(4 + nh, 8)
                for cc in range(2):
                    for nbk in range(2):
                        bank = 6 + (dcn[0] % 2)
                        dcn[0] += 1
                        for ft in range(8):
                            S.op("tensor", lambda e, ft=ft, cc=cc, nbk=nbk, bank=bank, wdv=wdv: e.matmul(
                                b.ps(bank), lhsT=gT[:, ft, cc * 128:(cc + 1) * 128], rhs=wdv[:, ft, nbk * 512:(nbk + 1) * 512],
                                start=(ft == 0), stop=(ft == 7)), reads=["gT", wdk], writes=["ps%d" % bank])
                        dst = ysb[cc][:, nh * 1024 + nbk * 512: nh * 1024 + (nbk + 1) * 512]
                        if nbk == 0:
                            S.op("vector", lambda e, dst=dst, bank=bank, cc=cc, ex=ex: e.tensor_scalar(
                                out=dst, in0=b.ps(bank), scalar1=gateT[:, cc, ex:ex + 1], scalar2=None, op0=ALU.mult),
                                 reads=["ps%d" % bank, "gateT"], writes=["ysb%d" % cc])
                        else:
                            S.op("scalar", lambda e, dst=dst, bank=bank, cc=cc, ex=ex: e.activation(
                                out=dst, in_=b.ps(bank), func=AF.Copy, scale=gateT[:, cc, ex:ex + 1]),
                                 reads=["ps%d" % bank, "gateT"], writes=["ysb%d" % cc])

            def scatter(ex):
                for cc in range(2):
                    S.op("gpsimd", lambda e, cc=cc, ex=ex: e.indirect_dma_start(
                        out=X[:, :], out_offset=bass.IndirectOffsetOnAxis(ap=idxT[:, cc, ex:ex + 1], axis=0),
                        in_=ysb[cc], in_offset=None, compute_op=ALU.add),
                         reads=["ysb%d" % cc, "idxT"], writes=["Xmoe"], dma=True)

            gather(0)
            for ex in range(NE):
                nxt = ex + 1 < NE
                if nxt:
                    gather(ex + 1)
                transposes(ex)
                hidden(ex, 0)
                if nxt:
                    load_piece(ex + 1, 0)
                    load_piece(ex + 1, 1)
                hidden(ex, 1)
                if nxt:
                    load_piece(ex + 1, 2)
                    load_piece(ex + 1, 3)
                down(ex, 0)
                if nxt:
                    load_piece(ex + 1, 4)
                down(ex, 1)
                if nxt:
                    load_piece(ex + 1, 5)
                scatter(ex)

        def phase_pool(X_src, X_dst):
            b.phase()
            global wring
            wring = [b.alloc("w%d" % i, [2048], BF16) for i in range(4)]
            hT = b.alloc("hT", [16, S_], BF16)
            xa = b.alloc("xa", [16, D_], BF16)
            strips = b.alloc("strips", [4, 6 * 512], BF16)
            edges = b.alloc("edges", [4, 2 * 128], BF16)
            scale_b = [b.alloc("scale_b0", [512], F32), b.alloc("scale_b1", [512], F32)]
            load_gain(mix_norm[1])
            wts = [wring[g].rearrange("p (k n) -> p k n", k=4) for g in range(4)]

            def load_consts():
                S.dma("gpsimd", strips.rearrange("p w (k n) -> p w k n", k=6), c_band.rearrange("w k p n -> p w k n"), writes=["strips"])
                S.dma("gpsimd", edges.rearrange("p w (k n) -> p w k n", k=2), c_bedge.rearrange("w k p n -> p w k n"), writes=["edges"])
                for g in range(4):
                    S.dma("gpsimd", wts[g][:, 0:4, :], pool_w[g].rearrange("(k p) n -> p k n", p=128), writes=["w%d" % g])
                for g in range(4):
                    sbg = scale_b[g % 2]
                    S.dma("gpsimd", sbg, pool_scale[g * 512:(g + 1) * 512].partition_broadcast(128), writes=["scale_b%d" % (g % 2)])
                    for k in range(4):
                        S.op("gpsimd", lambda e, g=g, k=k, sbg=sbg: e.tensor_tensor(out=wts[g][:, k, :], in0=wts[g][:, k, :], in1=sbg, op=ALU.mult),
                             reads=["w%d" % g, "scale_b%d" % (g % 2)], writes=["w%d" % g])
            xts = [b.alloc("xt0", [D_], F32), b.alloc("xt1", [D_], F32)]
            ssq2 = [b.alloc("ssq0", [1], F32), b.alloc("ssq1", [1], F32)]
            rstd2 = [b.alloc("rstd0", [1], F32), b.alloc("rstd1", [1], F32)]
            NT = S_ // 128
            S.dma("sync", xts[0], X_src[0:128, :], writes=["xt0"])
            for ti in range(NT):
                i = ti % 2
                xt, ssq, rstd = xts[i], ssq2[i], rstd2[i]
                if ti + 1 < NT:
                    S.dma("sync", xts[1 - i], X_src[(ti + 1) * 128:(ti + 2) * 128, :], writes=["xt%d" % (1 - i)])
                S.op("scalar", lambda e, xt=xt, ssq=ssq, ti=ti: e.activation(out=xa[:, ti, :], in_=xt, func=AF.Square, accum_out=ssq),
                     reads=["xt%d" % i], writes=["xa%d" % ti, "ssq%d" % i])
                S.op("scalar", lambda e, ssq=ssq, rstd=rstd: e.activation(out=rstd, in_=ssq, func=AF.Sqrt, scale=1.0 / D_, bias=eps),
                     reads=["ssq%d" % i], writes=["rstd%d" % i])
                S.op("vector", lambda e, rstd=rstd: e.reciprocal(out=rstd, in_=rstd), reads=["rstd%d" % i], writes=["rstd%d" % i])
                S.op("vector", lambda e, xt=xt, rstd=rstd, ti=ti: e.scalar_tensor_tensor(out=xa[:, ti, :], in0=xt, scalar=rstd, in1=gb,
                                                                                        op0=ALU.mult, op1=ALU.mult),
                     reads=["xt%d" % i, "rstd%d" % i, "gb"], writes=["xa%d" % ti])
                if ti == 2:
                    load_consts()
            sv = strips.rearrange("p w (k n) -> p w k n", k=6)
            ev = edges.rearrange("p w (k n) -> p w k n", k=2)
            pc = 0
            for J in range(4):
                for ch in range(16):
                    wi = ch // 4
                    bank = pc % 8
                    pc += 1
                    pk = "ps%d" % bank
                    for pos in range(4):
                        Sx = 4 * J + pos
                        kidx = pos
                        if Sx == 0:
                            kidx = 4
                        elif Sx == NT - 1:
                            kidx = 5
                        S.op("tensor", lambda e, Sx=Sx, ch=ch, wi=wi, kidx=kidx, bank=bank, pos=pos: e.matmul(
                            b.ps(bank), lhsT=xa[:, Sx, ch * 128:(ch + 1) * 128], rhs=sv[:, wi, kidx, :],
                            start=(pos == 0), stop=False), reads=["xa%d" % Sx, "strips"], writes=[pk])
                    last_full = True
                    if J > 0:
                        S.op("tensor", lambda e, J=J, ch=ch, wi=wi, bank=bank: e.matmul(
                            b.ps(bank)[:, 0:128], lhsT=xa[:, 4 * J - 1, ch * 128:(ch + 1) * 128], rhs=ev[:, wi, 0, :],
                            start=False, stop=False), reads=["xa%d" % (4 * J - 1), "edges"], writes=[pk])
                    if J < 3:
                        S.op("tensor", lambda e, J=J, ch=ch, wi=wi, bank=bank: e.matmul(
                            b.ps(bank)[:, 384:512], lhsT=xa[:, 4 * J + 4, ch * 128:(ch + 1) * 128], rhs=ev[:, wi, 1, :],
                            start=False, stop=False), reads=["xa%d" % (4 * J + 4), "edges"], writes=[pk])
                    dst = hT[:, ch, J * 512:(J + 1) * 512]
                    if pc % 2 == 0:
                        S.op("vector", lambda e, dst=dst, bank=bank: e.tensor_copy(out=dst, in_=b.ps(bank)), reads=[pk], writes=["hTc%d_%d" % (ch, J)])
                    else:
                        S.op("scalar", lambda e, dst=dst, bank=bank: e.copy(out=dst, in_=b.ps(bank)), reads=[pk], writes=["hTc%d_%d" % (ch, J)])
            for ti in range(S_ // 128):
                i = ti % 2
                r = xts[i]
                rk = "xt%d" % i
                rows = slice(ti * 128, (ti + 1) * 128)
                S.dma("sync", r, X_src[rows, :], writes=[rk])
                for g in range(4):
                    bank = (ti * 4 + g) % 8
                    for k in range(4):
                        S.op("tensor", lambda e, k=k, ti=ti, g=g, bank=bank: e.matmul(
                            b.ps(bank), lhsT=hT[:, g * 4 + k, ti * 128:(ti + 1) * 128], rhs=wts[g][:, k, :],
                            start=(k == 0), stop=(k == 3)), reads=["hTc%d_%d" % (g * 4 + k, ti // 4), "w%d" % g], writes=["ps%d" % bank])
                    S.op("vector", lambda e, g=g, bank=bank, r=r: e.tensor_tensor(out=r[:, g * 512:(g + 1) * 512], in0=b.ps(bank),
                                                                               in1=r[:, g * 512:(g + 1) * 512], op=ALU.add),
                         reads=["ps%d" % bank, rk], writes=[rk])
                S.dma("sync", X_dst[rows, :], r, reads=[rk], writes=["Xdst"])

        def phase_final(X_src):
            b.phase()
            xts = [b.alloc("xt0", [D_], F32), b.alloc("xt1", [D_], F32)]
            junk = b.alloc("njunk", [D_], BF16)
            ssq2 = [b.alloc("ssq0", [1], F32), b.alloc("ssq1", [1], F32)]
            rstd2 = [b.alloc("rstd0", [1], F32), b.alloc("rstd1", [1], F32)]
            yo = [b.alloc("yo0", [D_], F32), b.alloc("yo1", [D_], F32)]
            load_gain(final_norm)
            for ti in range(S_ // 128):
                rows = slice(ti * 128, (ti + 1) * 128)
                i = ti % 2
                xt, ssq, rstd, y = xts[i], ssq2[i], rstd2[i], yo[i]
                if ti == 0:
                    S.dma("sync", xt, X_src[rows, :], writes=["xt%d" % i])
                if ti + 1 < S_ // 128:
                    S.dma("sync", xts[1 - i], X_src[(ti + 1) * 128:(ti + 2) * 128, :], writes=["xt%d" % (1 - i)])
                S.op("scalar", lambda e, xt=xt, ssq=ssq: e.activation(out=junk, in_=xt, func=AF.Square, accum_out=ssq),
                     reads=["xt%d" % i], writes=["njunk", "ssq%d" % i])
                S.op("scalar", lambda e, ssq=ssq, rstd=rstd: e.activation(out=rstd, in_=ssq, func=AF.Sqrt, scale=1.0 / D_, bias=eps),
                     reads=["ssq%d" % i], writes=["rstd%d" % i])
                S.op("vector", lambda e, rstd=rstd: e.reciprocal(out=rstd, in_=rstd), reads=["rstd%d" % i], writes=["rstd%d" % i])
                S.op("vector", lambda e, xt=xt, y=y, rstd=rstd: e.scalar_tensor_tensor(out=y, in0=xt, scalar=rstd, in1=gb, op0=ALU.mult, op1=ALU.mult),
                     reads=["xt%d" % i, "rstd%d" % i, "gb"], writes=["yo%d" % i])
                S.dma("sync", y_out[rows, :], y, reads=["yo%d" % i], writes=["y"])

        def copy_out(X_src):
            b.phase()
            t = [b.alloc("co0", [D_], F32), b.alloc("co1", [D_], F32)]
            for ti in range(S_ // 128):
                rows = slice(ti * 128, (ti + 1) * 128)
                S.dma("sync", t[ti % 2], X_src[rows, :], writes=["co%d" % (ti % 2)])
                S.dma("sync", y_out[rows, :], t[ti % 2], reads=["co%d" % (ti % 2)], writes=["y"])

        def run():
            phase_attn_proj()
            phase_attention()
            phase_fourier()
            phase_outproj(w_out, x_in, XB)
            if stage == 1:
                return copy_out(XB)
            phase_cross(0, XB, XA)
            if stage == 2:
                return copy_out(XA)
            phase_moe(0, XA)
            if stage == 30:
                return
            if stage == 3:
                return copy_out(XA)
            phase_pool(XA, XB)
            if stage == 4:
                return copy_out(XB)
            phase_cross(1, XB, XA)
            if stage == 5:
                return copy_out(XA)
            phase_moe(1, XA)
            if stage == 6:
                return copy_out(XA)
            phase_final(XA)

        run()
        counts = S.finalize()
        print("instr counts", counts, flush=True)
    return nc


wring = []


def host_constants():
    S = S_
    rows = S // 64
    row_idx = np.repeat(np.arange(rows), 64).astype(np.float32)
    col_idx = np.tile(np.arange(64), rows).astype(np.float32)
    inv_freq = (1.0 / (10000.0 ** (np.arange(0, 64, 2, dtype=np.float32) / 64))).astype(np.float32)
    ang = np.concatenate([row_idx[:, None] * inv_freq[None, :], col_idx[:, None] * inv_freq[None, :]], axis=-1)
    cos = np.cos(ang).astype(np.float32)
    sin = np.sin(ang).astype(np.float32)
    c_rope = np.stack([np.tile(cos, (1, 4)), np.tile(sin, (1, 4))]).astype(np.float32)
    n = np.arange(S, dtype=np.int64)
    ph = (np.outer(n, n) % S).astype(np.float64) * (2 * np.pi / S)
    c_dftS = np.stack([np.cos(ph), np.sin(ph)]).astype(np.float32)
    c = np.arange(128, dtype=np.int64)
    pc = (np.outer(c, c) % 128).astype(np.float64) * (2 * np.pi / 128)
    c_dftC = np.stack([np.cos(pc) / 512.0, -np.sin(pc) / 512.0]).astype(np.float32)
    t = np.arange(S)
    bands = []
    bedges = []
    for w in (2, 4, 8, 16):
        lo = np.clip(t - w // 2, 0, S)
        hi = np.clip(t + w - w // 2, 0, S)
        M = np.zeros((S, S), np.float32)
        for tt_ in range(S):
            M[lo[tt_]:hi[tt_], tt_] = 1.0 / float(hi[tt_] - lo[tt_])
        M[np.arange(S), np.arange(S)] -= 1.0
        st = [M[(4 + p) * 128:(5 + p) * 128, 512:1024] for p in range(4)]
        st.append(M[0:128, 0:512])
        st.append(M[S - 128:S, S - 512:S])
        bands.append(np.stack(st))
        bedges.append(np.stack([M[3 * 128:4 * 128, 512:640], M[8 * 128:9 * 128, 7 * 128:8 * 128]]))
    c_band = np.stack(bands).astype(np.float32)
    c_bedge = np.stack(bedges).astype(np.float32)
    return {"c_rope": c_rope, "c_dftS": c_dftS, "c_dftC": c_dftC, "c_band": c_band, "c_bedge": c_bedge}


def make_in_maps(inputs, n_cores=8):
    g = {k: np.ascontiguousarray(np.asarray(v), dtype=np.float32) for k, v in inputs.items()}
    shared = {
        "mix_norm": g["mix_norm"], "attn_w_in": g["attn_w_in"][0], "q_gain": g["q_gain"][0], "k_gain": g["k_gain"][0],
        "fourier_w": g["fourier_w"][0], "attn_w_out": g["attn_w_out"][0], "pool_w": g["pool_w"][0],
        "pool_scale": g["pool_scale"][0], "cross_norm": g["cross_norm"], "mem_norm": g["mem_norm"],
        "cross_w_q": g["cross_w_q"], "cross_w_k": g["cross_w_k"], "cross_w_v": g["cross_w_v"], "cross_w_o": g["cross_w_o"],
        "ffn_norm": g["ffn_norm"], "router_w": g["router_w"], "expert_w_gate": g["expert_w_gate"],
        "expert_w_up": g["expert_w_up"], "expert_w_down": g["expert_w_down"], "final_norm": g["final_norm"],
    }
    shared.update(host_constants())
    maps = []
    for c in range(n_cores):
        m = dict(shared)
        m["x"] = g["x"][c % 4]
        m["mem"] = g["mem"][c % 4]
        maps.append(m)
    return maps


def kernel(**inputs):
    nc = build_program()
    maps = make_in_maps(inputs, 4)
    res = run_bass_kernel_spmd(nc, maps, core_ids=list(range(4)))
    out = np.stack([np.asarray(res.results[c]["y"], dtype=np.float32) for c in range(4)], axis=0)
    return out
```

```python
from contextlib import ExitStack
import numpy as np
import concourse.bass as bass
import concourse.mybir as mybir
from concourse.bass_utils import run_bass_kernel_spmd

F32 = mybir.dt.float32
BF16 = mybir.dt.bfloat16
I32 = mybir.dt.int32
U32 = mybir.dt.uint32
U8 = mybir.dt.uint8
ALU = mybir.AluOpType
AF = mybir.ActivationFunctionType
AX = mybir.AxisListType

S_ = 2048
D_ = 2048
NE = 16
CAP = 256
ENGS = ["sync", "scalar", "gpsimd", "vector", "tensor"]
DMA_K = 8


class Op:
    __slots__ = ("eng", "fn", "deps", "dma", "idx", "needed", "sig", "waits", "dma_j")

    def __init__(self, eng, fn, deps, dma):
        self.eng = eng
        self.fn = fn
        self.deps = deps
        self.dma = dma
        self.needed = False
        self.sig = None
        self.waits = []
        self.dma_j = None


class Sched:
    def __init__(self, nc):
        self.nc = nc
        self.ops = []
        self.last_w = {}
        self.readers = {}
        self.dma_count = {e: 0 for e in ENGS}
        self.dma_ops = {e: [] for e in ENGS}
        self.last_on = {e: None for e in ENGS}
        self.last_comp = {e: None for e in ENGS}
        self.fence = set()
        self.fenced = {e: True for e in ENGS}

    def barrier(self):
        f = set()
        for e in ENGS:
            if self.last_on[e] is not None:
                f.add(self.last_on[e])
            if self.last_comp[e] is not None:
                f.add(self.last_comp[e])
            n = self.dma_count[e]
            for j in range(max(0, n - DMA_K), n):
                f.add(self.dma_ops[e][j].idx)
        self.fence = f
        self.fenced = {e: False for e in ENGS}
        self.last_w = {}
        self.readers = {}

    def op(self, eng, fn, reads=(), writes=(), dma=False):
        pr = [r for r in reads if r.startswith("ps")]
        if pr:
            reads = [r for r in reads if not r.startswith("ps")]
            writes = list(writes) + pr
        deps = set()
        if not self.fenced[eng]:
            deps |= self.fence
            self.fenced[eng] = True
        for r in reads:
            w = self.last_w.get(r)
            if w is not None:
                deps.add(w)
        for w_ in writes:
            w = self.last_w.get(w_)
            if w is not None:
                deps.add(w)
            for r in self.readers.get(w_, ()):
                deps.add(r)
        o = Op(eng, fn, deps, dma)
        o.idx = len(self.ops)
        if dma:
            j = self.dma_count[eng]
            o.dma_j = j
            self.dma_count[eng] += 1
            if j >= DMA_K:
                deps.add(self.dma_ops[eng][j - DMA_K].idx)
            self.dma_ops[eng].append(o)
        deps.discard(o.idx)
        self.ops.append(o)
        self.last_on[eng] = o.idx
        if not dma:
            self.last_comp[eng] = o.idx
        for r in reads:
            self.readers.setdefault(r, []).append(o.idx)
        for w_ in writes:
            self.last_w[w_] = o.idx
            self.readers[w_] = []
        return o

    def dma(self, eng, out, in_, reads=(), writes=(), **kw):
        return self.op(eng, lambda e: e.dma_start(out=out, in_=in_, **kw), reads, writes, dma=True)

    def finalize(self):
        nc = self.nc
        ops = self.ops
        tail = []
        for e in ENGS:
            n = self.dma_count[e]
            for j in range(max(0, n - DMA_K), n):
                tail.append(self.dma_ops[e][j].idx)
        fin = Op("sync", None, set(tail), False)
        fin.idx = len(ops)
        ops.append(fin)
        for o in ops:
            for d in o.deps:
                p = ops[d]
                if p.eng == "tensor" and o.eng == "tensor" and not p.dma and not o.dma:
                    continue
                p.needed = True
        with ExitStack() as st:
            csem = {e: st.enter_context(nc.semaphore("c_" + e)) for e in ENGS}
            dsem = {e: [st.enter_context(nc.semaphore("d_%s%d" % (e, k))) for k in range(DMA_K)]
                    for e in ENGS if self.dma_count[e] > 0}
            cnt = {e: 0 for e in ENGS}
            for o in ops:
                if o.dma:
                    k = o.dma_j % DMA_K
                    o.sig = (dsem[o.eng][k], 16, 16 * (o.dma_j // DMA_K + 1))
                elif o.needed:
                    cnt[o.eng] += 1
                    o.sig = (csem[o.eng], 1, cnt[o.eng])
            seen = {e: {} for e in ENGS}
            for o in ops:
                need = {}
                for d in o.deps:
                    p = ops[d]
                    if p.sig is None:
                        continue
                    if p.eng == "tensor" and o.eng == "tensor" and not p.dma and not o.dma:
                        continue
                    sem, _, val = p.sig
                    key = id(sem)
                    if key not in need or need[key][1] < val:
                        need[key] = (sem, val)
                sn = seen[o.eng]
                for key, (sem, val) in need.items():
                    if sn.get(key, 0) >= val:
                        continue
                    sn[key] = val
                    o.waits.append((sem, val))
            by_eng = {e: [o for o in ops if o.eng == e] for e in ENGS}

            def emit(name, e):
                for o in by_eng[name]:
                    for sem, val in o.waits:
                        e.wait_ge(sem, val)
                    if o.fn is None:
                        continue
                    ins = o.fn(e)
                    if o.sig is not None:
                        ins.then_inc(o.sig[0], o.sig[1])

            with nc.Block() as block:
                @block.sync
                def _(e):
                    emit("sync", e)

                @block.scalar
                def _(e):
                    emit("scalar", e)

                @block.gpsimd
                def _(e):
                    emit("gpsimd", e)

                @block.vector
                def _(e):
                    emit("vector", e)

                @block.tensor
                def _(e):
                    emit("tensor", e)
        return {e: len(by_eng[e]) for e in ENGS}


ARENA = 207 * 1024


class B:
    def __init__(self, nc, st):
        self.nc = nc
        self.S = Sched(nc)
        self.arena = st.enter_context(nc.sbuf_tensor("arena", [128, ARENA], U8))
        self.off = 0
        self.mark = 0
        self.psf = [st.enter_context(nc.psum_tensor("ps%d" % i, [128, 512], F32)) for i in range(8)]
        self.wslot = 0
        self.uid = 0

    def alloc(self, name, free_shape, dt):
        n = 1
        for s in free_shape:
            n *= s
        nb = n * (2 if dt == BF16 else 4)
        nb = (nb + 63) // 64 * 64
        assert self.off + nb <= ARENA, (name, self.off, nb)
        v = self.arena[:, self.off:self.off + nb].bitcast(dt)
        self.off += nb
        nn = 1
        for s in free_shape:
            nn *= s
        v = v[:, 0:nn]
        if len(free_shape) == 2:
            v = v.rearrange("p (a b) -> p a b", a=free_shape[0])
        elif len(free_shape) == 3:
            v = v.rearrange("p (a b c) -> p a b c", a=free_shape[0], b=free_shape[1])
        return v

    def phase(self):
        self.S.barrier()
        self.off = self.mark

    def ps(self, i):
        return self.psf[i][:]

    def psb(self, i):
        return self.psf[i][:].bitcast(BF16).rearrange("p (a b) -> p a b", a=8)


def build_program(stage=99):
    nc = bass.Bass("TRN2", target_bir_lowering=False)

    def din(name, shape, dt=F32):
        return nc.dram_tensor(name, list(shape), dt, kind="ExternalInput").ap()

    def dscr(name, shape, dt):
        return nc.dram_tensor(name, list(shape), dt, kind="Internal").ap()

    x_in = din("x", [S_, D_])
    mem_in = din("mem", [256, D_])
    mix_norm = din("mix_norm", [2, D_])
    w_in = din("attn_w_in", [D_, 3072])
    q_gain = din("q_gain", [128])
    k_gain = din("k_gain", [128])
    fourier_w = din("fourier_w", [4, 128, 128])
    w_out = din("attn_w_out", [D_, D_])
    pool_w = din("pool_w", [4, 512, 512])
    pool_scale = din("pool_scale", [D_])
    cross_norm = din("cross_norm", [2, D_])
    mem_norm = din("mem_norm", [2, D_])
    cw_q = din("cross_w_q", [2, D_, D_])
    cw_k = din("cross_w_k", [2, D_, D_])
    cw_v = din("cross_w_v", [2, D_, D_])
    cw_o = din("cross_w_o", [2, D_, D_])
    ffn_norm = din("ffn_norm", [2, D_])
    router_w = din("router_w", [2, D_, NE])
    ew_gate = din("expert_w_gate", [2, NE, D_, 1024])
    ew_up = din("expert_w_up", [2, NE, D_, 1024])
    ew_down = din("expert_w_down", [2, NE, 1024, D_])
    final_norm = din("final_norm", [D_])
    c_rope = din("c_rope", [2, S_, 256])
    c_dftS = din("c_dftS", [2, S_, S_])
    c_dftC = din("c_dftC", [2, 128, 128])
    c_band = din("c_band", [4, 6, 128, 512])
    c_bedge = din("c_bedge", [4, 2, 128, 128])
    y_out = nc.dram_tensor("y", [S_, D_], F32, kind="ExternalOutput").ap()

    XA = dscr("XA", [S_, D_], F32)
    XB = dscr("XB", [S_, D_], F32)
    qT_d = dscr("qT_d", [12, 128, S_], BF16)
    kT_d = dscr("kT_d", [4, 128, S_], BF16)
    v_d = dscr("v_d", [S_, 512], BF16)
    fT_d = dscr("fT_d", [4, 128, S_], BF16)
    mixT_d = dscr("mixT_d", [16, 128, S_], BF16)
    hmoe_d = dscr("hmoe_d", [S_, D_], BF16)

    with ExitStack() as st:
        b = B(nc, st)
        S = b.S
        identf = b.alloc("identf", [128], F32)
        identb = b.alloc("identb", [128], BF16)
        onesb = b.alloc("onesb", [128], BF16)
        onesf = b.alloc("onesf", [128], F32)
        eps = b.alloc("eps", [1], F32)
        gb = b.alloc("gb", [D_], F32)
        S.op("gpsimd", lambda e: e.memset(identf, 0.0), writes=["identf"])
        S.op("gpsimd", lambda e: e.affine_select(out=identf, in_=identf, pattern=[[-1, 128]],
                                                 compare_op=ALU.not_equal, fill=1.0, base=0,
                                                 channel_multiplier=1), reads=["identf"], writes=["identf"])
        S.op("vector", lambda e: e.tensor_copy(out=identb, in_=identf), reads=["identf"], writes=["identb"])
        S.op("vector", lambda e: e.memset(onesb, 1.0), writes=["onesb"])
        S.op("vector", lambda e: e.memset(onesf, 1.0), writes=["onesf"])
        S.op("vector", lambda e: e.memset(eps, 1e-6), writes=["eps"])
        b.mark = b.off
        CONST = ["identf", "identb", "onesb", "onesf", "eps"]

        def keep_consts():
            pass

        def load_gain(gain_ap):
            S.dma("sync", gb, gain_ap.partition_broadcast(128), writes=["gb"])

        def norm_tile(src_rows, ti, xts, xn, ssq, rstd, out_fp32=None):
            xt = xts[ti % 2]
            xk = "xt%d" % (ti % 2)
            S.dma("sync", xt, src_rows, writes=[xk])
            S.op("scalar", lambda e: e.activation(out=xn, in_=xt, func=AF.Square, accum_out=ssq),
                 reads=[xk], writes=["xn", "ssq"])
            S.op("scalar", lambda e: e.activation(out=rstd, in_=ssq, func=AF.Sqrt, scale=1.0 / D_, bias=eps),
                 reads=["ssq"], writes=["rstd"])
            S.op("vector", lambda e: e.reciprocal(out=rstd, in_=rstd), reads=["rstd"], writes=["rstd"])
            if out_fp32 is not None:
                S.op("vector", lambda e: e.scalar_tensor_tensor(out=out_fp32, in0=xt, scalar=rstd, in1=gb,
                                                                op0=ALU.mult, op1=ALU.mult),
                     reads=[xk, "rstd", "gb"], writes=["xnf"])
                S.op("gpsimd", lambda e: e.tensor_copy(out=xn, in_=out_fp32), reads=["xnf"], writes=["xn"])
            else:
                S.op("vector", lambda e: e.scalar_tensor_tensor(out=xn, in0=xt, scalar=rstd, in1=gb,
                                                                op0=ALU.mult, op1=ALU.mult),
                     reads=[xk, "rstd", "gb"], writes=["xn"])

        def transpose_to(hT, col0, xn, kc=16):
            for half in range(kc // 8):
                pb = b.psb(half)
                pk = "ps%d" % half
                for k in range(8):
                    kk = half * 8 + k
                    S.op("tensor", lambda e, k=k, kk=kk, pb=pb: e.transpose(out=pb[:, k, :], in_=xn[:, kk * 128:(kk + 1) * 128],
                                                                           identity=identb),
                         reads=["xn"], writes=[pk])
                eng = "vector" if half == 0 else "scalar"
                dst = hT[:, half * 8:(half + 1) * 8, col0:col0 + 128]
                if eng == "vector":
                    S.op("vector", lambda e, dst=dst, pb=pb: e.tensor_copy(out=dst, in_=pb), reads=[pk], writes=["hT"])
                else:
                    S.op("scalar", lambda e, dst=dst, pb=pb: e.copy(out=dst, in_=pb), reads=[pk], writes=["hT"])

        def norm_loop(rows_of, n_tiles, hT, hkey="hT", junk=None):
            xts = [b.alloc("xt0", [D_], F32), b.alloc("xt1", [D_], F32)]
            xn2 = [b.alloc("xn0", [D_], BF16), b.alloc("xn1", [D_], BF16)]
            if junk is None:
                junk = b.alloc("njunk", [D_], BF16)
            ssq2 = [b.alloc("ssq0", [1], F32), b.alloc("ssq1", [1], F32)]
            rstd2 = [b.alloc("rstd0", [1], F32), b.alloc("rstd1", [1], F32)]

            def load(ti):
                S.dma("sync", xts[ti % 2], rows_of(ti), writes=["xt%d" % (ti % 2)])

            def early(ti):
                i = ti % 2
                xt, xn, ssq, rstd = xts[i], xn2[i], ssq2[i], rstd2[i]
                S.op("scalar", lambda e: e.activation(out=junk, in_=xt, func=AF.Square, accum_out=ssq),
                     reads=["xt%d" % i], writes=["njunk", "ssq%d" % i])
                S.op("scalar", lambda e: e.activation(out=rstd, in_=ssq, func=AF.Sqrt, scale=1.0 / D_, bias=eps),
                     reads=["ssq%d" % i], writes=["rstd%d" % i])
                S.op("vector", lambda e: e.reciprocal(out=rstd, in_=rstd), reads=["rstd%d" % i], writes=["rstd%d" % i])
                S.op("vector", lambda e: e.scalar_tensor_tensor(out=xn, in0=xt, scalar=rstd, in1=gb, op0=ALU.mult, op1=ALU.mult),
                     reads=["xt%d" % i, "rstd%d" % i, "gb"], writes=["xn%d" % i])

            def late(ti):
                i = ti % 2
                xn = xn2[i]
                for half in range(2):
                    pb = b.psb(half)
                    pk = "ps%d" % half
                    for k in range(8):
                        kk = half * 8 + k
                        S.op("tensor", lambda e, k=k, kk=kk, pb=pb: e.transpose(out=pb[:, k, :], in_=xn[:, kk * 128:(kk + 1) * 128],
                                                                               identity=identb),
                             reads=["xn%d" % i], writes=[pk])
                    dst = hT[:, half * 8:(half + 1) * 8, ti * 128:(ti + 1) * 128]
                    if half == 0:
                        S.op("vector", lambda e, dst=dst, pb=pb: e.tensor_copy(out=dst, in_=pb), reads=[pk], writes=[hkey])
                    else:
                        S.op("scalar", lambda e, dst=dst, pb=pb: e.copy(out=dst, in_=pb), reads=[pk], writes=[hkey])

            load(0)
            for ti in range(n_tiles + 1):
                if ti + 1 < n_tiles:
                    load(ti + 1)
                if ti < n_tiles:
                    early(ti)
                if ti >= 1:
                    late(ti - 1)

        def norm_phase_alloc():
            xts = [b.alloc("xt0", [D_], F32), b.alloc("xt1", [D_], F32)]
            xn = b.alloc("xn", [D_], BF16)
            ssq = b.alloc("ssq", [1], F32)
            rstd = b.alloc("rstd", [1], F32)
            return xts, xn, ssq, rstd

        def wpiece(src_ap, kdim=16):
            slot = b.wslot % len(wring)
            b.wslot += 1
            wt = wring[slot].rearrange("p (k n) -> p k n", k=kdim)
            S.dma("gpsimd", wt, src_ap, writes=["w%d" % slot])
            return wt, "w%d" % slot

        def linear_tok(hT, T, w_src, n_cols, epilogue, kc=16, banks=(2, 3, 4, 5, 6, 7), hkey="hT"):
            cnt = 0
            nblk = n_cols // 512
            nxt = wpiece(w_src(0).rearrange("(k p) n -> p k n", p=128))
            for nb in range(nblk):
                wt, wk = nxt
                if nb + 1 < nblk:
                    nxt = wpiece(w_src(nb + 1).rearrange("(k p) n -> p k n", p=128))
                for ti in range(T // 128):
                    bank = banks[cnt % len(banks)]
                    cnt += 1
                    pk = "ps%d" % bank
                    for k in range(kc):
                        S.op("tensor", lambda e, k=k, ti=ti, bank=bank, wt=wt: e.matmul(
                            b.ps(bank), lhsT=hT[:, k, ti * 128:(ti + 1) * 128], rhs=wt[:, k, :],
                            start=(k == 0), stop=(k == kc - 1)), reads=[hkey, wk], writes=[pk])
                    epilogue(nb, ti, b.ps(bank), pk)

        def linear_feat(hT, T, w_src, n_cols, epilogue, kc=16, banks=(2, 3, 4, 5, 6, 7), hkey="hT"):
            cnt = 0
            nblk = n_cols // 512
            nxt = wpiece(w_src(0).rearrange("(k p) n -> p k n", p=128))
            for nb in range(nblk):
                wt, wk = nxt
                if nb + 1 < nblk:
                    nxt = wpiece(w_src(nb + 1).rearrange("(k p) n -> p k n", p=128))
                for nt in range(4):
                    for tb in range(T // 512):
                        bank = banks[cnt % len(banks)]
                        cnt += 1
                        pk = "ps%d" % bank
                        for k in range(kc):
                            S.op("tensor", lambda e, k=k, nt=nt, tb=tb, bank=bank, wt=wt: e.matmul(
                                b.ps(bank), lhsT=wt[:, k, nt * 128:(nt + 1) * 128],
                                rhs=hT[:, k, tb * 512:(tb + 1) * 512],
                                start=(k == 0), stop=(k == kc - 1)), reads=[hkey, wk], writes=[pk])
                        epilogue(nb * 4 + nt, tb, b.ps(bank), pk)

        TG = 1024
        NTG = S_ // TG

        def make_resid_epilogue(X_src, X_dst, t_base, order, scale_b=None):
            NRX = 3
            rx = [b.alloc("rx%d" % i, [512], F32) for i in range(NRX)]
            ctr = [0]
            issued = [0]
            sc_tmp = b.alloc("sc_tmp", [512], F32) if scale_b is not None else None

            def issue_loads(upto):
                while issued[0] < min(upto, len(order)):
                    k = issued[0]
                    nb_, ti_ = order[k]
                    rows = slice(t_base + ti_ * 128, t_base + (ti_ + 1) * 128)
                    cols = slice(nb_ * 512, (nb_ + 1) * 512)
                    S.dma("sync", rx[k % NRX], X_src[rows, cols], writes=["rx%d" % (k % NRX)])
                    issued[0] += 1

            def ep(nb, ti, ps, pk):
                k = ctr[0]
                ctr[0] += 1
                assert order[k] == (nb, ti), (order[k], nb, ti)
                issue_loads(k + 2)
                i = k % NRX
                r = rx[i]
                rk = "rx%d" % i
                rows = slice(t_base + ti * 128, t_base + (ti + 1) * 128)
                cols = slice(nb * 512, (nb + 1) * 512)
                if scale_b is not None:
                    S.op("vector", lambda e: e.tensor_tensor(out=sc_tmp, in0=ps, in1=scale_b[:, cols], op=ALU.mult),
                         reads=[pk, "scale_b"], writes=["sc_tmp"])
                    S.op("gpsimd", lambda e: e.tensor_tensor(out=r, in0=sc_tmp, in1=r, op=ALU.add),
                         reads=["sc_tmp", rk], writes=[rk])
                else:
                    S.op("vector", lambda e: e.tensor_tensor(out=r, in0=ps, in1=r, op=ALU.add),
                         reads=[pk, rk], writes=[rk])
                S.dma("sync", X_dst[rows, cols], r, reads=[rk], writes=["Xdst"])
            return ep

        def phase_attn_proj():
            for tg in range(NTG):
                b.phase()
                global wring
                wring = [b.alloc("w%d" % i, [8192], BF16) for i in range(3)]
                hT = b.alloc("hT", [16, TG], BF16)
                load_gain(mix_norm[0])
                norm_loop(lambda ti: x_in[tg * TG + ti * 128: tg * TG + (ti + 1) * 128, :], TG // 128, hT)
                NB3 = 3
                stage = [b.alloc("stg0", [4, TG], BF16), b.alloc("stg1", [4, TG], BF16)]
                junk = [b.alloc("junk%d" % i, [512], BF16) for i in range(NB3)]
                xg = [b.alloc("xg%d" % i, [512], F32) for i in range(NB3)]
                ro = [b.alloc("ro%d" % i, [512], F32) for i in range(NB3)]
                tt = [[b.alloc("tt%d_%d" % (i, j), [256], F32) for j in range(4)] for i in range(NB3)]
                qr = [b.alloc("qr%d" % i, [512], BF16) for i in range(NB3)]
                ssq4 = [b.alloc("ssq4_%d" % i, [4], F32) for i in range(NB3)]
                rs4 = [b.alloc("rs4_%d" % i, [4], F32) for i in range(NB3)]
                gq4 = b.alloc("gq4", [512], F32)
                gk4 = b.alloc("gk4", [512], F32)
                cs = [b.alloc("cs0", [2, 256], F32), b.alloc("cs1", [2, 256], F32)]
                vst = [b.alloc("vst0", [512], BF16), b.alloc("vst1", [512], BF16)]
                for h in range(4):
                    S.dma("sync", gq4[:, h * 128:(h + 1) * 128], q_gain.partition_broadcast(128), writes=["gq4"])
                    S.dma("sync", gk4[:, h * 128:(h + 1) * 128], k_gain.partition_broadcast(128), writes=["gk4"])
                cctr = [0]
                stB = {}
                stC = {}

                def load_cs(n):
                    ti_ = n % (TG // 128)
                    rows = slice(tg * TG + ti_ * 128, tg * TG + (ti_ + 1) * 128)
                    S.dma("sync", cs[n % 2][:, 0, :], c_rope[0, rows, :], writes=["cs%d" % (n % 2)])
                    S.dma("sync", cs[n % 2][:, 1, :], c_rope[1, rows, :], writes=["cs%d" % (n % 2)])

                def run_stage(d, n):
                    f = d.pop(n, None)
                    if f is not None:
                        f()

                def flush():
                    n = cctr[0]
                    run_stage(stB, n - 1)
                    run_stage(stC, n - 2)
                    run_stage(stC, n - 1)

                def qk_epilogue(nb, ti, ps, pk):
                    gg, ggk = (gq4, "gq4") if nb < 3 else (gk4, "gk4")
                    n = cctr[0]
                    cctr[0] += 1
                    eb = n % NB3
                    c2 = cs[n % 2]
                    ck = "cs%d" % (n % 2)
                    if n == 0:
                        load_cs(0)
                    if n + 1 < 4 * (TG // 128):
                        load_cs(n + 1)
                    jk, sq, rs = junk[eb], ssq4[eb], rs4[eb]
                    x_, r_, q_ = xg[eb], ro[eb], qr[eb]
                    t1, t2, t3, t4 = tt[eb]
                    K = lambda nm: "%s%d" % (nm, eb)
                    for h in range(4):
                        S.op("scalar", lambda e, h=h: e.activation(out=jk[:, h * 128:(h + 1) * 128],
                                                                   in_=ps[:, h * 128:(h + 1) * 128], func=AF.Square,
                                                                   accum_out=sq[:, h:h + 1]),
                             reads=[pk], writes=[K("junk"), K("ssq4")])
                    S.op("scalar", lambda e: e.activation(out=rs, in_=sq, func=AF.Sqrt, scale=1.0 / 128, bias=eps),
                         reads=[K("ssq4")], writes=[K("rs4")])
                    S.op("vector", lambda e: e.tensor_tensor(out=x_, in0=ps, in1=gg, op=ALU.mult), reads=[pk, ggk], writes=[K("xg")])
                    xv = x_.rearrange("p (x two) -> p x two", two=2)
                    rv = r_.rearrange("p (x two) -> p x two", two=2)
                    x1 = xv[:, :, 0]
                    x2 = xv[:, :, 1]
                    cc = c2[:, 0, :]
                    ss = c2[:, 1, :]
                    S.op("gpsimd", lambda e: e.tensor_tensor(out=t2, in0=x2, in1=ss, op=ALU.mult), reads=[K("xg"), ck], writes=[K("t2")])
                    S.op("gpsimd", lambda e: e.tensor_tensor(out=t3, in0=x1, in1=ss, op=ALU.mult), reads=[K("xg"), ck], writes=[K("t3")])
                    S.op("gpsimd", lambda e: e.tensor_tensor(out=t4, in0=x2, in1=cc, op=ALU.mult), reads=[K("xg"), ck], writes=[K("t4")])
                    S.op("vector", lambda e: e.tensor_tensor(out=t1, in0=x1, in1=cc, op=ALU.mult), reads=[K("xg"), ck], writes=[K("t1")])

                    def stage_b():
                        S.op("vector", lambda e: e.reciprocal(out=rs, in_=rs), reads=[K("rs4")], writes=[K("rs4")])
                        S.op("vector", lambda e: e.tensor_tensor(out=rv[:, :, 0], in0=t1, in1=t2, op=ALU.subtract),
                             reads=[K("t1"), K("t2")], writes=[K("ro")])
                        S.op("vector", lambda e: e.tensor_tensor(out=rv[:, :, 1], in0=t3, in1=t4, op=ALU.add),
                             reads=[K("t3"), K("t4")], writes=[K("ro")])
                        for h in range(4):
                            S.op("vector", lambda e, h=h: e.tensor_scalar(out=q_[:, h * 128:(h + 1) * 128], in0=r_[:, h * 128:(h + 1) * 128],
                                                                         scalar1=rs[:, h:h + 1], scalar2=None, op0=ALU.mult),
                                 reads=[K("ro"), K("rs4")], writes=[K("qr%d" % h)])

                    pb = b.psb(n % 2)
                    pbk = "ps%d" % (n % 2)
                    sg = stage[nb % 2]

                    def stage_c():
                        for h in range(4):
                            S.op("tensor", lambda e, h=h: e.transpose(out=pb[:, h, :], in_=q_[:, h * 128:(h + 1) * 128], identity=identb),
                                 reads=[K("qr%d" % h)], writes=[pbk])
                        S.op("scalar", lambda e: e.copy(out=sg[:, :, ti * 128:(ti + 1) * 128], in_=pb[:, 0:4, :]),
                             reads=[pbk], writes=["stg%d" % (nb % 2)])
                        if ti == TG // 128 - 1:
                            cols = slice(tg * TG, (tg + 1) * TG)
                            if nb < 3:
                                dst = qT_d[nb * 4:(nb + 1) * 4, :, cols]
                            else:
                                dst = kT_d[:, :, cols]
                            S.dma("sync", dst.rearrange("h p t -> p h t"), sg, reads=["stg%d" % (nb % 2)], writes=["qkT_d"])

                    stB[n] = stage_b
                    stC[n] = stage_c
                    run_stage(stB, n - 1)
                    run_stage(stC, n - 2)

                def w_src(nb):
                    return w_in[:, nb * 512:(nb + 1) * 512]

                linear_tok(hT, TG, w_src, 4 * 512, qk_epilogue)
                flush()

                def v_epilogue(nb, ti, ps, pk):
                    vb = vst[ti % 2]
                    S.op("scalar", lambda e: e.copy(out=vb, in_=ps), reads=[pk], writes=["vst%d" % (ti % 2)])
                    S.dma("sync", v_d[tg * TG + ti * 128: tg * TG + (ti + 1) * 128, :], vb, reads=["vst%d" % (ti % 2)], writes=["v_d"])

                linear_tok(hT, TG, lambda nb: w_in[:, 2048:2560], 512, v_epilogue)

                fst = [b.alloc("fst0", [512], BF16), b.alloc("fst1", [512], BF16)]
                fctr = [0]

                def f_epilogue(nt, tb, ps, pk):
                    i = fctr[0] % 2
                    fctr[0] += 1
                    S.op("vector", lambda e: e.tensor_copy(out=fst[i], in_=ps), reads=[pk], writes=["fst%d" % i])
                    S.dma("sync", fT_d[nt, :, tg * TG + tb * 512: tg * TG + (tb + 1) * 512], fst[i],
                          reads=["fst%d" % i], writes=["fT_d"])

                linear_feat(hT, TG, lambda nb: w_in[:, 2560:3072], 512, f_epilogue)

        def phase_attention():
            b.phase()
            kT = b.alloc("kT", [4, S_], BF16)
            V = b.alloc("V", [16, 512], BF16)
            qT = [b.alloc("qT0", [S_], BF16), b.alloc("qT1", [S_], BF16)]
            eT = [b.alloc("eT%d" % i, [512], BF16) for i in range(4)]
            rsum = [b.alloc("rsum0", [512], F32), b.alloc("rsum1", [512], F32)]
            ost = [b.alloc("ost0", [512], BF16), b.alloc("ost1", [512], BF16)]
            S.dma("sync", kT, kT_d.rearrange("h p t -> p h t"), reads=["qkT_d"], writes=["kT"])
            S.dma("sync", V, v_d.rearrange("(j p) d -> p j d", p=128), reads=["v_d"], writes=["V"])
            scale = 128 ** -0.5
            items = [(h, qb, st_) for h in range(12) for qb in range(4) for st_ in range(16)]

            def load_q(h):
                S.dma("sync", qT[h % 2], qT_d[h], reads=["qkT_d"], writes=["qT%d" % (h % 2)])

            def emit_sc(i):
                h, qb, st_ = items[i]
                g = h // 3
                q = qT[h % 2]
                scb = 2 + (i % 2)
                S.op("tensor", lambda e: e.matmul(
                    b.ps(scb), lhsT=kT[:, g, st_ * 128:(st_ + 1) * 128], rhs=q[:, qb * 512:(qb + 1) * 512],
                    start=True, stop=True), reads=["kT", "qT%d" % (h % 2)], writes=["ps%d" % scb])

            load_q(0)
            emit_sc(0)
            for i, (h, qb, st_) in enumerate(items):
                g = h // 3
                blk = i // 16
                ob = 4 + (blk % 2)
                sb_ = 6 + (blk % 2)
                scb = 2 + (i % 2)
                ei = i % 4
                if qb == 0 and st_ == 0 and h + 1 < 12:
                    load_q(h + 1)
                if i + 1 < len(items):
                    emit_sc(i + 1)
                S.op("scalar", lambda e, scb=scb, ei=ei: e.activation(out=eT[ei], in_=b.ps(scb), func=AF.Exp, scale=scale),
                     reads=["ps%d" % scb], writes=["eT%d" % ei])
                S.op("tensor", lambda e, st_=st_, ob=ob, ei=ei, g=g: e.matmul(
                    b.ps(ob), lhsT=V[:, st_, g * 128:(g + 1) * 128], rhs=eT[ei], start=(st_ == 0), stop=(st_ == 15)),
                     reads=["V", "eT%d" % ei], writes=["ps%d" % ob])
                S.op("tensor", lambda e, st_=st_, sb_=sb_, ei=ei: e.matmul(
                    b.ps(sb_), lhsT=onesb, rhs=eT[ei], start=(st_ == 0), stop=(st_ == 15)),
                     reads=["eT%d" % ei], writes=["ps%d" % sb_])
                if st_ == 15:
                    rs = rsum[blk % 2]
                    rk = "rsum%d" % (blk % 2)
                    o = ost[blk % 2]
                    okk = "ost%d" % (blk % 2)
                    S.op("vector", lambda e, sb_=sb_, rs=rs: e.reciprocal(out=rs, in_=b.ps(sb_)), reads=["ps%d" % sb_], writes=[rk])
                    S.op("vector", lambda e, ob=ob, o=o, rs=rs: e.tensor_tensor(out=o, in0=b.ps(ob), in1=rs, op=ALU.mult),
                         reads=["ps%d" % ob, rk], writes=[okk])
                    S.dma("sync", mixT_d[h, :, qb * 512:(qb + 1) * 512], o, reads=[okk], writes=["mixT_d"])

        def phase_fourier():
            b.phase()
            global wring
            wring = [b.alloc("w%d" % i, [8192], BF16) for i in range(4)]
            dC = b.alloc("dC", [2, 128], F32)
            fw = b.alloc("fw", [4, 128], F32)
            AB = b.alloc("AB", [4, 256], BF16)
            fT = b.alloc("fT", [4, S_], BF16)
            P12 = b.alloc("P12", [16, 4 * 256], BF16)
            ost = [b.alloc("ost0", [512], BF16), b.alloc("ost1", [512], BF16)]
            S.dma("sync", dC, c_dftC.rearrange("a p c -> p a c"), writes=["dC"])
            S.dma("sync", fw, fourier_w.rearrange("g p c -> p g c"), writes=["fw"])
            S.dma("sync", fT, fT_d.rearrange("g p t -> p g t"), reads=["fT_d"], writes=["fT"])
            for g in range(4):
                for a in range(2):
                    S.op("tensor", lambda e, g=g, a=a: e.matmul(b.ps(2)[:, a * 128:(a + 1) * 128], lhsT=dC[:, a, :], rhs=fw[:, g, :],
                                                                start=True, stop=True), reads=["dC", "fw"], writes=["ps2"])
                S.op("vector", lambda e, g=g: e.tensor_copy(out=AB[:, g, :], in_=b.ps(2)[:, 0:256]), reads=["ps2"], writes=["AB"])
            for j in range(16):
                for half in range(2):
                    bank = 3 + half
                    for gg in range(2):
                        g = half * 2 + gg
                        S.op("tensor", lambda e, g=g, gg=gg, j=j, bank=bank: e.matmul(
                            b.ps(bank)[:, gg * 256:(gg + 1) * 256], lhsT=fT[:, g, j * 128:(j + 1) * 128], rhs=AB[:, g, :],
                            start=True, stop=True), reads=["fT", "AB"], writes=["ps%d" % bank])
                    eng = "vector" if half == 0 else "scalar"
                    dst = P12[:, j, half * 512:(half + 1) * 512]
                    if eng == "vector":
                        S.op("vector", lambda e, dst=dst, bank=bank: e.tensor_copy(out=dst, in_=b.ps(bank)), reads=["ps%d" % bank], writes=["P12"])
                    else:
                        S.op("scalar", lambda e, dst=dst, bank=bank: e.copy(out=dst, in_=b.ps(bank)), reads=["ps%d" % bank], writes=["P12"])
            oc = 0
            for tb in range(4):
                cw, ck = wpiece(c_dftS[0][:, tb * 512:(tb + 1) * 512].rearrange("(k p) n -> p k n", p=128))
                sw, sk = wpiece(c_dftS[1][:, tb * 512:(tb + 1) * 512].rearrange("(k p) n -> p k n", p=128))
                for g in range(4):
                    bank = 5 + (oc % 2)
                    for j in range(16):
                        S.op("tensor", lambda e, g=g, j=j, bank=bank, cw=cw: e.matmul(
                            b.ps(bank), lhsT=P12[:, j, g * 256:g * 256 + 128], rhs=cw[:, j, :], start=(j == 0), stop=False),
                             reads=["P12", ck], writes=["ps%d" % bank])
                    for j in range(16):
                        S.op("tensor", lambda e, g=g, j=j, bank=bank, sw=sw: e.matmul(
                            b.ps(bank), lhsT=P12[:, j, g * 256 + 128:g * 256 + 256], rhs=sw[:, j, :], start=False, stop=(j == 15)),
                             reads=["P12", sk], writes=["ps%d" % bank])
                    o = ost[oc % 2]
                    okk = "ost%d" % (oc % 2)
                    oc += 1
                    S.op("vector", lambda e, o=o, bank=bank: e.tensor_copy(out=o, in_=b.ps(bank)), reads=["ps%d" % bank], writes=[okk])
                    S.dma("sync", mixT_d[12 + g, :, tb * 512:(tb + 1) * 512], o, reads=[okk], writes=["mixT_d"])

        def phase_outproj(W, X_src, X_dst):
            b.phase()
            wres = [b.alloc("wres%d" % i, [16, 512], BF16) for i in range(4)]
            for nb in range(4):
                S.dma("gpsimd", wres[nb], W[:, nb * 512:(nb + 1) * 512].rearrange("(k p) n -> p k n", p=128), writes=["wres%d" % nb])
            hTs = [b.alloc("hTa", [16, 512], BF16), b.alloc("hTb", [16, 512], BF16)]
            rx = [b.alloc("rxa", [D_], F32), b.alloc("rxb", [D_], F32), b.alloc("rxc", [D_], F32)]
            NT = S_ // 128
            cnt = 0

            def load_r(ti):
                S.dma("sync", rx[ti % 3], X_src[ti * 128:(ti + 1) * 128, :], writes=["rx%d" % (ti % 3)])

            def load_h(tb):
                S.dma("sync", hTs[tb % 2], mixT_d[:, :, tb * 512:(tb + 1) * 512].rearrange("k p t -> p k t"), reads=["mixT_d"],
                      writes=["hTs%d" % (tb % 2)])

            load_h(0)
            load_r(0)
            load_r(1)
            for ti in range(NT):
                tb = ti // 4
                if ti % 4 == 0 and tb + 1 < NT // 4:
                    load_h(tb + 1)
                if ti + 2 < NT:
                    load_r(ti + 2)
                hT = hTs[tb % 2]
                hk = "hTs%d" % (tb % 2)
                r = rx[ti % 3]
                rk = "rx%d" % (ti % 3)
                tc = (ti % 4) * 128
                for nb in range(4):
                    bank = cnt % 8
                    cnt += 1
                    for k in range(16):
                        S.op("tensor", lambda e, k=k, nb=nb, bank=bank, hT=hT, tc=tc: e.matmul(
                            b.ps(bank), lhsT=hT[:, k, tc:tc + 128], rhs=wres[nb][:, k, :], start=(k == 0), stop=(k == 15)),
                             reads=[hk, "wres%d" % nb], writes=["ps%d" % bank])
                    S.op("vector", lambda e, nb=nb, bank=bank, r=r: e.tensor_tensor(out=r[:, nb * 512:(nb + 1) * 512], in0=b.ps(bank),
                                                                                 in1=r[:, nb * 512:(nb + 1) * 512], op=ALU.add),
                         reads=["ps%d" % bank, rk], writes=[rk])
                S.dma("sync", X_dst[ti * 128:(ti + 1) * 128, :], r, reads=[rk], writes=["Xdst"])

        def phase_cross(layer, X_src, X_dst):
            b.phase()
            global wring
            Aq = b.alloc("Aq", [16, 4 * 256], BF16)
            VW = b.alloc("VW", [2, 4 * D_], BF16)
            mark_main = b.off
            wring = [b.alloc("w%d" % i, [8192], BF16) for i in range(3)]
            kTm = b.alloc("kTm", [16, 256], BF16)
            vTm = b.alloc("vTm", [16, 256], BF16)
            memT = b.alloc("hT", [16, 256], BF16)
            WqT = [b.alloc("WqT0", [4, D_], BF16), b.alloc("WqT1", [4, D_], BF16)]
            load_gain(mem_norm[layer])
            norm_loop(lambda ti: mem_in[ti * 128:(ti + 1) * 128, :], 2, memT)
            cnt = 0
            for Wm, dstT, dkey in ((cw_k, kTm, "kTm"), (cw_v, vTm, "vTm")):
                nxt = wpiece(Wm[layer][:, 0:512].rearrange("(k p) n -> p k n", p=128))
                for nb in range(4):
                    wt, wk = nxt
                    if nb + 1 < 4:
                        nxt = wpiece(Wm[layer][:, (nb + 1) * 512:(nb + 2) * 512].rearrange("(k p) n -> p k n", p=128))
                    for nt in range(4):
                        bank = 2 + (cnt % 4)
                        cnt += 1
                        for k in range(16):
                            S.op("tensor", lambda e, k=k, nt=nt, bank=bank, wt=wt: e.matmul(
                                b.ps(bank)[:, 0:256], lhsT=wt[:, k, nt * 128:(nt + 1) * 128], rhs=memT[:, k, :],
                                start=(k == 0), stop=(k == 15)), reads=["hT", wk], writes=["ps%d" % bank])
                        dst = dstT[:, nb * 4 + nt, :]
                        if cnt % 2 == 0:
                            S.op("vector", lambda e, dst=dst, bank=bank: e.tensor_copy(out=dst, in_=b.ps(bank)[:, 0:256]), reads=["ps%d" % bank], writes=[dkey])
                        else:
                            S.op("scalar", lambda e, dst=dst, bank=bank: e.copy(out=dst, in_=b.ps(bank)[:, 0:256]), reads=["ps%d" % bank], writes=[dkey])
            nxt = wpiece(cw_q[layer][:, 0:512].rearrange("(k p) n -> p k n", p=128))
            for h in range(4):
                wt, wk = nxt
                if h + 1 < 4:
                    nxt = wpiece(cw_q[layer][:, (h + 1) * 512:(h + 2) * 512].rearrange("(k p) n -> p k n", p=128))
                WT = WqT[h % 2]
                wtk = "WqT%d" % (h % 2)
                tcn = 0
                for c in range(4):
                    for k0 in (0, 8):
                        half = tcn % 2
                        tcn += 1
                        pb = b.psb(half)
                        for k in range(8):
                            S.op("tensor", lambda e, k=k, k0=k0, c=c, pb=pb, wt=wt: e.transpose(
                                out=pb[:, k, :], in_=wt[:, k0 + k, c * 128:(c + 1) * 128], identity=identb),
                                 reads=[wk], writes=["ps%d" % half])
                        dst = WT[:, c, k0 * 128:(k0 + 8) * 128].rearrange("p (a b) -> p a b", a=8)
                        if half == 0:
                            S.op("vector", lambda e, dst=dst, pb=pb: e.tensor_copy(out=dst, in_=pb), reads=["ps0"], writes=[wtk])
                        else:
                            S.op("scalar", lambda e, dst=dst, pb=pb: e.copy(out=dst, in_=pb), reads=["ps1"], writes=[wtk])
                for k in range(16):
                    bank = 2 + (cnt % 4)
                    cnt += 1
                    for c in range(4):
                        S.op("tensor", lambda e, k=k, c=c, h=h, bank=bank, WT=WT: e.matmul(
                            b.ps(bank)[:, 0:256], lhsT=WT[:, c, k * 128:(k + 1) * 128], rhs=kTm[:, h * 4 + c, :],
                            start=(c == 0), stop=(c == 3)), reads=[wtk, "kTm"], writes=["ps%d" % bank])
                    dst = Aq[:, k, h * 256:(h + 1) * 256]
                    if k % 2 == 0:
                        S.op("vector", lambda e, dst=dst, bank=bank: e.tensor_copy(out=dst, in_=b.ps(bank)[:, 0:256]), reads=["ps%d" % bank], writes=["Aq"])
                    else:
                        S.op("scalar", lambda e, dst=dst, bank=bank: e.copy(out=dst, in_=b.ps(bank)[:, 0:256]), reads=["ps%d" % bank], writes=["Aq"])
            nxt = wpiece(cw_o[layer][:, 0:512].rearrange("(k p) n -> p k n", p=128))
            for nb in range(4):
                wt, wk = nxt
                if nb + 1 < 4:
                    nxt = wpiece(cw_o[layer][:, (nb + 1) * 512:(nb + 2) * 512].rearrange("(k p) n -> p k n", p=128))
                for h in range(4):
                    for mt in range(2):
                        bank = 2 + (cnt % 4)
                        cnt += 1
                        for c in range(4):
                            S.op("tensor", lambda e, c=c, h=h, mt=mt, bank=bank, wt=wt: e.matmul(
                                b.ps(bank), lhsT=vTm[:, h * 4 + c, mt * 128:(mt + 1) * 128], rhs=wt[:, h * 4 + c, :],
                                start=(c == 0), stop=(c == 3)), reads=["vTm", wk], writes=["ps%d" % bank])
                        dst = VW[:, mt, h * D_ + nb * 512: h * D_ + (nb + 1) * 512]
                        if cnt % 2 == 0:
                            S.op("vector", lambda e, dst=dst, bank=bank: e.tensor_copy(out=dst, in_=b.ps(bank)), reads=["ps%d" % bank], writes=["VW"])
                        else:
                            S.op("scalar", lambda e, dst=dst, bank=bank: e.copy(out=dst, in_=b.ps(bank)), reads=["ps%d" % bank], writes=["VW"])
            scale = 512 ** -0.5
            for tg in range(NTG):
                S.barrier()
                b.off = mark_main
                hT = b.alloc("hTq", [16, TG], BF16)
                eTs = [b.alloc("eTs0", [8, 512], BF16), b.alloc("eTs1", [8, 512], BF16)]
                rsum2 = [b.alloc("rsum_a", [512], F32), b.alloc("rsum_b", [512], F32)]
                load_gain(cross_norm[layer])
                norm_loop(lambda ti: X_src[tg * TG + ti * 128: tg * TG + (ti + 1) * 128, :], TG // 128, hT)
                NTB = TG // 512
                rxs = [b.alloc("rxw%d" % i, [D_], F32) for i in range(3)]

                def load_res(n):
                    rows = slice(tg * TG + n * 128, tg * TG + (n + 1) * 128)
                    S.dma("sync", rxs[n % 3], X_src[rows, :], writes=["rxw%d" % (n % 3)])
                scn = [0]
                rcn = [0]

                def sums_norm(tb, h):
                    et = eTs[tb % 2]
                    ek = "eTs%d_%d" % (tb % 2, h)
                    rs = rsum2[rcn[0] % 2]
                    rk = "rsum%d" % (rcn[0] % 2)
                    rcn[0] += 1
                    for mt in range(2):
                        S.op("tensor", lambda e, mt=mt: e.matmul(b.ps(3), lhsT=onesb, rhs=et[:, h * 2 + mt, :], start=(mt == 0), stop=(mt == 1)),
                             reads=[ek], writes=["ps3"])
                    S.op("vector", lambda e: e.reciprocal(out=rs, in_=b.ps(3)), reads=["ps3"], writes=[rk])
                    for mt in range(2):
                        S.op("vector", lambda e, mt=mt: e.tensor_tensor(out=et[:, h * 2 + mt, :], in0=et[:, h * 2 + mt, :], in1=rs, op=ALU.mult),
                             reads=[ek, rk], writes=[ek])

                def stage_scores(tb):
                    et = eTs[tb % 2]
                    for h in range(4):
                        ek = "eTs%d_%d" % (tb % 2, h)
                        for mt in range(2):
                            bank = scn[0] % 3
                            scn[0] += 1
                            for k in range(16):
                                S.op("tensor", lambda e, k=k, h=h, mt=mt, bank=bank: e.matmul(
                                    b.ps(bank), lhsT=Aq[:, k, h * 256 + mt * 128: h * 256 + (mt + 1) * 128],
                                    rhs=hT[:, k, tb * 512:(tb + 1) * 512], start=(k == 0), stop=(k == 15)),
                                     reads=["Aq", "hT"], writes=["ps%d" % bank])
                            S.op("scalar", lambda e, h=h, mt=mt, bank=bank: e.activation(out=et[:, h * 2 + mt, :], in_=b.ps(bank), func=AF.Exp, scale=scale),
                                 reads=["ps%d" % bank], writes=[ek])
                        if h >= 1:
                            sums_norm(tb, h - 1)
                    sums_norm(tb, 3)

                ocn = [0]

                def stage_out(tb):
                    et = eTs[tb % 2]
                    for t4 in range(4):
                        n = tb * 4 + t4
                        if n == 0:
                            load_res(0)
                        if n + 1 < TG // 128:
                            load_res(n + 1)
                        r = rxs[n % 3]
                        rk = "rxw%d" % (n % 3)
                        for nb in range(4):
                            bank = 4 + (ocn[0] % 4)
                            ocn[0] += 1
                            i = 0
                            for h in range(4):
                                for mt in range(2):
                                    S.op("tensor", lambda e, h=h, mt=mt, t4=t4, nb=nb, bank=bank, i=i: e.matmul(
                                        b.ps(bank), lhsT=et[:, h * 2 + mt, t4 * 128:(t4 + 1) * 128],
                                        rhs=VW[:, mt, h * D_ + nb * 512: h * D_ + (nb + 1) * 512], start=(i == 0), stop=(i == 7)),
                                         reads=["eTs%d_%d" % (tb % 2, h), "VW"], writes=["ps%d" % bank])
                                    i += 1
                            S.op("vector", lambda e, nb=nb, bank=bank, r=r: e.tensor_tensor(out=r[:, nb * 512:(nb + 1) * 512], in0=b.ps(bank),
                                                                                         in1=r[:, nb * 512:(nb + 1) * 512], op=ALU.add),
                                 reads=["ps%d" % bank, rk], writes=[rk])
                        S.dma("sync", X_dst[tg * TG + n * 128: tg * TG + (n + 1) * 128, :], r, reads=[rk], writes=["Xdst"])

                stage_scores(0)
                for tb in range(NTB):
                    if tb + 1 < NTB:
                        stage_scores(tb + 1)
                    stage_out(tb)

        def phase_moe(layer, X):
            b.phase()
            global wring
            wring = [b.alloc("w%d" % i, [8192], BF16) for i in range(6)]
            for i_ in range(6):
                src_, kd_ = ((ew_gate if i_ in (0, 2) else ew_up)[layer, 0][:, (i_ // 2) * 512:(i_ // 2 + 1) * 512].rearrange("(k p) n -> p k n", p=128), 16) \
                    if i_ < 4 else (ew_down[layer, 0][:, (i_ - 4) * 1024:(i_ - 3) * 1024].rearrange("(k p) n -> p k n", p=128), 8)
                S.dma("gpsimd", wring[i_].rearrange("p (k n) -> p k n", k=kd_), src_, writes=["w%d" % i_])
            logT = b.alloc("logT", [S_], F32)
            mark_inner = b.off
            xts = [b.alloc("xt0", [D_], F32), b.alloc("xt1", [D_], F32)]
            xnb = [b.alloc("xn0", [D_], BF16), b.alloc("xn1", [D_], BF16)]
            xnf2 = [b.alloc("xnf0", [D_], F32), b.alloc("xnf1", [D_], F32)]
            junk = b.alloc("njunk", [D_], BF16)
            ssq2 = [b.alloc("ssq0", [1], F32), b.alloc("ssq1", [1], F32)]
            rstd2 = [b.alloc("rstd0", [1], F32), b.alloc("rstd1", [1], F32)]
            hTf = b.alloc("hTf", [16, 128], F32)
            wrA = b.alloc("wrA", [16, 48], F32)
            wrB = b.alloc("wrB", [16, 48], F32)
            load_gain(ffn_norm[layer])
            S.op("vector", lambda e: e.memset(wrA, 0.0), writes=["wr"])
            S.op("vector", lambda e: e.memset(wrB, 0.0), writes=["wr"])
            S.dma("sync", wrA[:, :, 0:16], router_w[layer].rearrange("(k p) e -> p k e", p=128), writes=["wr"])
            S.dma("sync", wrB[:, :, 32:48], router_w[layer].rearrange("(k p) e -> p k e", p=128), writes=["wr"])
            S.op("vector", lambda e: e.memset(logT[0:48, 0:1024], -30000.0), writes=["logT"])

            def m_load(ti):
                S.dma("sync", xts[ti % 2], X[ti * 128:(ti + 1) * 128, :], writes=["xt%d" % (ti % 2)])

            def m_early(ti):
                i = ti % 2
                rows = slice(ti * 128, (ti + 1) * 128)
                xt, xn, xnf, ssq, rstd = xts[i], xnb[i], xnf2[i], ssq2[i], rstd2[i]
                S.op("scalar", lambda e: e.activation(out=junk, in_=xt, func=AF.Square, accum_out=ssq),
                     reads=["xt%d" % i], writes=["njunk", "ssq%d" % i])
                S.op("scalar", lambda e: e.activation(out=rstd, in_=ssq, func=AF.Sqrt, scale=1.0 / D_, bias=eps),
                     reads=["ssq%d" % i], writes=["rstd%d" % i])
                S.op("vector", lambda e: e.reciprocal(out=rstd, in_=rstd), reads=["rstd%d" % i], writes=["rstd%d" % i])
                S.op("vector", lambda e: e.scalar_tensor_tensor(out=xnf, in0=xt, scalar=rstd, in1=gb, op0=ALU.mult, op1=ALU.mult),
                     reads=["xt%d" % i, "rstd%d" % i, "gb"], writes=["xnf%d" % i])
                S.op("gpsimd", lambda e: e.tensor_copy(out=xn, in_=xnf), reads=["xnf%d" % i], writes=["xn%d" % i])
                S.dma("sync", hmoe_d[rows, :], xn, reads=["xn%d" % i], writes=["hmoe_d"])

            def m_late(ti):
                i = ti % 2
                xnf = xnf2[i]
                for q4 in range(4):
                    bank = 2 + q4
                    for k in range(4):
                        kk = q4 * 4 + k
                        S.op("tensor", lambda e, k=k, kk=kk, bank=bank: e.transpose(
                            out=b.ps(bank)[:, k * 128:(k + 1) * 128], in_=xnf[:, kk * 128:(kk + 1) * 128], identity=identf),
                             reads=["xnf%d" % i], writes=["ps%d" % bank])
                    dst = hTf[:, q4 * 4:(q4 + 1) * 4, :]
                    if q4 % 2 == 0:
                        S.op("vector", lambda e, dst=dst, bank=bank: e.tensor_copy(
                            out=dst, in_=b.ps(bank).rearrange("p (a b) -> p a b", a=4)), reads=["ps%d" % bank], writes=["hTf%d" % q4])
                    else:
                        S.op("scalar", lambda e, dst=dst, bank=bank: e.copy(
                            out=dst, in_=b.ps(bank).rearrange("p (a b) -> p a b", a=4)), reads=["ps%d" % bank], writes=["hTf%d" % q4])
                wsel = wrA if ti < 8 else wrB
                for k in range(16):
                    S.op("tensor", lambda e, k=k, wsel=wsel: e.matmul(b.ps(6)[0:48, 0:128], lhsT=wsel[:, k, :], rhs=hTf[:, k, :],
                                                                     start=(k == 0), stop=(k == 15)), reads=["wr", "hTf%d" % (k // 4)], writes=["ps6"])
                p0 = 0 if ti < 8 else 32
                tcol = (ti % 8) * 128
                S.op("vector", lambda e, p0=p0, tcol=tcol: e.tensor_copy(out=logT[p0:p0 + 16, tcol:tcol + 128], in_=b.ps(6)[p0:p0 + 16, 0:128]),
                     reads=["ps6"], writes=["logT"])

            m_load(0)
            for ti in range(S_ // 128 + 1):
                if ti + 1 < S_ // 128:
                    m_load(ti + 1)
                if ti < S_ // 128:
                    m_early(ti)
                if ti >= 1:
                    m_late(ti - 1)
            S.barrier()
            b.off = mark_inner
            HS = S_ // 2
            aff = b.alloc("aff", [HS], F32)
            work = b.alloc("work", [HS], F32)
            vals = b.alloc("vals", [CAP], F32)
            idxu = b.alloc("idxu", [CAP], U32)
            idxf = b.alloc("idxf", [CAP], F32)
            ob = b.alloc("ob", [48], F32)
            offs = b.alloc("offs", [1], F32)
            Jm = b.alloc("Jm", [128], F32)
            VT = b.alloc("VT", [2, 48], F32)
            IT = b.alloc("IT", [2, 48], F32)
            BrV = b.alloc("BrV", [2, NE], F32)
            BrI = b.alloc("BrI", [2, NE], F32)
            msk = b.alloc("msk", [2, NE], F32)
            dif = b.alloc("dif", [2, NE], F32)
            gateT = b.alloc("gateT", [2, NE], F32)
            idxT = b.alloc("idxT", [2, NE], I32)
            S.op("gpsimd", lambda e: e.memset(ob[0:48, :], 0.0), writes=["ob"])
            S.op("gpsimd", lambda e: e.memset(ob[0:32, 0:32], 1.0), writes=["ob"])
            S.op("gpsimd", lambda e: e.memset(ob[32:48, 32:48], 1.0), writes=["ob"])
            S.op("gpsimd", lambda e: e.memset(offs[0:48, :], 0.0), writes=["offs"])
            S.op("gpsimd", lambda e: e.memset(offs[32:48, :], float(HS)), writes=["offs"])
            S.op("gpsimd", lambda e: e.memset(Jm, 0.0), writes=["Jm"])
            S.op("gpsimd", lambda e: e.affine_select(out=Jm, in_=Jm, pattern=[[1, 128]], compare_op=ALU.not_equal, fill=1.0,
                                                     base=-127, channel_multiplier=1), reads=["Jm"], writes=["Jm"])
            S.op("scalar", lambda e: e.activation(out=aff[0:48, :], in_=logT[0:48, 0:HS], func=AF.Exp), reads=["logT"], writes=["aff"])
            for tb in range(2):
                S.op("tensor", lambda e, tb=tb: e.matmul(b.ps(2)[0:48, :], lhsT=ob[0:48, 0:48], rhs=aff[0:48, tb * 512:(tb + 1) * 512],
                                                         start=True, stop=True), reads=["aff", "ob"], writes=["ps2"])
                S.op("vector", lambda e, tb=tb: e.reciprocal(out=work[0:48, tb * 512:(tb + 1) * 512], in_=b.ps(2)[0:48, :]),
                     reads=["ps2"], writes=["work"])
            S.op("vector", lambda e: e.tensor_tensor(out=aff[0:48, :], in0=aff[0:48, :], in1=work[0:48, :], op=ALU.mult),
                 reads=["aff", "work"], writes=["aff"])
            S.op("vector", lambda e: e.tensor_copy(out=work[0:48, :], in_=aff[0:48, :]), reads=["aff"], writes=["work"])
            for r in range(CAP // 8):
                S.op("vector", lambda e, r=r: e.max(out=vals[0:48, r * 8:(r + 1) * 8], in_=work[0:48, :]), reads=["work"], writes=["vals"])
                S.op("vector", lambda e, r=r: e.max_index(out=idxu[0:48, r * 8:(r + 1) * 8], in_max=vals[0:48, r * 8:(r + 1) * 8],
                                                          in_values=work[0:48, :]), reads=["work", "vals"], writes=["idxu"])
                S.op("vector", lambda e, r=r: e.match_replace(out=work[0:48, :], in_to_replace=vals[0:48, r * 8:(r + 1) * 8],
                                                              in_values=work[0:48, :], imm_value=-1.0),
                     reads=["work", "vals", "idxu"], writes=["work"])
            S.op("vector", lambda e: e.tensor_copy(out=idxf[0:48, :], in_=idxu[0:48, :]), reads=["idxu"], writes=["idxf"])
            S.op("vector", lambda e: e.tensor_scalar(out=idxf[0:48, :], in0=idxf[0:48, :], scalar1=0.0, scalar2=float(HS - 1),
                                                     op0=ALU.max, op1=ALU.min), reads=["idxf"], writes=["idxf"])
            S.op("vector", lambda e: e.tensor_scalar(out=idxf[0:48, :], in0=idxf[0:48, :], scalar1=offs[0:48, :], scalar2=None,
                                                     op0=ALU.add), reads=["idxf", "offs"], writes=["idxf"])
            for cc in range(2):
                S.op("tensor", lambda e, cc=cc: e.transpose(out=b.ps(3)[:, 0:48], in_=vals[0:48, cc * 128:(cc + 1) * 128],
                                                            identity=identf[0:48, 0:48]), reads=["vals"], writes=["ps3"])
                S.op("vector", lambda e, cc=cc: e.tensor_copy(out=VT[:, cc, :], in_=b.ps(3)[:, 0:48]), reads=["ps3"], writes=["VT"])
                S.op("tensor", lambda e, cc=cc: e.transpose(out=b.ps(4)[:, 0:48], in_=idxf[0:48, cc * 128:(cc + 1) * 128],
                                                            identity=identf[0:48, 0:48]), reads=["idxf"], writes=["ps4"])
                S.op("vector", lambda e, cc=cc: e.tensor_copy(out=IT[:, cc, :], in_=b.ps(4)[:, 0:48]), reads=["ps4"], writes=["IT"])
            for cc in range(2):
                S.op("tensor", lambda e, cc=cc: e.matmul(b.ps(3)[:, 0:16], lhsT=Jm, rhs=VT[:, 1 - cc, 32:48], start=True, stop=True),
                     reads=["Jm", "VT"], writes=["ps3"])
                S.op("vector", lambda e, cc=cc: e.tensor_copy(out=BrV[:, cc, :], in_=b.ps(3)[:, 0:16]), reads=["ps3"], writes=["BrV"])
                S.op("tensor", lambda e, cc=cc: e.matmul(b.ps(4)[:, 0:16], lhsT=Jm, rhs=IT[:, 1 - cc, 32:48], start=True, stop=True),
                     reads=["Jm", "IT"], writes=["ps4"])
                S.op("vector", lambda e, cc=cc: e.tensor_copy(out=BrI[:, cc, :], in_=b.ps(4)[:, 0:16]), reads=["ps4"], writes=["BrI"])
            S.op("vector", lambda e: e.tensor_tensor(out=gateT, in0=VT[:, :, 0:16], in1=BrV, op=ALU.max), reads=["VT", "BrV"], writes=["gateT"])
            S.op("vector", lambda e: e.tensor_tensor(out=msk, in0=VT[:, :, 0:16], in1=BrV, op=ALU.is_gt), reads=["VT", "BrV"], writes=["msk"])
            S.op("vector", lambda e: e.tensor_tensor(out=dif, in0=IT[:, :, 0:16], in1=BrI, op=ALU.subtract), reads=["IT", "BrI"], writes=["dif"])
            S.op("vector", lambda e: e.tensor_tensor(out=dif, in0=dif, in1=msk, op=ALU.mult), reads=["dif", "msk"], writes=["dif"])
            S.op("vector", lambda e: e.tensor_tensor(out=dif, in0=dif, in1=BrI, op=ALU.add), reads=["dif", "BrI"], writes=["dif"])
            S.op("vector", lambda e: e.tensor_copy(out=idxT, in_=dif), reads=["dif"], writes=["idxT"])
            if stage == 30:
                return
            xs = [b.alloc("xs%d" % i, [D_], BF16) for i in range(4)]
            xsT = [b.alloc("xsT0", [16, CAP], BF16), b.alloc("xsT1", [16, CAP], BF16)]
            gT = b.alloc("gT", [8, CAP], BF16)
            sa = [b.alloc("sa0", [CAP], F32), b.alloc("sa1", [CAP], F32)]
            ysb = [b.alloc("ysb0", [D_], F32), b.alloc("ysb1", [D_], F32)]

            def piece_src(ex, i):
                if i in (0, 2):
                    fh = i // 2
                    return ew_gate[layer, ex][:, fh * 512:(fh + 1) * 512].rearrange("(k p) n -> p k n", p=128), 16
                if i in (1, 3):
                    fh = i // 2
                    return ew_up[layer, ex][:, fh * 512:(fh + 1) * 512].rearrange("(k p) n -> p k n", p=128), 16
                nh = i - 4
                return ew_down[layer, ex][:, nh * 1024:(nh + 1) * 1024].rearrange("(k p) n -> p k n", p=128), 8

            def load_piece(ex, i):
                src, kd = piece_src(ex, i)
                S.dma("gpsimd", wring[i].rearrange("p (k n) -> p k n", k=kd), src, writes=["w%d" % i])

            def piece(i, kd):
                return wring[i].rearrange("p (k n) -> p k n", k=kd), "w%d" % i

            def gather(ex):
                for cc in range(2):
                    xi = (2 * ex + cc) % 4
                    S.op("gpsimd", lambda e, xi=xi, cc=cc, ex=ex: e.indirect_dma_start(
                        out=xs[xi], out_offset=None, in_=hmoe_d[:, :],
                        in_offset=bass.IndirectOffsetOnAxis(ap=idxT[:, cc, ex:ex + 1], axis=0)),
                         reads=["hmoe_d", "idxT"], writes=["xs%d" % xi], dma=True)

            def transposes(ex):
                xT = xsT[ex % 2]
                xTk = "xsT%d" % (ex % 2)
                for cc in range(2):
                    xi = (2 * ex + cc) % 4
                    xsb = xs[xi]
                    xk = "xs%d" % xi
                    for half in range(2):
                        pb = b.psb(half)
                        for k in range(8):
                            kk = half * 8 + k
                            S.op("tensor", lambda e, k=k, kk=kk, pb=pb, xsb=xsb: e.transpose(
                                out=pb[:, k, :], in_=xsb[:, kk * 128:(kk + 1) * 128], identity=identb),
                                 reads=[xk], writes=["ps%d" % half])
                        dst = xT[:, half * 8:(half + 1) * 8, cc * 128:(cc + 1) * 128]
                        if half == 0:
                            S.op("vector", lambda e, dst=dst, pb=pb: e.tensor_copy(out=dst, in_=pb), reads=["ps0"], writes=[xTk])
                        else:
                            S.op("scalar", lambda e, dst=dst, pb=pb: e.copy(out=dst, in_=pb), reads=["ps1"], writes=[xTk])

            hcn = [0]

            def hidden(ex, fh):
                xT = xsT[ex % 2]
                xTk = "xsT%d" % (ex % 2)
                wg, wgk = piece(2 * fh, 16)
                wu, wuk = piece(2 * fh + 1, 16)
                for ft in range(4):
                    hc = hcn[0]
                    hcn[0] += 1
                    bg = 2 + 2 * (hc % 2)
                    bu = bg + 1
                    si = hc % 2
                    for k in range(16):
                        S.op("tensor", lambda e, k=k, ft=ft, bg=bg, wg=wg, xT=xT: e.matmul(
                            b.ps(bg)[:, 0:256], lhsT=wg[:, k, ft * 128:(ft + 1) * 128], rhs=xT[:, k, :],
                            start=(k == 0), stop=(k == 15)), reads=[wgk, xTk], writes=["ps%d" % bg])
                    for k in range(16):
                        S.op("tensor", lambda e, k=k, ft=ft, bu=bu, wu=wu, xT=xT: e.matmul(
                            b.ps(bu)[:, 0:256], lhsT=wu[:, k, ft * 128:(ft + 1) * 128], rhs=xT[:, k, :],
                            start=(k == 0), stop=(k == 15)), reads=[wuk, xTk], writes=["ps%d" % bu])
                    S.op("scalar", lambda e, bg=bg, si=si: e.activation(out=sa[si], in_=b.ps(bg)[:, 0:256], func=AF.Silu),
                         reads=["ps%d" % bg], writes=["sa%d" % si])
                    S.op("vector", lambda e, bu=bu, si=si, fh=fh, ft=ft: e.tensor_tensor(
                        out=gT[:, fh * 4 + ft, :], in0=b.ps(bu)[:, 0:256], in1=sa[si], op=ALU.mult),
                         reads=["ps%d" % bu, "sa%d" % si], writes=["gT"])

            dcn = [0]

            def down(ex, nh):
                wdv, wdk = piece(4 + nh, 8)
                for cc in range(2):
                    for nbk in range(2):
                        bank = 6 + (dcn[0] % 2)
                        dcn[0] += 1
                        for ft in range(8):
                            S.op("tensor", lambda e, ft=ft, cc=cc, nbk=nbk, bank=bank, wdv=wdv: e.matmul(
                                b.ps(bank), lhsT=gT[:, ft, cc * 128:(cc + 1) * 128], rhs=wdv[:, ft, nbk * 512:(nbk + 1) * 512],
                                start=(ft == 0), stop=(ft == 7)), reads=["gT", wdk], writes=["ps%d" % bank])
                        dst = ysb[cc][:, nh * 1024 + nbk * 512: nh * 1024 + (nbk + 1) * 512]
                        if nbk == 0:
                            S.op("vector", lambda e, dst=dst, bank=bank, cc=cc, ex=ex: e.tensor_scalar(
                                out=dst, in0=b.ps(bank), scalar1=gateT[:, cc, ex:ex + 1], scalar2=None, op0=ALU.mult),
                                 reads=["ps%d" % bank, "gateT"], writes=["ysb%d" % cc])
                        else:
                            S.op("scalar", lambda e, dst=dst, bank=bank, cc=cc, ex=ex: e.activation(
                                out=dst, in_=b.ps(bank), func=AF.Copy, scale=gateT[:, cc, ex:ex + 1]),
                                 reads=["ps%d" % bank, "gateT"], writes=["ysb%d" % cc])

            def scatter(ex):
                for cc in range(2):
                    S.op("gpsimd", lambda e, cc=cc, ex=ex: e.indirect_dma_start(
                        out=X[:, :], out_offset=bass.IndirectOffsetOnAxis(ap=idxT[:, cc, ex:ex + 1], axis=0),
                        in_=ysb[cc], in_offset=None, compute_op=ALU.add),
                         reads=["ysb%d" % cc, "idxT"], writes=["Xmoe"], dma=True)

            gather(0)
            for ex in range(NE):
                nxt = ex + 1 < NE
                if nxt:
                    gather(ex + 1)
                transposes(ex)
                hidden(ex, 0)
                if nxt:
                    load_piece(ex + 1, 0)
                    load_piece(ex + 1, 1)
                hidden(ex, 1)
                if nxt:
                    load_piece(ex + 1, 2)
                    load_piece(ex + 1, 3)
                down(ex, 0)
                if nxt:
                    load_piece(ex + 1, 4)
                down(ex, 1)
                if nxt:
                    load_piece(ex + 1, 5)
                scatter(ex)

        def phase_pool(X_src, X_dst):
            b.phase()
            global wring
            wring = [b.alloc("w%d" % i, [2048], BF16) for i in range(4)]
            hT = b.alloc("hT", [16, S_], BF16)
            xa = b.alloc("xa", [16, D_], BF16)
            strips = b.alloc("strips", [4, 6 * 512], BF16)
            edges = b.alloc("edges", [4, 2 * 128], BF16)
            scale_b = [b.alloc("scale_b0", [512], F32), b.alloc("scale_b1", [512], F32)]
            load_gain(mix_norm[1])
            wts = [wring[g].rearrange("p (k n) -> p k n", k=4) for g in range(4)]

            def load_consts():
                S.dma("gpsimd", strips.rearrange("p w (k n) -> p w k n", k=6), c_band.rearrange("w k p n -> p w k n"), writes=["strips"])
                S.dma("gpsimd", edges.rearrange("p w (k n) -> p w k n", k=2), c_bedge.rearrange("w k p n -> p w k n"), writes=["edges"])
                for g in range(4):
                    S.dma("gpsimd", wts[g][:, 0:4, :], pool_w[g].rearrange("(k p) n -> p k n", p=128), writes=["w%d" % g])
                for g in range(4):
                    sbg = scale_b[g % 2]
                    S.dma("gpsimd", sbg, pool_scale[g * 512:(g + 1) * 512].partition_broadcast(128), writes=["scale_b%d" % (g % 2)])
                    for k in range(4):
                        S.op("gpsimd", lambda e, g=g, k=k, sbg=sbg: e.tensor_tensor(out=wts[g][:, k, :], in0=wts[g][:, k, :], in1=sbg, op=ALU.mult),
                             reads=["w%d" % g, "scale_b%d" % (g % 2)], writes=["w%d" % g])
            xts = [b.alloc("xt0", [D_], F32), b.alloc("xt1", [D_], F32)]
            ssq2 = [b.alloc("ssq0", [1], F32), b.alloc("ssq1", [1], F32)]
            rstd2 = [b.alloc("rstd0", [1], F32), b.alloc("rstd1", [1], F32)]
            NT = S_ // 128
            S.dma("sync", xts[0], X_src[0:128, :], writes=["xt0"])
            for ti in range(NT):
                i = ti % 2
                xt, ssq, rstd = xts[i], ssq2[i], rstd2[i]
                if ti + 1 < NT:
                    S.dma("sync", xts[1 - i], X_src[(ti + 1) * 128:(ti + 2) * 128, :], writes=["xt%d" % (1 - i)])
                S.op("scalar", lambda e, xt=xt, ssq=ssq, ti=ti: e.activation(out=xa[:, ti, :], in_=xt, func=AF.Square, accum_out=ssq),
                     reads=["xt%d" % i], writes=["xa%d" % ti, "ssq%d" % i])
                S.op("scalar", lambda e, ssq=ssq, rstd=rstd: e.activation(out=rstd, in_=ssq, func=AF.Sqrt, scale=1.0 / D_, bias=eps),
                     reads=["ssq%d" % i], writes=["rstd%d" % i])
                S.op("vector", lambda e, rstd=rstd: e.reciprocal(out=rstd, in_=rstd), reads=["rstd%d" % i], writes=["rstd%d" % i])
                S.op("vector", lambda e, xt=xt, rstd=rstd, ti=ti: e.scalar_tensor_tensor(out=xa[:, ti, :], in0=xt, scalar=rstd, in1=gb,
                                                                                        op0=ALU.mult, op1=ALU.mult),
                     reads=["xt%d" % i, "rstd%d" % i, "gb"], writes=["xa%d" % ti])
                if ti == 2:
                    load_consts()
            sv = strips.rearrange("p w (k n) -> p w k n", k=6)
            ev = edges.rearrange("p w (k n) -> p w k n", k=2)
            pc = 0
            for J in range(4):
                for ch in range(16):
                    wi = ch // 4
                    bank = pc % 8
                    pc += 1
                    pk = "ps%d" % bank
                    for pos in range(4):
                        Sx = 4 * J + pos
                        kidx = pos
                        if Sx == 0:
                            kidx = 4
                        elif Sx == NT - 1:
                            kidx = 5
                        S.op("tensor", lambda e, Sx=Sx, ch=ch, wi=wi, kidx=kidx, bank=bank, pos=pos: e.matmul(
                            b.ps(bank), lhsT=xa[:, Sx, ch * 128:(ch + 1) * 128], rhs=sv[:, wi, kidx, :],
                            start=(pos == 0), stop=False), reads=["xa%d" % Sx, "strips"], writes=[pk])
                    last_full = True
                    if J > 0:
                        S.op("tensor", lambda e, J=J, ch=ch, wi=wi, bank=bank: e.matmul(
                            b.ps(bank)[:, 0:128], lhsT=xa[:, 4 * J - 1, ch * 128:(ch + 1) * 128], rhs=ev[:, wi, 0, :],
                            start=False, stop=False), reads=["xa%d" % (4 * J - 1), "edges"], writes=[pk])
                    if J < 3:
                        S.op("tensor", lambda e, J=J, ch=ch, wi=wi, bank=bank: e.matmul(
                            b.ps(bank)[:, 384:512], lhsT=xa[:, 4 * J + 4, ch * 128:(ch + 1) * 128], rhs=ev[:, wi, 1, :],
                            start=False, stop=False), reads=["xa%d" % (4 * J + 4), "edges"], writes=[pk])
                    dst = hT[:, ch, J * 512:(J + 1) * 512]
                    if pc % 2 == 0:
                        S.op("vector", lambda e, dst=dst, bank=bank: e.tensor_copy(out=dst, in_=b.ps(bank)), reads=[pk], writes=["hTc%d_%d" % (ch, J)])
                    else:
                        S.op("scalar", lambda e, dst=dst, bank=bank: e.copy(out=dst, in_=b.ps(bank)), reads=[pk], writes=["hTc%d_%d" % (ch, J)])
            for ti in range(S_ // 128):
                i = ti % 2
                r = xts[i]
                rk = "xt%d" % i
                rows = slice(ti * 128, (ti + 1) * 128)
                S.dma("sync", r, X_src[rows, :], writes=[rk])
                for g in range(4):
                    bank = (ti * 4 + g) % 8
                    for k in range(4):
                        S.op("tensor", lambda e, k=k, ti=ti, g=g, bank=bank: e.matmul(
                            b.ps(bank), lhsT=hT[:, g * 4 + k, ti * 128:(ti + 1) * 128], rhs=wts[g][:, k, :],
                            start=(k == 0), stop=(k == 3)), reads=["hTc%d_%d" % (g * 4 + k, ti // 4), "w%d" % g], writes=["ps%d" % bank])
                    S.op("vector", lambda e, g=g, bank=bank, r=r: e.tensor_tensor(out=r[:, g * 512:(g + 1) * 512], in0=b.ps(bank),
                                                                               in1=r[:, g * 512:(g + 1) * 512], op=ALU.add),
                         reads=["ps%d" % bank, rk], writes=[rk])
                S.dma("sync", X_dst[rows, :], r, reads=[rk], writes=["Xdst"])

        def phase_final(X_src):
            b.phase()
            xts = [b.alloc("xt0", [D_], F32), b.alloc("xt1", [D_], F32)]
            junk = b.alloc("njunk", [D_], BF16)
            ssq2 = [b.alloc("ssq0", [1], F32), b.alloc("ssq1", [1], F32)]
            rstd2 = [b.alloc("rstd0", [1], F32), b.alloc("rstd1", [1], F32)]
            yo = [b.alloc("yo0", [D_], F32), b.alloc("yo1", [D_], F32)]
            load_gain(final_norm)
            for ti in range(S_ // 128):
                rows = slice(ti * 128, (ti + 1) * 128)
                i = ti % 2
                xt, ssq, rstd, y = xts[i], ssq2[i], rstd2[i], yo[i]
                if ti == 0:
                    S.dma("sync", xt, X_src[rows, :], writes=["xt%d" % i])
                if ti + 1 < S_ // 128:
                    S.dma("sync", xts[1 - i], X_src[(ti + 1) * 128:(ti + 2) * 128, :], writes=["xt%d" % (1 - i)])
                S.op("scalar", lambda e, xt=xt, ssq=ssq: e.activation(out=junk, in_=xt, func=AF.Square, accum_out=ssq),
                     reads=["xt%d" % i], writes=["njunk", "ssq%d" % i])
                S.op("scalar", lambda e, ssq=ssq, rstd=rstd: e.activation(out=rstd, in_=ssq, func=AF.Sqrt, scale=1.0 / D_, bias=eps),
                     reads=["ssq%d" % i], writes=["rstd%d" % i])
                S.op("vector", lambda e, rstd=rstd: e.reciprocal(out=rstd, in_=rstd), reads=["rstd%d" % i], writes=["rstd%d" % i])
                S.op("vector", lambda e, xt=xt, y=y, rstd=rstd: e.scalar_tensor_tensor(out=y, in0=xt, scalar=rstd, in1=gb, op0=ALU.mult, op1=ALU.mult),
                     reads=["xt%d" % i, "rstd%d" % i, "gb"], writes=["yo%d" % i])
                S.dma("sync", y_out[rows, :], y, reads=["yo%d" % i], writes=["y"])

        def copy_out(X_src):
            b.phase()
            t = [b.alloc("co0", [D_], F32), b.alloc("co1", [D_], F32)]
            for ti in range(S_ // 128):
                rows = slice(ti * 128, (ti + 1) * 128)
                S.dma("sync", t[ti % 2], X_src[rows, :], writes=["co%d" % (ti % 2)])
                S.dma("sync", y_out[rows, :], t[ti % 2], reads=["co%d" % (ti % 2)], writes=["y"])

        def run():
            phase_attn_proj()
            phase_attention()
            phase_fourier()
            phase_outproj(w_out, x_in, XB)
            if stage == 1:
                return copy_out(XB)
            phase_cross(0, XB, XA)
            if stage == 2:
                return copy_out(XA)
            phase_moe(0, XA)
            if stage == 30:
                return
            if stage == 3:
                return copy_out(XA)
            phase_pool(XA, XB)
            if stage == 4:
                return copy_out(XB)
            phase_cross(1, XB, XA)
            if stage == 5:
                return copy_out(XA)
            phase_moe(1, XA)
            if stage == 6:
                return copy_out(XA)
            phase_final(XA)

        run()
        counts = S.finalize()
        print("instr counts", counts, flush=True)
    return nc


wring = []


def host_constants():
    S = S_
    rows = S // 64
    row_idx = np.repeat(np.arange(rows), 64).astype(np.float32)
    col_idx = np.tile(np.arange(64), rows).astype(np.float32)
    inv_freq = (1.0 / (10000.0 ** (np.arange(0, 64, 2, dtype=np.float32) / 64))).astype(np.float32)
    ang = np.concatenate([row_idx[:, None] * inv_freq[None, :], col_idx[:, None] * inv_freq[None, :]], axis=-1)
    cos = np.cos(ang).astype(np.float32)
    sin = np.sin(ang).astype(np.float32)
    c_rope = np.stack([np.tile(cos, (1, 4)), np.tile(sin, (1, 4))]).astype(np.float32)
    n = np.arange(S, dtype=np.int64)
    ph = (np.outer(n, n) % S).astype(np.float64) * (2 * np.pi / S)
    c_dftS = np.stack([np.cos(ph), np.sin(ph)]).astype(np.float32)
    c = np.arange(128, dtype=np.int64)
    pc = (np.outer(c, c) % 128).astype(np.float64) * (2 * np.pi / 128)
    c_dftC = np.stack([np.cos(pc) / 512.0, -np.sin(pc) / 512.0]).astype(np.float32)
    t = np.arange(S)
    bands = []
    bedges = []
    for w in (2, 4, 8, 16):
        lo = np.clip(t - w // 2, 0, S)
        hi = np.clip(t + w - w // 2, 0, S)
        M = np.zeros((S, S), np.float32)
        for tt_ in range(S):
            M[lo[tt_]:hi[tt_], tt_] = 1.0 / float(hi[tt_] - lo[tt_])
        M[np.arange(S), np.arange(S)] -= 1.0
        st = [M[(4 + p) * 128:(5 + p) * 128, 512:1024] for p in range(4)]
        st.append(M[0:128, 0:512])
        st.append(M[S - 128:S, S - 512:S])
        bands.append(np.stack(st))
        bedges.append(np.stack([M[3 * 128:4 * 128, 512:640], M[8 * 128:9 * 128, 7 * 128:8 * 128]]))
    c_band = np.stack(bands).astype(np.float32)
    c_bedge = np.stack(bedges).astype(np.float32)
    return {"c_rope": c_rope, "c_dftS": c_dftS, "c_dftC": c_dftC, "c_band": c_band, "c_bedge": c_bedge}


def make_in_maps(inputs, n_cores=8):
    g = {k: np.ascontiguousarray(np.asarray(v), dtype=np.float32) for k, v in inputs.items()}
    shared = {
        "mix_norm": g["mix_norm"], "attn_w_in": g["attn_w_in"][0], "q_gain": g["q_gain"][0], "k_gain": g["k_gain"][0],
        "fourier_w": g["fourier_w"][0], "attn_w_out": g["attn_w_out"][0], "pool_w": g["pool_w"][0],
        "pool_scale": g["pool_scale"][0], "cross_norm": g["cross_norm"], "mem_norm": g["mem_norm"],
        "cross_w_q": g["cross_w_q"], "cross_w_k": g["cross_w_k"], "cross_w_v": g["cross_w_v"], "cross_w_o": g["cross_w_o"],
        "ffn_norm": g["ffn_norm"], "router_w": g["router_w"], "expert_w_gate": g["expert_w_gate"],
        "expert_w_up": g["expert_w_up"], "expert_w_down": g["expert_w_down"], "final_norm": g["final_norm"],
    }
    shared.update(host_constants())
    maps = []
    for c in range(n_cores):
        m = dict(shared)
        m["x"] = g["x"][c % 4]
        m["mem"] = g["mem"][c % 4]
        maps.append(m)
    return maps


def kernel(**inputs):
    nc = build_program()
    maps = make_in_maps(inputs, 4)
    res = run_bass_kernel_spmd(nc, maps, core_ids=list(range(4)))
    out = np.stack([np.asarray(res.results[c]["y"], dtype=np.float32) for c in range(4)], axis=0)
    return out
```
